# Optimizing a Trainium2 kernel written in Bass

```python
import jax, jax.numpy as jnp
from jax import lax
import numpy as np

D_MODEL = 2048
BATCH = 8
SEQ = 2048
DEPTH = 2

MOBA_HEADS = 8
MOBA_HEAD_DIM = 128
MOBA_BLOCK = 256
MOBA_TOPK = 3
MOBA_Q_CHUNK = 64
RET_HEADS = 4
RET_KEY_DIM = 256
RET_VALUE_DIM = 512
RET_CHUNK = 128
ROPE_BASE = 10000.0
MEM_LEN = 256
MEM_HEADS = 4
MEM_HEAD_DIM = 256
N_BRANCHES = 3
MOBA_WIDTH = MOBA_HEADS * MOBA_HEAD_DIM
RET_QK_WIDTH = RET_HEADS * RET_KEY_DIM
RET_V_WIDTH = RET_HEADS * RET_VALUE_DIM
MEM_WIDTH = MEM_HEADS * MEM_HEAD_DIM
IN_WIDTH = 3 * MOBA_WIDTH + 2 * RET_QK_WIDTH + 2 * RET_V_WIDTH + MEM_WIDTH + N_BRANCHES * D_MODEL
N_GROUPS = 4
EXPERTS_PER_GROUP = 8
N_EXPERTS = N_GROUPS * EXPERTS_PER_GROUP
EXPERT_TOPK = 2
EXPERT_FF = 512
EXPERT_BLOCK = 128
DEEPNORM_ALPHA = (2 * DEPTH) ** 0.25
DEEPNORM_BETA = (8 * DEPTH) ** -0.25
LN_EPS = 1e-5
GN_EPS = 1e-6
NEG_INF = -1e30

kernel_name = "hybrid_moba_retention_memory_hmoe_deepnorm"

F32 = jnp.float32


def layer_norm(x, g, b):
    xf = x.astype(F32)
    mu = xf.mean(-1, keepdims=True)
    var = jnp.mean(jnp.square(xf - mu), -1, keepdims=True)
    return ((xf - mu) * lax.rsqrt(var + LN_EPS) * g.astype(F32) + b.astype(F32)).astype(x.dtype)


def rotary(x, pos):
    half = x.shape[-1] // 2
    inv = ROPE_BASE ** (-jnp.linspace(0.0, 1.0, half, dtype=F32))
    ang = pos.astype(F32)[:, None] * inv[None, :]
    cos = jnp.cos(ang)[None, :, None, :]
    sin = jnp.sin(ang)[None, :, None, :]
    x1 = x[..., :half].astype(F32)
    x2 = x[..., half:].astype(F32)
    return jnp.concatenate([x1 * cos - x2 * sin, x1 * sin + x2 * cos], -1).astype(x.dtype)


def moba_attention(q, k, v):
    bsz, seq, nh, dh = q.shape
    n_blk = -(-seq // MOBA_BLOCK)
    s_pad = n_blk * MOBA_BLOCK
    top_n = min(MOBA_TOPK, n_blk)
    padw = ((0, 0), (0, s_pad - seq), (0, 0), (0, 0))
    qh = jnp.pad(q, padw).transpose(0, 2, 1, 3)
    kb = jnp.pad(k, padw).transpose(0, 2, 1, 3).reshape(bsz, nh, n_blk, MOBA_BLOCK, dh)
    vb = jnp.pad(v, padw).transpose(0, 2, 1, 3).reshape(bsz, nh, n_blk, MOBA_BLOCK, dh)
    k_mean = kb.astype(F32).mean(axis=3)
    q_blk = jnp.arange(s_pad) // MOBA_BLOCK
    past = jnp.arange(n_blk)[None, :] < q_blk[:, None]
    gate = jnp.einsum('bhsd,bhnd->bhsn', qh.astype(F32), k_mean)
    gate = jnp.where(past, gate, NEG_INF)
    _, sel = lax.top_k(gate, top_n)
    valid = sel < q_blk[:, None]
    scale = dh ** -0.5
    n_qc = s_pad // MOBA_Q_CHUNK

    def chunk_major(t):
        t = t.reshape(bsz, nh, n_qc, MOBA_Q_CHUNK, *t.shape[3:])
        return jnp.moveaxis(t, 2, 1)

    qc, selc, validc = chunk_major(qh), chunk_major(sel), chunk_major(valid)

    def one_batch(args):
        q_b, sel_b, valid_b, k_b, v_b = args

        def one_chunk(cargs):
            c, q_c, sel_c, valid_c = cargs
            k_sel = jax.vmap(lambda kh, sh: kh[sh])(k_b, sel_c)
            v_sel = jax.vmap(lambda vh, sh: vh[sh])(v_b, sel_c)
            s_sel = jnp.einsum('hqd,hqnkd->hqnk', q_c, k_sel, preferred_element_type=F32) * scale
            s_sel = jnp.where(valid_c[..., None], s_sel, NEG_INF)
            s_sel = s_sel.reshape(nh, MOBA_Q_CHUNK, top_n * MOBA_BLOCK)
            q_pos = c * MOBA_Q_CHUNK + jnp.arange(MOBA_Q_CHUNK)
            own = (c * MOBA_Q_CHUNK) // MOBA_BLOCK
            k_own = lax.dynamic_index_in_dim(k_b, own, axis=1, keepdims=False)
            v_own = lax.dynamic_index_in_dim(v_b, own, axis=1, keepdims=False)
            k_pos = own * MOBA_BLOCK + jnp.arange(MOBA_BLOCK)
            s_own = jnp.einsum('hqd,hkd->hqk', q_c, k_own, preferred_element_type=F32) * scale
            s_own = jnp.where(k_pos[None, :] <= q_pos[:, None], s_own, NEG_INF)
            p = jax.nn.softmax(jnp.concatenate([s_sel, s_own], -1), axis=-1)
            p_sel = p[..., :top_n * MOBA_BLOCK].reshape(nh, MOBA_Q_CHUNK, top_n, MOBA_BLOCK).astype(v_b.dtype)
            p_own = p[..., top_n * MOBA_BLOCK:].astype(v_b.dtype)
            return (jnp.einsum('hqnk,hqnkd->hqd', p_sel, v_sel)
                    + jnp.einsum('hqk,hkd->hqd', p_own, v_own))

        return lax.map(one_chunk, (jnp.arange(n_qc), q_b, sel_b, valid_b))

    out = lax.map(one_batch, (qc, selc, validc, kb, vb))
    out = jnp.moveaxis(out, 2, 1).reshape(bsz, nh, s_pad, dh)[:, :, :seq]
    return out.transpose(0, 2, 1, 3).reshape(bsz, seq, nh * dh)


def retention(q, k, v, g):
    bsz, seq, nh, dk = q.shape
    dv = v.shape[-1]
    pos = jnp.arange(seq)
    q = rotary(q, pos)
    k = rotary(k, pos) * (dk ** -0.5)
    n_c = seq // RET_CHUNK
    log_g = jnp.log1p(-jnp.power(2.0, -5.0 - jnp.arange(nh, dtype=F32)))
    idx = jnp.arange(RET_CHUNK, dtype=F32)
    diff = idx[:, None] - idx[None, :]
    decay = jnp.where(diff >= 0, jnp.exp(log_g[:, None, None] * jnp.maximum(diff, 0.0)), 0.0)

    def to_chunks(t):
        return t.reshape(bsz, n_c, RET_CHUNK, nh, t.shape[-1]).transpose(0, 3, 1, 2, 4).astype(F32)

    qc, kc, vc = to_chunks(q), to_chunks(k), to_chunks(v)
    scores = jnp.einsum('bhnid,bhnjd->bhnij', qc, kc) * decay[None, :, None]
    inner = jnp.einsum('bhnij,bhnje->bhnie', scores, vc)
    zeta = jnp.exp(log_g[:, None] * (RET_CHUNK - 1 - idx)[None, :])
    kv = jnp.einsum('bhnjd,bhnje->nbhde', kc * zeta[None, :, None, :, None], vc)
    chunk_decay = jnp.exp(log_g * RET_CHUNK)[None, :, None, None]

    def step(state, kv_n):
        return chunk_decay * state + kv_n, state

    _, states = lax.scan(step, jnp.zeros((bsz, nh, dk, dv), F32), kv)
    xi = jnp.exp(log_g[:, None] * (idx + 1.0)[None, :])
    cross = jnp.einsum('bhnid,nbhde->bhnie', qc * xi[None, :, None, :, None], states)
    o = (inner + cross).transpose(0, 2, 3, 1, 4).reshape(bsz, seq, nh, dv)
    mu = o.mean(-1, keepdims=True)
    var = jnp.mean(jnp.square(o - mu), -1, keepdims=True)
    o = ((o - mu) * lax.rsqrt(var + GN_EPS)).reshape(bsz, seq, nh * dv)
    return (jax.nn.silu(g.astype(F32)) * o).astype(v.dtype)


def memory_attention(qm, mem, w_mem_kv):
    bsz, seq, _ = qm.shape
    kv = mem @ w_mem_kv
    km, vm = jnp.split(kv, 2, axis=-1)
    q = qm.reshape(bsz, seq, MEM_HEADS, MEM_HEAD_DIM)
    km = km.reshape(bsz, -1, MEM_HEADS, MEM_HEAD_DIM)
    vm = vm.reshape(bsz, -1, MEM_HEADS, MEM_HEAD_DIM)
    s = jnp.einsum('bshd,bmhd->bhsm', q, km, preferred_element_type=F32) * (MEM_HEAD_DIM ** -0.5)
    p = jax.nn.softmax(s, axis=-1).astype(vm.dtype)
    return jnp.einsum('bhsm,bmhd->bshd', p, vm).reshape(bsz, seq, MEM_WIDTH)


def hybrid_mixer(u, mem, w_in, p_moba, p_ret, p_mem, w_mem_kv, w_o):
    bsz, seq, _ = u.shape
    proj = u @ w_in
    sizes = ((MOBA_WIDTH,) * 3 + (RET_QK_WIDTH,) * 2 + (RET_V_WIDTH,) * 2
             + (MEM_WIDTH,) + (D_MODEL,) * N_BRANCHES)
    cuts = [int(c) for c in np.cumsum(sizes)[:-1]]
    aq, ak, av, rq, rk, rv, rg, mq, ga, gr, gm = jnp.split(proj, cuts, axis=-1)

    def heads(t, h):
        return t.reshape(bsz, seq, h, -1)

    y_a = moba_attention(heads(aq, MOBA_HEADS), heads(ak, MOBA_HEADS), heads(av, MOBA_HEADS))
    y_r = retention(heads(rq, RET_HEADS), heads(rk, RET_HEADS), heads(rv, RET_HEADS), rg)
    y_m = memory_attention(mq, mem, w_mem_kv)
    merged = (jax.nn.sigmoid(ga) * (y_a @ p_moba)
              + jax.nn.sigmoid(gr) * (y_r @ p_ret)
              + jax.nn.sigmoid(gm) * (y_m @ p_mem))
    return merged @ w_o


def hier_moe(x, w_group, b_group, w_expert, b_expert, w_gate_up, w_down):
    bsz, seq, d = x.shape
    n_tok = bsz * seq
    xt = x.reshape(n_tok, d)
    g_logits = (xt @ w_group).astype(F32) + b_group.astype(F32)
    g_prob = jax.nn.softmax(g_logits, axis=-1)
    _, g_sel = lax.top_k(g_logits, 1)
    p_g = jnp.take_along_axis(g_prob, g_sel, axis=1)
    e_logits = ((xt @ w_expert).astype(F32) + b_expert.astype(F32)).reshape(n_tok, N_GROUPS, EXPERTS_PER_GROUP)
    e_in = jnp.take_along_axis(e_logits, g_sel[:, :, None], axis=1)[:, 0]
    top_v, top_i = lax.top_k(e_in, EXPERT_TOPK)
    weights = p_g * jax.nn.softmax(top_v, axis=-1)
    expert_id = g_sel * EXPERTS_PER_GROUP + top_i

    n_asg = n_tok * EXPERT_TOPK
    flat_e = expert_id.reshape(n_asg)
    flat_t = jnp.repeat(jnp.arange(n_tok, dtype=jnp.int32), EXPERT_TOPK)
    flat_w = weights.reshape(n_asg)
    order = jnp.argsort(flat_e)
    e_s, t_s, w_s = flat_e[order], flat_t[order], flat_w[order]
    counts = jnp.bincount(flat_e, length=N_EXPERTS)
    starts = jnp.cumsum(counts) - counts
    padded = ((counts + EXPERT_BLOCK - 1) // EXPERT_BLOCK) * EXPERT_BLOCK
    pad_ends = jnp.cumsum(padded)
    pad_starts = pad_ends - padded
    dest = pad_starts[e_s] + (jnp.arange(n_asg) - starts[e_s])
    n_pad = (-(-n_asg // EXPERT_BLOCK) + N_EXPERTS) * EXPERT_BLOCK
    row_tok = jnp.zeros((n_pad,), jnp.int32).at[dest].set(t_s)
    row_w = jnp.zeros((n_pad,), F32).at[dest].set(w_s)
    n_blocks = n_pad // EXPERT_BLOCK
    block_e = jnp.clip(jnp.searchsorted(pad_ends, jnp.arange(n_blocks) * EXPERT_BLOCK, side='right'),
                       0, N_EXPERTS - 1)
    x_rows = xt[row_tok].reshape(n_blocks, EXPERT_BLOCK, d)

    def run_block(args):
        xb, e = args
        gate, up = jnp.split(xb @ w_gate_up[e], 2, axis=-1)
        return (jax.nn.silu(gate) * up) @ w_down[e]

    y_rows = lax.map(run_block, (x_rows, block_e)).reshape(n_pad, d)
    out = jax.ops.segment_sum(y_rows * row_w[:, None].astype(y_rows.dtype), row_tok, num_segments=n_tok)
    return out.reshape(bsz, seq, d)


def setup_inputs(seed: int = 0) -> dict:
    key = jax.random.key(seed)
    ks = jax.random.split(key, 18)

    def nrm(k, shape, scale):
        return jax.random.normal(k, shape, F32) * scale

    L = DEPTH
    return {
        "x": nrm(ks[0], (BATCH, SEQ, D_MODEL), 1.0),
        "mem": nrm(ks[1], (BATCH, MEM_LEN, D_MODEL), 1.0),
        "w_in": nrm(ks[2], (L, D_MODEL, IN_WIDTH), D_MODEL ** -0.5),
        "p_moba": nrm(ks[3], (L, MOBA_WIDTH, D_MODEL), MOBA_WIDTH ** -0.5),
        "p_ret": nrm(ks[4], (L, RET_V_WIDTH, D_MODEL), RET_V_WIDTH ** -0.5),
        "p_mem": nrm(ks[5], (L, MEM_WIDTH, D_MODEL), MEM_WIDTH ** -0.5),
        "w_mem_kv": nrm(ks[6], (L, D_MODEL, 2 * MEM_WIDTH), D_MODEL ** -0.5),
        "w_o": nrm(ks[7], (L, D_MODEL, D_MODEL), D_MODEL ** -0.5 * DEEPNORM_BETA),
        "ln1_g": 1.0 + nrm(ks[8], (L, D_MODEL), 0.02),
        "ln1_b": nrm(ks[9], (L, D_MODEL), 0.02),
        "w_group": nrm(ks[10], (L, D_MODEL, N_GROUPS), D_MODEL ** -0.5),
        "b_group": nrm(ks[11], (L, N_GROUPS), 0.01),
        "w_expert": nrm(ks[12], (L, D_MODEL, N_EXPERTS), D_MODEL ** -0.5),
        "b_expert": nrm(ks[13], (L, N_EXPERTS), 0.01),
        "w_gate_up": nrm(ks[14], (L, N_EXPERTS, D_MODEL, 2 * EXPERT_FF), D_MODEL ** -0.5),
        "w_down": nrm(ks[15], (L, N_EXPERTS, EXPERT_FF, D_MODEL), EXPERT_FF ** -0.5 * DEEPNORM_BETA),
        "ln2_g": 1.0 + nrm(ks[16], (L, D_MODEL), 0.02),
        "ln2_b": nrm(ks[17], (L, D_MODEL), 0.02),
    }


def reference(x, mem, w_in, p_moba, p_ret, p_mem, w_mem_kv, w_o, ln1_g, ln1_b,
              w_group, b_group, w_expert, b_expert, w_gate_up, w_down, ln2_g, ln2_b):
    for l in range(DEPTH):
        mix = hybrid_mixer(x, mem, w_in[l], p_moba[l], p_ret[l], p_mem[l], w_mem_kv[l], w_o[l])
        x = layer_norm(DEEPNORM_ALPHA * x + mix, ln1_g[l], ln1_b[l])
        ffn = hier_moe(x, w_group[l], b_group[l], w_expert[l], b_expert[l], w_gate_up[l], w_down[l])
        x = layer_norm(DEEPNORM_ALPHA * x + ffn, ln2_g[l], ln2_b[l])
    return x
```

```python
import math
import numpy as np
import ml_dtypes
import concourse.bass as bass
import concourse.mybir as mybir
from concourse.bass_utils import run_bass_kernel_spmd
from contextlib import ExitStack

F32 = mybir.dt.float32
BF16 = mybir.dt.bfloat16
I32 = mybir.dt.int32
AF = mybir.ActivationFunctionType
ALU = mybir.AluOpType
AX = mybir.AxisListType

T = 2048
D = 2048
DEPTH = 2
ALPHA = float((2 * DEPTH) ** 0.25)
LN_EPS = 1e-5
GN_EPS = 1e-6
NEG = -30000.0
N_EXP = 32
BLK_ROWS = 256
N_BLK = 48
OOB = float(2 ** 20)


class Buf:
    def __init__(self, ap, name=""):
        self.t = ap
        self.name = name
        self.w = []
        self.pr = []
        self.r = []
        self.bf = None

    def __getitem__(self, idx):
        return self.t[idx]


class _Rec:
    def __init__(self):
        self.call = None

    def __getattr__(self, name):
        def f(*a, **k):
            self.call = (name, a, k)
            return self
        return f


class RegConst:
    def __init__(self, v):
        self.v = v


_REG_CACHE = {}


def _resolve(e, k):
    out = {}
    for key, val in k.items():
        if isinstance(val, RegConst):
            ck = (id(e), val.v)
            if ck not in _REG_CACHE:
                _REG_CACHE[ck] = e.to_reg(val.v)
            val = _REG_CACHE[ck]
        out[key] = val
    return out


def _record(fn):
    r = _Rec()
    fn(r)
    return r.call


class EngState:
    def __init__(self, name):
        self.name = name
        self.cnt = 0
        self.ops = []
        self.seen = {}


class Sched:
    N_DMA_SEMS = 16

    def __init__(self, nc, stack):
        self.nc = nc
        self.sems = {}
        self.engs = {}
        for name in ["pe", "dve", "act", "pool", "sp"]:
            self.sems[name] = stack.enter_context(nc.semaphore("s_" + name))
            self.engs[name] = EngState(name)
        self.dma_sems = {}
        self.dma_next = {}
        self.dma_uses = {}
        for q in ["sp", "pool"]:
            lst = []
            for i in range(self.N_DMA_SEMS):
                key = "d_%s_%d" % (q, i)
                self.sems[key] = stack.enter_context(nc.semaphore(key))
                lst.append(key)
                self.dma_uses[key] = 0
            self.dma_sems[q] = lst
            self.dma_next[q] = 0

    def _wait(self, eng, deps):
        best = {}
        for d in deps:
            if d is None:
                continue
            k, v = d
            if best.get(k, 0) < v:
                best[k] = v
        for k, v in best.items():
            if k == eng.name and eng.name == "pe":
                continue
            if eng.seen.get(k, 0) >= v:
                continue
            eng.seen[k] = v
            sem = self.sems[k]
            eng.ops.append(lambda e, sem=sem, v=v: e.wait_ge(sem, v))

    @staticmethod
    def _deps(reads, writes, is_dma=False):
        deps = []
        for b in reads:
            deps.extend(b.w)
        for b in writes:
            deps.extend(b.r)
            if is_dma:
                deps.extend(t for t in b.w if not t[0].startswith("d_"))
                deps.extend(b.pr)
            else:
                deps.extend(b.w)
        return deps

    @staticmethod
    def _commit(tok, reads, writes, is_dma=False):
        for b in writes:
            if is_dma and not b.r and all(t[0].startswith("d_") for t in b.w):
                b.w = b.w + [tok]
            else:
                b.pr = list(b.r)
                b.w = [tok]
            b.r = []
        for b in reads:
            if b not in writes:
                b.r.append(tok)

    def op(self, engname, fn, reads=(), writes=()):
        eng = self.engs[engname]
        self._wait(eng, self._deps(reads, writes))
        eng.cnt += 1
        tok = (eng.name, eng.cnt)
        sem = self.sems[eng.name]
        name, a, k = _record(fn)
        eng.ops.append(lambda e, name=name, a=a, k=k, sem=sem: getattr(e, name)(*a, **k).then_inc(sem, 1))
        self._commit(tok, reads, writes)
        return tok

    def dma(self, q, fn, reads=(), writes=()):
        eng = self.engs[q]
        i = self.dma_next[q]
        self.dma_next[q] = (i + 1) % self.N_DMA_SEMS
        key = self.dma_sems[q][i]
        deps = self._deps(reads, writes, True)
        if self.dma_uses[key] > 0:
            deps.append((key, 16 * self.dma_uses[key]))
        self._wait(eng, deps)
        self.dma_uses[key] += 1
        tok = (key, 16 * self.dma_uses[key])
        sem = self.sems[key]
        name, a, k = _record(fn)
        eng.ops.append(lambda e, name=name, a=a, k=k, sem=sem: getattr(e, name)(*a, **_resolve(e, k)).then_inc(sem, 16))
        self._commit(tok, reads, writes, True)
        return tok

    def raw_dma(self, q, fn, reads=(), writes=()):
        eng = self.engs[q]
        i = self.dma_next[q]
        self.dma_next[q] = (i + 1) % self.N_DMA_SEMS
        key = self.dma_sems[q][i]
        deps = self._deps(reads, writes)
        if self.dma_uses[key] > 0:
            deps.append((key, 16 * self.dma_uses[key]))
        self._wait(eng, deps)
        self.dma_uses[key] += 1
        tok = (key, 16 * self.dma_uses[key])
        sem = self.sems[key]
        eng.ops.append(lambda e, fn=fn, sem=sem: fn(e).then_inc(sem, 16))
        self._commit(tok, reads, writes)
        return tok

    def _all_tokens(self):
        deps = []
        for key, n in self.dma_uses.items():
            if n > 0:
                deps.append((key, 16 * n))
        for name in ["pe", "dve", "act", "pool"]:
            if self.engs[name].cnt > 0:
                deps.append((name, self.engs[name].cnt))
        return deps

    def barrier(self):
        deps = self._all_tokens()
        for name in ["pe", "dve", "act", "pool", "sp"]:
            self._wait(self.engs[name], deps)

    def finish(self):
        self._wait(self.engs["sp"], self._all_tokens())

    def emit(self):
        nc = self.nc
        engs = self.engs
        with nc.Block() as block:
            @block.tensor
            def _(e):
                for f in engs["pe"].ops:
                    f(e)

            @block.vector
            def _(e):
                for f in engs["dve"].ops:
                    f(e)

            @block.scalar
            def _(e):
                for f in engs["act"].ops:
                    f(e)

            @block.gpsimd
            def _(e):
                for f in engs["pool"].ops:
                    f(e)

            @block.sync
            def _(e):
                for f in engs["sp"].ops:
                    f(e)


class Arena:
    def __init__(self, t, nbytes):
        self.t = t
        self.nbytes = nbytes
        self.off = 0

    def reset(self):
        self.off = 0

    def alloc(self, free_shape, dtype, parts=128):
        n = 1
        for s in free_shape:
            n *= s
        esz = 4 if dtype in (F32, I32) else 2
        nb = (n * esz + 63) // 64 * 64
        assert self.off + nb <= self.nbytes, ("arena overflow", self.off, nb)
        v = self.t[0:parts, self.off // 2:(self.off + n * esz) // 2]
        self.off += nb
        if dtype in (F32, I32):
            v = v.bitcast(dtype)
        if len(free_shape) == 2:
            v = v.rearrange("p (a b) -> p a b", a=free_shape[0])
        elif len(free_shape) == 3:
            v = v.rearrange("p (a b c) -> p a b c", a=free_shape[0], b=free_shape[1])
        return Buf(v)


def _consts():
    c = {}
    bf = ml_dtypes.bfloat16
    c["c_ident"] = np.eye(128, dtype=np.float32).astype(bf)
    c["c_identf"] = np.eye(128, dtype=np.float32)
    c["c_ones"] = np.ones((128, 128), np.float32).astype(bf)
    half = 128
    inv = (10000.0 ** (-np.linspace(0.0, 1.0, half, dtype=np.float32))).astype(np.float32)
    pos = np.arange(T, dtype=np.float32)
    ang = (pos[None, :] * inv[:, None]).astype(np.float32)
    cos = np.cos(ang).astype(np.float32)
    sin = np.sin(ang).astype(np.float32)
    c["c_rot"] = np.stack([cos, sin, cos / 16.0, sin / 16.0], 0).astype(np.float32)
    nh = 4
    log_g = np.log1p(-np.power(2.0, -5.0 - np.arange(nh, dtype=np.float64)))
    idx = np.arange(128, dtype=np.float64)
    diff = idx[None, :] - idx[:, None]
    decT = np.where(diff >= 0, np.exp(log_g[:, None, None] * np.maximum(diff, 0.0)[None]), 0.0)
    c["c_decT"] = np.ascontiguousarray(decT.transpose(1, 0, 2)).astype(np.float32)
    xi = np.exp(log_g[:, None] * (idx + 1.0)[None, :])
    xi_t = np.tile(xi, (1, T // 128))
    c["c_xi"] = np.ascontiguousarray(np.broadcast_to(xi_t[:, None, :], (nh, 128, T))).astype(np.float32)
    zeta = np.exp(log_g[:, None] * (127.0 - idx)[None, :])
    c["c_zeta"] = np.ascontiguousarray(zeta.T).astype(np.float32)
    cd = np.exp(log_g * 128.0)
    qt = np.arange(16)[:, None]
    n = np.arange(8)[None, :]
    past = (n < (qt // 2)).astype(np.float32).reshape(1, 128)
    own = (n == (qt // 2)).astype(np.float32).reshape(1, 128)
    c["c_moba"] = np.ascontiguousarray(np.stack([
        np.broadcast_to((past - 1.0) * 1e30, (128, 128)),
        np.broadcast_to(past, (128, 128)),
        np.broadcast_to(own, (128, 128))], 0)).astype(np.float32)
    e8 = np.zeros((128, 1024), np.float32)
    for i in range(8):
        e8[i, i * 128:(i + 1) * 128] = 1.0
    c["c_e8"] = e8.astype(bf)
    k = np.arange(128)[:, None, None]
    j = np.arange(4)[None, :, None]
    q = np.arange(512)[None, None, :]
    c["c_cm"] = np.where((128 * j + k) > q, NEG, 0.0).astype(np.float32).astype(bf)
    pp = np.arange(128)
    c["c_ustrict"] = (pp[:, None] < pp[None, :]).astype(np.float32).astype(bf)
    c["c_thr"] = np.ascontiguousarray(np.broadcast_to((float(BLK_ROWS) * np.arange(N_BLK, dtype=np.float32))[None, :], (128, N_BLK)))
    c["c_base"] = np.concatenate([(pp[:, None] * 8 + np.arange(8)[None, :]), (pp[:, None] * 4 + np.arange(4)[None, :])], 1).astype(np.float32)
    return c, [float(v) for v in cd]


def build(n_layers=DEPTH, debug=False, no_indirect=False, stop_after=None):
    _REG_CACHE.clear()
    nc = bass.Bass("TRN2", target_bir_lowering=False)
    consts, CD = _consts()

    def din(name, shape, dt=F32):
        return nc.dram_tensor(name, list(shape), dt, kind="ExternalInput").ap()

    def dscr(name, shape, dt):
        return nc.dram_tensor(name, list(shape), dt, kind=("ExternalOutput" if debug else "Internal")).ap()

    x_in = din("x", [T, D])
    xT_in = din("xT", [D, T])
    memT_in = din("memT", [D, 256])
    w_in = din("w_in", [DEPTH, D, 16384])
    p_moba = din("p_moba", [DEPTH, 1024, D])
    p_ret = din("p_ret", [DEPTH, 2048, D])
    p_mem = din("p_mem", [DEPTH, 1024, D])
    w_mem_kv = din("w_mem_kv", [DEPTH, D, 2048])
    w_o = din("w_o", [DEPTH, D, D])
    ln_par = din("ln_par", [DEPTH, 4, 128, D])
    w_r = din("w_r", [DEPTH, D, 36])
    b_r = din("b_r", [DEPTH, 128, 36])
    w_gup = din("w_gup", [DEPTH * N_EXP * 128 * 8, 2048])
    w_dnp = din("w_dnp", [DEPTH * N_EXP * 128 * 4, 2048])
    cin = {}
    for k, v in consts.items():
        cin[k] = din(k, v.shape, BF16 if v.dtype == ml_dtypes.bfloat16 else F32)
    out = nc.dram_tensor("out", [T, D], F32, kind="ExternalOutput").ap()

    QaT = dscr("QaT", [8, 128, T], BF16)
    KaT = dscr("KaT", [8, 128, T], BF16)
    Va = dscr("Va", [T, 1024], BF16)
    RqT = dscr("RqT", [8, 128, T], BF16)
    RkT = dscr("RkT", [8, 128, T], BF16)
    Rv = dscr("Rv", [T, 2048], BF16)
    RgT = dscr("RgT", [16, 128, T], BF16)
    MqT = dscr("MqT", [8, 128, T], BF16)
    GT = dscr("GT", [48, 128, T], BF16)
    YT = dscr("YT", [32, 128, T], BF16)
    MT = dscr("MT", [16, 128, T], BF16)
    X1 = dscr("X1", [T, D], F32)
    X1T = dscr("X1T", [16, 128, T], BF16)
    X2 = dscr("X2", [T, D], F32)
    X2T = dscr("X2T", [16, 128, T], BF16)
    X1B = dscr("X1B", [T, D], BF16)
    Xs = dscr("Xs", [N_BLK * BLK_ROWS, D], BF16)
    Ys = dscr("Ys", [N_BLK * BLK_ROWS, D], F32)

    if debug:
        DBG_dest = nc.dram_tensor("DBG_dest", [128, 32], I32, kind="ExternalOutput").ap()
        DBG_blk = nc.dram_tensor("DBG_blk", [128, N_BLK], F32, kind="ExternalOutput").ap()
        DBG_wab = nc.dram_tensor("DBG_wab", [128, 32], F32, kind="ExternalOutput").ap()
    with ExitStack() as st:
        S = Sched(nc, st)
        ARENA_BYTES = 198 * 1024
        arena_t = st.enter_context(nc.sbuf_tensor("arena", [128, ARENA_BYTES // 2], BF16))
        ar = Arena(arena_t, ARENA_BYTES)
        ident = Buf(st.enter_context(nc.sbuf_tensor("ident", [128, 128], BF16)))
        identf = Buf(st.enter_context(nc.sbuf_tensor("identf", [128, 128], F32)))
        ones = Buf(st.enter_context(nc.sbuf_tensor("ones", [128, 128], BF16)))
        def pers(name, shape, dt):
            return Buf(st.enter_context(nc.sbuf_tensor(name, shape, dt)))
        ustrict = pers("ustrict", [128, 128], BF16)
        selS_all = pers("selS_all", [128, 16, 32], BF16)
        selA_all = pers("selA_all", [128, 16, 32], BF16)
        wab_all = pers("wab_all", [128, 16, 2], F32)
        dest_i = pers("dest_i", [128, 16, 2], I32)
        blkE = pers("blkE", [128, N_BLK], F32)
        blkU = pers("blkU", [128, N_BLK], F32)
        ps = []
        for i in range(8):
            b = Buf(st.enter_context(nc.psum_tensor("ps%d" % i, [128, 512], F32)))
            b.bf = b.t[:, :].bitcast(BF16)
            ps.append(b)
        psn = [0]

        psm = [0]

        def nextps_m():
            p = ps[3 + (psm[0] % 3)]
            psm[0] += 1
            return p

        def nextps():
            p = ps[psn[0] % 8]
            psn[0] += 1
            return p

        S.dma("sp", lambda e: e.dma_start(out=ident[:, :], in_=cin["c_ident"][:, :]), writes=[ident])
        S.dma("sp", lambda e: e.dma_start(out=identf[:, :], in_=cin["c_identf"][:, :]), writes=[identf])
        S.dma("sp", lambda e: e.dma_start(out=ones[:, :], in_=cin["c_ones"][:, :]), writes=[ones])
        S.dma("sp", lambda e: e.dma_start(out=ustrict[:, :], in_=cin["c_ustrict"][:, :]), writes=[ustrict])

        def sl(i, n):
            return slice(i * n, (i + 1) * n)

        def mm(pt, o, l, r, start, stop, reads):
            S.op("pe", lambda e: e.matmul(o, l, r, start=start, stop=stop), reads=reads, writes=[pt])

        def load_w512(wbuf, wsrc2d, col0, nk=16):
            src = wsrc2d[:, col0:col0 + 512].rearrange("(kc p) f -> p kc f", p=128)
            S.dma("pool", lambda e: e.dma_start(out=wbuf[:, 0:nk, :], in_=src), writes=[wbuf])

        def load_actT(actT, L):
            if L == 0:
                src = xT_in.rearrange("(kc p) t -> p kc t", p=128)
                for kc in range(16):
                    S.dma("pool", lambda e, kc=kc: e.dma_start(out=actT[:, kc, :], in_=src[:, kc, :]), writes=[actT])
            else:
                S.dma("sp", lambda e: e.dma_start(out=actT[:, :, :], in_=X2T.rearrange("c p t -> p c t")), writes=[actT])

        def phase_A(L):
            S.barrier()
            ar.reset()
            actT = ar.alloc([16, T], BF16)
            load_actT(actT, L)
            rot = ar.alloc([4, T], F32)
            S.dma("sp", lambda e: e.dma_start(out=rot[:, :, :], in_=cin["c_rot"].rearrange("c p t -> p c t")), writes=[rot])
            wb = [ar.alloc([16, 512], BF16) for _ in range(2)]
            ot = [ar.alloc([T], BF16) for _ in range(4)]
            tm = [ar.alloc([512], BF16) for _ in range(3)]
            tmp = [ar.alloc([512], F32) for _ in range(4)]
            W = w_in[L]
            cnt = {"wb": 0, "ot": 0, "tm": 0}

            def proj_chunk(wbuf, c, tg):
                p = nextps()
                for kc in range(16):
                    mm(p, p[:, :], wbuf[:, kc, sl(c, 128)], actT[:, kc, sl(tg, 512)], kc == 0, kc == 15, [wbuf, actT])
                return p

            def tform(col0, ncols, kind, dest, dchunk0):
                for g in range(ncols // 512):
                    wbuf = wb[cnt["wb"] % 2]
                    cnt["wb"] += 1
                    load_w512(wbuf, W, col0 + g * 512)
                    if kind == "rot" or kind == "rotk":
                        ci, si = (0, 1) if kind == "rot" else (2, 3)
                        for pr in range(2):
                            o1 = ot[cnt["ot"] % 4]
                            o2 = ot[(cnt["ot"] + 1) % 4]
                            cnt["ot"] += 2
                            for tg in range(4):
                                p1 = proj_chunk(wbuf, 2 * pr, tg)
                                p2 = proj_chunk(wbuf, 2 * pr + 1, tg)
                                ts = sl(tg, 512)
                                S.op("dve", lambda e, p1=p1, ts=ts: e.tensor_tensor(tmp[0][:, :], p1[:, :], rot[:, ci, ts], ALU.mult), reads=[p1, rot], writes=[tmp[0]])
                                S.op("dve", lambda e, p2=p2, ts=ts: e.tensor_tensor(tmp[1][:, :], p2[:, :], rot[:, si, ts], ALU.mult), reads=[p2, rot], writes=[tmp[1]])
                                S.op("dve", lambda e, p1=p1, ts=ts: e.tensor_tensor(tmp[2][:, :], p1[:, :], rot[:, si, ts], ALU.mult), reads=[p1, rot], writes=[tmp[2]])
                                S.op("dve", lambda e, p2=p2, ts=ts: e.tensor_tensor(tmp[3][:, :], p2[:, :], rot[:, ci, ts], ALU.mult), reads=[p2, rot], writes=[tmp[3]])
                                S.op("pool", lambda e, o1=o1, ts=ts: e.tensor_tensor(o1[:, ts], tmp[0][:, :], tmp[1][:, :], ALU.subtract), reads=[tmp[0], tmp[1]], writes=[o1])
                                S.op("pool", lambda e, o2=o2, ts=ts: e.tensor_tensor(o2[:, ts], tmp[2][:, :], tmp[3][:, :], ALU.add), reads=[tmp[2], tmp[3]], writes=[o2])
                            ch = dchunk0 + g * 4 + pr * 2
                            S.dma("sp", lambda e, o1=o1, ch=ch: e.dma_start(out=dest[ch, :, :], in_=o1[:, :]), reads=[o1])
                            S.dma("sp", lambda e, o2=o2, ch=ch: e.dma_start(out=dest[ch + 1, :, :], in_=o2[:, :]), reads=[o2])
                    else:
                        for c in range(4):
                            o = ot[cnt["ot"] % 4]
                            cnt["ot"] += 1
                            for tg in range(4):
                                p = proj_chunk(wbuf, c, tg)
                                ts = sl(tg, 512)
                                if kind == "copy":
                                    if tg % 2 == 0:
                                        S.op("act", lambda e, p=p, o=o, ts=ts: e.copy(o[:, ts], p[:, :]), reads=[p], writes=[o])
                                    else:
                                        S.op("dve", lambda e, p=p, o=o, ts=ts: e.tensor_copy(o[:, ts], p[:, :]), reads=[p], writes=[o])
                                else:
                                    fn = AF.Silu if kind == "silu" else AF.Sigmoid
                                    S.op("act", lambda e, p=p, o=o, ts=ts, fn=fn: e.activation(o[:, ts], p[:, :], fn), reads=[p], writes=[o])
                            ch = dchunk0 + g * 4 + c
                            S.dma("sp", lambda e, o=o, ch=ch: e.dma_start(out=dest[ch, :, :], in_=o[:, :]), reads=[o])

            def tokmaj(col0, ncols, dest):
                for g in range(ncols // 512):
                    wbuf = wb[cnt["wb"] % 2]
                    cnt["wb"] += 1
                    load_w512(wbuf, W, col0 + g * 512)
                    for tt in range(16):
                        p = nextps()
                        for kc in range(16):
                            mm(p, p[:, :], actT[:, kc, sl(tt, 128)], wbuf[:, kc, :], kc == 0, kc == 15, [wbuf, actT])
                        o = tm[cnt["tm"] % 3]
                        cnt["tm"] += 1
                        if tt % 2 == 0:
                            S.op("act", lambda e, p=p, o=o: e.copy(o[:, :], p[:, :]), reads=[p], writes=[o])
                        else:
                            S.op("dve", lambda e, p=p, o=o: e.tensor_copy(o[:, :], p[:, :]), reads=[p], writes=[o])
                        S.dma("sp", lambda e, o=o, tt=tt, g=g: e.dma_start(out=dest[sl(tt, 128), sl(g, 512)], in_=o[:, :]), reads=[o])

            tform(0, 1024, "copy", QaT, 0)
            tform(1024, 1024, "copy", KaT, 0)
            tokmaj(2048, 1024, Va)
            tform(3072, 1024, "rot", RqT, 0)
            tform(4096, 1024, "rotk", RkT, 0)
            tokmaj(5120, 2048, Rv)
            tform(9216, 1024, "copy", MqT, 0)
            tform(7168, 2048, "silu", RgT, 0)
            tform(10240, 6144, "sigmoid", GT, 0)

        def phase_B(L):
            S.barrier()
            ar.reset()
            NB3 = 3
            QT = [ar.alloc([T], BF16) for _ in range(NB3)]
            KT = [ar.alloc([T], BF16) for _ in range(NB3)]
            V = [ar.alloc([16, 128], BF16) for _ in range(NB3)]
            mob = ar.alloc([3, 128], F32)
            e8 = ar.alloc([1024], BF16)
            cm = ar.alloc([4, 512], BF16)
            S.dma("sp", lambda e: e.dma_start(out=mob[:, :, :], in_=cin["c_moba"].rearrange("c p t -> p c t")), writes=[mob])
            S.dma("sp", lambda e: e.dma_start(out=e8[:, :], in_=cin["c_e8"][:, :]), writes=[e8])
            S.dma("sp", lambda e: e.dma_start(out=cm[:, :, :], in_=cin["c_cm"][:, :, :]), writes=[cm])
            km = [ar.alloc([8], F32) for _ in range(2)]
            kmb = [ar.alloc([8], BF16) for _ in range(2)]
            gm = ar.alloc([16, 8], F32)
            mx = ar.alloc([16, 8], F32)
            sel = ar.alloc([16, 8], F32)
            mbb = ar.alloc([128], BF16)
            MbT = [ar.alloc([T], BF16) for _ in range(2)]
            for m_ in MbT:
                S.op("pool", lambda e, m_=m_: e.memset(m_[:, :], 0.0), writes=[m_])
            PT = [ar.alloc([512], BF16) for _ in range(3)]
            rden = [ar.alloc([512], F32) for _ in range(2)]
            yo = [ar.alloc([T], BF16) for _ in range(2)]
            Vsrc = Va.rearrange("(tt p) f -> p tt f", p=128)

            def load(h):
                b = h % NB3
                S.dma("sp", lambda e: e.dma_start(out=QT[b][:, :], in_=QaT[h, :, :]), writes=[QT[b]])
                S.dma("sp", lambda e: e.dma_start(out=KT[b][:, :], in_=KaT[h, :, :]), writes=[KT[b]])
                S.dma("sp", lambda e: e.dma_start(out=V[b][:, :, :], in_=Vsrc[:, :, sl(h, 128)]), writes=[V[b]])

            def setup_a(h):
                k = KT[h % NB3]
                S.op("dve", lambda e: e.tensor_reduce(km[h % 2][:, :], k[:, :].rearrange("p (n j) -> p n j", n=8), AX.X, ALU.add), reads=[k], writes=[km[h % 2]])
                S.op("act", lambda e: e.mul(kmb[h % 2][:, :], km[h % 2][:, :], 1.0 / 256.0), reads=[km[h % 2]], writes=[kmb[h % 2]])

            def setup_b(h):
                q = QT[h % NB3]
                gp = ps[6]
                for qt in range(16):
                    mm(gp, gp[:, sl(qt, 8)], q[:, sl(qt, 128)], kmb[h % 2][:, :], True, True, [q, kmb[h % 2]])
                S.op("dve", lambda e: e.tensor_tensor(gm[:, :, :], gp[:, 0:128].rearrange("p (a b) -> p a b", a=16), mob[:, 0, :].rearrange("p (a b) -> p a b", a=16), ALU.add), reads=[gp, mob], writes=[gm])
                for qt in range(16):
                    S.op("dve", lambda e, qt=qt: e.max(mx[:, qt, :], gm[:, qt, :]), reads=[gm], writes=[mx])
                for qt in range(16):
                    S.op("dve", lambda e, qt=qt: e.tensor_scalar(sel[:, qt, :], gm[:, qt, :], mx[:, qt, 2:3], None, ALU.is_ge), reads=[gm, mx], writes=[sel])
                selv = sel[:, :, :].rearrange("p a b -> p (a b)")
                S.op("dve", lambda e: e.tensor_tensor(selv, selv, mob[:, 1, :], ALU.mult), reads=[sel, mob], writes=[sel])
                S.op("dve", lambda e: e.tensor_tensor(selv, selv, mob[:, 2, :], ALU.add), reads=[sel, mob], writes=[sel])
                S.op("dve", lambda e: e.tensor_scalar(mbb[:, :], selv, -1.0, -NEG, ALU.add, ALU.mult), reads=[sel], writes=[mbb])

            def setup_c(h):
                M = MbT[h % 2]
                for half in range(2):
                    tp = ps[6 + half]
                    for j in range(8):
                        qt = half * 8 + j
                        S.op("pe", lambda e, tp=tp, j=j, qt=qt: e.transpose(tp.bf[0:8, sl(j, 128)], mbb[:, sl(qt, 8)], ident[:, :]), reads=[mbb, ident], writes=[tp])
                    S.op("act", lambda e, tp=tp, half=half: e.copy(M[0:8, sl(half, 1024)], tp.bf[0:8, 0:1024]), reads=[tp], writes=[M])

            cnt = {"it": 0, "npt": 0}

            def main(h, mid):
                q, k, v, M, y = QT[h % NB3], KT[h % NB3], V[h % NB3], MbT[h % 2], yo[h % 2]
                items = [(g, kt) for g in range(4) for kt in range(4 * g + 4)]
                banks = {}
                for g in range(4):
                    banks[g] = (ps[2 + cnt["it"] % 2], ps[4 + cnt["it"] % 2])
                    cnt["it"] += 1
                sp_pt = {}

                def emit_S(i):
                    g, kt = items[i]
                    Sp = ps[cnt["npt"] % 2]
                    pt = PT[cnt["npt"] % 3]
                    cnt["npt"] += 1
                    sp_pt[i] = pt
                    diag = kt >= 4 * g
                    mm(Sp, Sp[:, :], k[:, sl(kt, 128)], q[:, sl(g, 512)], True, False, [k, q])
                    mm(Sp, Sp[:, :], e8[:, sl(kt // 2, 128)], M[:, sl(g, 512)], False, not diag, [e8, M])
                    if diag:
                        mm(Sp, Sp[:, :], ident[:, :], cm[:, kt - 4 * g, :], False, True, [ident, cm])
                    S.op("act", lambda e: e.activation(pt[:, :], Sp[:, :], AF.Exp, scale=128.0 ** -0.5), reads=[Sp], writes=[pt])

                def emit_PV(i):
                    g, kt = items[i]
                    Op, Dp = banks[g]
                    pt = sp_pt[i]
                    nkt = 4 * g + 4
                    mm(Op, Op[:, :], v[:, kt, :], pt[:, :], kt == 0, kt == nkt - 1, [v, pt])
                    mm(Dp, Dp[:, :], ones[:, :], pt[:, :], kt == 0, kt == nkt - 1, [ones, pt])
                    if kt == nkt - 1:
                        rd = rden[g % 2]
                        S.op("dve", lambda e: e.reciprocal(rd[:, :], Dp[:, :]), reads=[Dp], writes=[rd])
                        S.op("dve", lambda e: e.tensor_tensor(y[:, sl(g, 512)], Op[:, :], rd[:, :], ALU.mult), reads=[Op, rd], writes=[y])
                        if g == 0 and mid is not None:
                            mid()

                emit_S(0)
                for i in range(len(items)):
                    if i + 1 < len(items):
                        emit_S(i + 1)
                    emit_PV(i)
                S.dma("sp", lambda e: e.dma_start(out=YT[h, :, :], in_=y[:, :]), reads=[y])

            load(0)
            load(1)
            setup_a(0)
            setup_b(0)
            setup_c(0)
            setup_a(1)
            for h in range(8):
                if h + 2 < 8:
                    load(h + 2)
                nxt = (lambda h=h: setup_b(h + 1)) if h + 1 < 8 else None
                main(h, nxt)
                if h + 1 < 8:
                    setup_c(h + 1)
                if h + 2 < 8:
                    setup_a(h + 2)

        def phase_C(L):
            S.barrier()
            ar.reset()
            Rq = [ar.alloc([2, T], BF16) for _ in range(2)]
            Rk = [ar.alloc([2, T], BF16) for _ in range(2)]
            Rvt = [ar.alloc([16, 512], BF16) for _ in range(2)]
            Rg = [ar.alloc([4, T], BF16) for _ in range(2)]
            xi = [ar.alloc([T], F32) for _ in range(2)]
            decT = ar.alloc([4, 128], F32)
            zeta = ar.alloc([4], F32)
            S.dma("sp", lambda e: e.dma_start(out=decT[:, :, :], in_=cin["c_decT"][:, :, :]), writes=[decT])
            S.dma("sp", lambda e: e.dma_start(out=zeta[:, :], in_=cin["c_zeta"][:, :]), writes=[zeta])
            Qxi = ar.alloc([2, T], BF16)
            Kz = ar.alloc([16, 256], BF16)
            state = ar.alloc([2, 512], F32)
            stateb = ar.alloc([2, 512], BF16)
            onb = [ar.alloc([512], BF16) for _ in range(2)]
            STb = [ar.alloc([128], BF16) for _ in range(2)]
            st6 = ar.alloc([6], F32)
            mv = ar.alloc([2], F32)
            lnv = ar.alloc([1], F32)
            rstd = ar.alloc([1], F32)
            nmr = ar.alloc([1], F32)
            yr = [ar.alloc([4, T], BF16) for _ in range(2)]
            Rvsrc = Rv.rearrange("(tt p) f -> p tt f", p=128)

            def load(h):
                b = h % 2
                S.dma("sp", lambda e: e.dma_start(out=Rq[b][:, :, :], in_=RqT[2 * h:2 * h + 2].rearrange("c p t -> p c t")), writes=[Rq[b]])
                S.dma("sp", lambda e: e.dma_start(out=Rk[b][:, :, :], in_=RkT[2 * h:2 * h + 2].rearrange("c p t -> p c t")), writes=[Rk[b]])
                S.dma("sp", lambda e: e.dma_start(out=Rvt[b][:, :, :], in_=Rvsrc[:, :, sl(h, 512)]), writes=[Rvt[b]])
                S.dma("sp", lambda e: e.dma_start(out=Rg[b][:, :, :], in_=RgT[4 * h:4 * h + 4].rearrange("c p t -> p c t")), writes=[Rg[b]])
                S.dma("sp", lambda e: e.dma_start(out=xi[b][:, :], in_=cin["c_xi"][h, :, :]), writes=[xi[b]])

            load(0)
            for h in range(4):
                if h + 1 < 4:
                    load(h + 1)
                b = h % 2
                rq, rk, rv, rg, x_i, y = Rq[b], Rk[b], Rvt[b], Rg[b], xi[b], yr[b]
                for dc in range(2):
                    S.op("dve", lambda e, dc=dc: e.tensor_tensor(Qxi[:, dc, :], rq[:, dc, :], x_i[:, :], ALU.mult), reads=[rq, x_i], writes=[Qxi])
                for n in range(16):
                    tp = ps[5 + n % 2]
                    for dc in range(2):
                        S.op("pe", lambda e, tp=tp, dc=dc, n=n: e.transpose(tp.bf[:, sl(dc, 128)], rk[:, dc, sl(n, 128)], ident[:, :]), reads=[rk, ident], writes=[tp])
                    S.op("act", lambda e, tp=tp, n=n: e.activation(Kz[:, n, :], tp.bf[:, 0:256], AF.Identity, scale=zeta[:, h:h + 1]), reads=[tp, zeta], writes=[Kz])
                def emit_ST(n):
                    ns = sl(n, 128)
                    STp = ps[0]
                    for dc in range(2):
                        mm(STp, STp[:, 0:128], rk[:, dc, ns], rq[:, dc, ns], dc == 0, dc == 1, [rk, rq])
                    sb = STb[n % 2]
                    S.op("dve", lambda e: e.tensor_tensor(sb[:, :], STp[:, 0:128], decT[:, h, :], ALU.mult), reads=[STp, decT], writes=[sb])

                def emit_O(n):
                    ns = sl(n, 128)
                    sb = STb[n % 2]
                    Op = ps[1 + n % 2]
                    mm(Op, Op[:, :], sb[:, :], rv[:, n, :], True, n == 0, [sb, rv])
                    if n > 0:
                        for dc in range(2):
                            mm(Op, Op[:, :], Qxi[:, dc, ns], stateb[:, dc, :], False, dc == 1, [Qxi, stateb])
                    if n < 15:
                        for dc in range(2):
                            kv = ps[3 + dc]
                            mm(kv, kv[:, :], Kz[:, n, sl(dc, 128)], rv[:, n, :], True, True, [Kz, rv])
                            if n == 0:
                                S.op("dve", lambda e, kv=kv, dc=dc: e.tensor_copy(state[:, dc, :], kv[:, :]), reads=[kv], writes=[state])
                            else:
                                S.op("dve", lambda e, kv=kv, dc=dc: e.scalar_tensor_tensor(state[:, dc, :], state[:, dc, :], CD[h], kv[:, :], ALU.mult, ALU.add), reads=[kv, state], writes=[state])
                        S.op("act", lambda e: e.copy(stateb[:, :, :], state[:, :, :]), reads=[state], writes=[stateb])
                    S.op("dve", lambda e: e.bn_stats(st6[:, :], Op[:, :]), reads=[Op], writes=[st6])
                    S.op("dve", lambda e: e.bn_aggr(mv[:, :], st6[:, :]), reads=[st6], writes=[mv])
                    S.op("act", lambda e: e.activation(lnv[:, :], mv[:, 1:2], AF.Ln, bias=GN_EPS), reads=[mv], writes=[lnv])
                    S.op("act", lambda e: e.activation(rstd[:, :], lnv[:, :], AF.Exp, scale=-0.5), reads=[lnv], writes=[rstd])
                    S.op("dve", lambda e: e.scalar_tensor_tensor(nmr[:, :], mv[:, 0:1], -1.0, rstd[:, :], ALU.mult, ALU.mult), reads=[mv, rstd], writes=[nmr])
                    ob = onb[n % 2]
                    S.op("act", lambda e: e.activation(ob[:, :], Op[:, :], AF.Identity, bias=nmr[:, 0:1], scale=rstd[:, 0:1]), reads=[Op, nmr, rstd], writes=[ob])

                def emit_T(n):
                    ns = sl(n, 128)
                    ob = onb[n % 2]
                    tp = ps[7]
                    for ec in range(4):
                        S.op("pe", lambda e, ec=ec: e.transpose(tp.bf[:, sl(ec, 128)], ob[:, sl(ec, 128)], ident[:, :]), reads=[ob, ident], writes=[tp])
                    S.op("dve", lambda e: e.tensor_tensor(y[:, :, ns], tp.bf[:, 0:512].rearrange("p (a b) -> p a b", a=4), rg[:, :, ns], ALU.mult), reads=[tp, rg], writes=[y])

                emit_ST(0)
                for n in range(16):
                    if n + 1 < 16:
                        emit_ST(n + 1)
                    emit_O(n)
                    if n > 0:
                        emit_T(n - 1)
                emit_T(15)
                S.dma("sp", lambda e, y=y, h=h: e.dma_start(out=YT[8 + 4 * h:12 + 4 * h].rearrange("c p t -> p c t"), in_=y[:, :, :]), reads=[y])

        def phase_D(L):
            S.barrier()
            ar.reset()
            memT = ar.alloc([16, 256], BF16)
            S.dma("pool", lambda e: e.dma_start(out=memT[:, :, :], in_=memT_in.rearrange("(kc p) m -> p kc m", p=128)), writes=[memT])
            wb = [ar.alloc([16, 512], BF16) for _ in range(2)]
            kmT = ar.alloc([8, 256], BF16)
            vm = ar.alloc([2, 1024], BF16)
            Mq = [ar.alloc([2, T], BF16) for _ in range(2)]
            PT = [ar.alloc([512], BF16) for _ in range(4)]
            rden = [ar.alloc([512], F32) for _ in range(2)]
            ym = [ar.alloc([2, T], BF16) for _ in range(2)]
            W = w_mem_kv[L]
            for g in range(4):
                wbuf = wb[g % 2]
                load_w512(wbuf, W, g * 512)
                if g < 2:
                    for c in range(4):
                        p = nextps()
                        for kc in range(16):
                            mm(p, p[:, 0:256], wbuf[:, kc, sl(c, 128)], memT[:, kc, :], kc == 0, kc == 15, [wbuf, memT])
                        S.op("act", lambda e, p=p, g=g, c=c: e.copy(kmT[:, g * 4 + c, :], p[:, 0:256]), reads=[p], writes=[kmT])
                else:
                    for mt in range(2):
                        p = nextps()
                        for kc in range(16):
                            mm(p, p[:, :], memT[:, kc, sl(mt, 128)], wbuf[:, kc, :], kc == 0, kc == 15, [wbuf, memT])
                        S.op("dve", lambda e, p=p, g=g, mt=mt: e.tensor_copy(vm[:, mt, sl(g - 2, 512)], p[:, :]), reads=[p], writes=[vm])

            def load(h):
                S.dma("sp", lambda e: e.dma_start(out=Mq[h % 2][:, :, :], in_=MqT[2 * h:2 * h + 2].rearrange("c p t -> p c t")), writes=[Mq[h % 2]])

            load(0)
            npt = 0
            for h in range(4):
                if h + 1 < 4:
                    load(h + 1)
                mq, y = Mq[h % 2], ym[h % 2]
                for g in range(4):
                    pts = []
                    for mt in range(2):
                        Sp = ps[mt]
                        for dc in range(2):
                            mm(Sp, Sp[:, :], kmT[:, 2 * h + dc, sl(mt, 128)], mq[:, dc, sl(g, 512)], dc == 0, dc == 1, [kmT, mq])
                        pt = PT[npt % 4]
                        npt += 1
                        S.op("act", lambda e, Sp=Sp, pt=pt: e.activation(pt[:, :], Sp[:, :], AF.Exp, scale=1.0 / 16.0), reads=[Sp], writes=[pt])
                        pts.append(pt)
                    Dp = ps[4 + g % 2]
                    for mt in range(2):
                        mm(Dp, Dp[:, :], ones[:, :], pts[mt][:, :], mt == 0, mt == 1, [ones, pts[mt]])
                    rd = rden[g % 2]
                    S.op("dve", lambda e, rd=rd, Dp=Dp: e.reciprocal(rd[:, :], Dp[:, :]), reads=[Dp], writes=[rd])
                    for dc in range(2):
                        Op = ps[2 + dc]
                        for mt in range(2):
                            mm(Op, Op[:, :], vm[:, mt, sl(2 * h + dc, 128)], pts[mt][:, :], mt == 0, mt == 1, [vm, pts[mt]])
                        S.op("dve", lambda e, rd=rd, Op=Op, y=y, g=g, dc=dc: e.tensor_tensor(y[:, dc, sl(g, 512)], Op[:, :], rd[:, :], ALU.mult), reads=[Op, rd], writes=[y])
                S.dma("sp", lambda e, y=y, h=h: e.dma_start(out=YT[24 + 2 * h:26 + 2 * h].rearrange("c p t -> p c t"), in_=y[:, :, :]), reads=[y])

        def phase_E(L):
            S.barrier()
            ar.reset()
            yT = ar.alloc([32, T], BF16)
            for c0 in range(0, 32, 8):
                S.dma("sp", lambda e, c0=c0: e.dma_start(out=yT[:, c0:c0 + 8, :], in_=YT[c0:c0 + 8].rearrange("c p t -> p c t")), writes=[yT])
            wbE = [ar.alloc([32, 256], BF16) for _ in range(2)]
            sg = [ar.alloc([3, 2, 512], BF16) for _ in range(2)]
            tmp = [ar.alloc([512], F32) for _ in range(6)]
            mo = [ar.alloc([2, T], BF16) for _ in range(1)]
            it = 0

            def loadw(fg):
                wbuf = wbE[fg % 2]
                cs = slice(fg * 256, fg * 256 + 256)
                S.dma("pool", lambda e: e.dma_start(out=wbuf[:, 0:8, :], in_=p_moba[L][:, cs].rearrange("(kc p) f -> p kc f", p=128)), writes=[wbuf])
                S.dma("pool", lambda e: e.dma_start(out=wbuf[:, 8:24, :], in_=p_ret[L][:, cs].rearrange("(kc p) f -> p kc f", p=128)), writes=[wbuf])
                S.dma("pool", lambda e: e.dma_start(out=wbuf[:, 24:32, :], in_=p_mem[L][:, cs].rearrange("(kc p) f -> p kc f", p=128)), writes=[wbuf])

            loadw(0)
            for fg in range(8):
                if fg + 1 < 8:
                    loadw(fg + 1)
                wbuf = wbE[fg % 2]
                mout = mo[0]
                for tg in range(4):
                    ts = sl(tg, 512)
                    sgt = sg[it % 2]
                    it += 1
                    for br in range(3):
                        S.dma("sp", lambda e, br=br: e.dma_start(out=sgt[:, br, :, :], in_=GT[br * 16 + fg * 2:br * 16 + fg * 2 + 2, :, ts].rearrange("c p t -> p c t")), writes=[sgt])
                    for c in range(2):
                        zs = []
                        for (k0, k1) in ((0, 8), (8, 24), (24, 32)):
                            p = nextps()
                            for kc in range(k0, k1):
                                mm(p, p[:, :], wbuf[:, kc, sl(c, 128)], yT[:, kc, ts], kc == k0, kc == k1 - 1, [wbuf, yT])
                            zs.append(p)
                        t3 = tmp[(c % 2) * 3:(c % 2) * 3 + 3]
                        for br in range(3):
                            S.op("dve", lambda e, br=br: e.tensor_tensor(t3[br][:, :], zs[br][:, :], sgt[:, br, c, :], ALU.mult), reads=[zs[br], sgt], writes=[t3[br]])
                        S.op("pool", lambda e: e.tensor_tensor(t3[0][:, :], t3[0][:, :], t3[1][:, :], ALU.add), reads=[t3[1]], writes=[t3[0]])
                        S.op("pool", lambda e: e.tensor_tensor(mout[:, c, ts], t3[0][:, :], t3[2][:, :], ALU.add), reads=[t3[0], t3[2]], writes=[mout])
                S.dma("sp", lambda e: e.dma_start(out=MT[fg * 2:fg * 2 + 2].rearrange("c p t -> p c t"), in_=mout[:, :, :]), reads=[mout])

        def ln_tail(y, G, Bv, sm, x_dst, xT_dst, tt, x1b, xTt, router=None):
            st = sm["st"]
            for fs in range(4):
                S.op("dve", lambda e, fs=fs: e.bn_stats(st[:, fs, :], y[:, sl(fs, 512)]), reads=[y], writes=[st])
            S.op("dve", lambda e: e.bn_aggr(sm["mv"][:, :], st[:, :, :].rearrange("p a b -> p (a b)")), reads=[st], writes=[sm["mv"]])
            S.op("act", lambda e: e.activation(sm["lnv"][:, :], sm["mv"][:, 1:2], AF.Ln, bias=LN_EPS), reads=[sm["mv"]], writes=[sm["lnv"]])
            S.op("act", lambda e: e.activation(sm["rstd"][:, :], sm["lnv"][:, :], AF.Exp, scale=-0.5), reads=[sm["lnv"]], writes=[sm["rstd"]])
            S.op("dve", lambda e: e.scalar_tensor_tensor(sm["nmr"][:, :], sm["mv"][:, 0:1], -1.0, sm["rstd"][:, :], ALU.mult, ALU.mult), reads=[sm["mv"], sm["rstd"]], writes=[sm["nmr"]])
            S.op("act", lambda e: e.activation(y[:, :], y[:, :], AF.Identity, bias=sm["nmr"][:, 0:1], scale=sm["rstd"][:, 0:1]), reads=[sm["nmr"], sm["rstd"]], writes=[y])
            S.op("pool", lambda e: e.tensor_tensor(y[:, :], y[:, :], G[:, :], ALU.mult), reads=[G], writes=[y])
            S.op("dve", lambda e: e.tensor_tensor(y[:, :], y[:, :], Bv[:, :], ALU.add), reads=[Bv], writes=[y])
            S.dma("sp", lambda e: e.dma_start(out=x_dst[sl(tt, 128), :], in_=y[:, :]), reads=[y])
            if xT_dst is None:
                return
            S.op("act", lambda e: e.copy(x1b[:, :], y[:, :]), reads=[y], writes=[x1b])
            if router is not None:
                S.dma("sp", lambda e: e.dma_start(out=X1B[sl(tt, 128), :], in_=x1b[:, :]), reads=[x1b])
            for half in range(2):
                tp = ps[4 + half]
                for j in range(8):
                    kc = half * 8 + j
                    S.op("pe", lambda e, tp=tp, j=j, kc=kc: e.transpose(tp.bf[:, sl(j, 128)], x1b[:, sl(kc, 128)], ident[:, :]), reads=[x1b, ident], writes=[tp])
                if half == 0:
                    S.op("act", lambda e, tp=tp: e.copy(xTt[:, 0:8, :], tp.bf[:, :].rearrange("p (a b) -> p a b", a=8)), reads=[tp], writes=[xTt])
                else:
                    S.op("dve", lambda e, tp=tp: e.tensor_copy(xTt[:, 8:16, :], tp.bf[:, :].rearrange("p (a b) -> p a b", a=8)), reads=[tp], writes=[xTt])
            S.dma("sp", lambda e: e.dma_start(out=xT_dst[:, :, sl(tt, 128)].rearrange("c p t -> p c t"), in_=xTt[:, :, :]), reads=[xTt])
            if router is not None:
                router(xTt, tt)

        def ln_small():
            return {"st": ar.alloc([4, 6], F32), "mv": ar.alloc([2], F32), "lnv": ar.alloc([1], F32), "rstd": ar.alloc([1], F32), "nmr": ar.alloc([1], F32)}

        def phase_F(L):
            S.barrier()
            ar.reset()
            wo = [ar.alloc([16, 512], BF16) for _ in range(4)]
            for g in range(4):
                src = w_o[L][:, sl(g, 512)].rearrange("(kc p) f -> p kc f", p=128)
                S.dma("pool", lambda e, src=src, g=g: e.dma_start(out=wo[g][:, :, :], in_=src), writes=[wo[g]])
            G = ar.alloc([D], F32)
            Bv = ar.alloc([D], F32)
            S.dma("sp", lambda e: e.dma_start(out=G[:, :], in_=ln_par[L, 0, :, :]), writes=[G])
            S.dma("sp", lambda e: e.dma_start(out=Bv[:, :], in_=ln_par[L, 1, :, :]), writes=[Bv])
            wr = ar.alloc([16, 36], BF16)
            S.dma("pool", lambda e: e.dma_start(out=wr[:, :, :], in_=w_r[L].rearrange("(kc p) f -> p kc f", p=128)), writes=[wr])
            br = ar.alloc([36], F32)
            S.dma("sp", lambda e: e.dma_start(out=br[:, :], in_=b_r[L, :, :]), writes=[br])
            mT = [ar.alloc([16, 128], BF16) for _ in range(2)]
            xres = [ar.alloc([D], F32) for _ in range(2)]
            ys = [ar.alloc([D], F32) for _ in range(2)]
            x1b = [ar.alloc([D], BF16) for _ in range(2)]
            xTt = [ar.alloc([16, 128], BF16) for _ in range(2)]
            sms = [ln_small() for _ in range(2)]
            lg = ar.alloc([36], F32)
            r_ = {k: ar.alloc([n], F32) for k, n in (("gmax", 1), ("ngmax", 1), ("ge", 4), ("gsum", 1), ("gmask", 4), ("pen", 4),
                                                        ("em", 32), ("mx", 8), ("sel", 32), ("nv1", 1), ("ex", 32), ("e2", 1), ("den", 1), ("rr", 1), ("W", 32))}
            xsrc = x_in if L == 0 else X2

            def router(xt, tt):
                lp = ps[6]
                for kc in range(16):
                    mm(lp, lp[:, 0:36], xt[:, kc, :], wr[:, kc, :], kc == 0, kc == 15, [xt, wr])
                R = r_
                S.op("dve", lambda e: e.tensor_tensor(lg[:, :], lp[:, 0:36], br[:, :], ALU.add), reads=[lp, br], writes=[lg])
                S.op("dve", lambda e: e.tensor_reduce(R["gmax"][:, :], lg[:, 0:4], AX.X, ALU.max), reads=[lg], writes=[R["gmax"]])
                S.op("dve", lambda e: e.tensor_scalar(R["ngmax"][:, :], R["gmax"][:, :], -1.0, None, ALU.mult), reads=[R["gmax"]], writes=[R["ngmax"]])
                S.op("act", lambda e: e.activation(R["ge"][:, :], lg[:, 0:4], AF.Exp, bias=R["ngmax"][:, 0:1], accum_out=R["gsum"][:, 0:1]), reads=[lg, R["ngmax"]], writes=[R["ge"], R["gsum"]])
                S.op("dve", lambda e: e.tensor_scalar(R["gmask"][:, :], lg[:, 0:4], R["gmax"][:, 0:1], None, ALU.is_ge), reads=[lg, R["gmax"]], writes=[R["gmask"]])
                S.op("dve", lambda e: e.tensor_scalar(R["pen"][:, :], R["gmask"][:, :], -1.0, 1e30, ALU.add, ALU.mult), reads=[R["gmask"]], writes=[R["pen"]])
                for g in range(4):
                    S.op("dve", lambda e, g=g: e.tensor_scalar(R["em"][:, sl(g, 8)], lg[:, 4 + g * 8:12 + g * 8], R["pen"][:, g:g + 1], None, ALU.add), reads=[lg, R["pen"]], writes=[R["em"]])
                S.op("dve", lambda e: e.max(R["mx"][:, :], R["em"][:, :]), reads=[R["em"]], writes=[R["mx"]])
                S.op("dve", lambda e: e.tensor_scalar(R["sel"][:, :], R["em"][:, :], R["mx"][:, 1:2], None, ALU.is_ge), reads=[R["em"], R["mx"]], writes=[R["sel"]])
                S.op("dve", lambda e: e.tensor_scalar(R["nv1"][:, :], R["mx"][:, 0:1], -1.0, None, ALU.mult), reads=[R["mx"]], writes=[R["nv1"]])
                S.op("act", lambda e: e.activation(R["ex"][:, :], R["em"][:, :], AF.Exp, bias=R["nv1"][:, 0:1]), reads=[R["em"], R["nv1"]], writes=[R["ex"]])
                S.op("act", lambda e: e.activation(R["e2"][:, :], R["mx"][:, 1:2], AF.Exp, bias=R["nv1"][:, 0:1]), reads=[R["mx"], R["nv1"]], writes=[R["e2"]])
                S.op("dve", lambda e: e.scalar_tensor_tensor(R["den"][:, :], R["e2"][:, :], 1.0, R["gsum"][:, :], ALU.add, ALU.mult), reads=[R["e2"], R["gsum"]], writes=[R["den"]])
                S.op("dve", lambda e: e.reciprocal(R["rr"][:, :], R["den"][:, :]), reads=[R["den"]], writes=[R["rr"]])
                S.op("act", lambda e: e.copy(selS_all[:, tt, :], R["sel"][:, :]), reads=[R["sel"]], writes=[selS_all])
                S.op("dve", lambda e: e.tensor_scalar(selA_all[:, tt, :], R["em"][:, :], R["mx"][:, 0:1], None, ALU.is_ge), reads=[R["em"], R["mx"]], writes=[selA_all])
                S.op("act", lambda e: e.copy(wab_all[:, tt, 0:1], R["rr"][:, :]), reads=[R["rr"]], writes=[wab_all])
                S.op("dve", lambda e: e.tensor_tensor(wab_all[:, tt, 1:2], R["rr"][:, :], R["e2"][:, :], ALU.mult), reads=[R["rr"], R["e2"]], writes=[wab_all])

            def load(tt):
                b = tt % 2
                S.dma("sp", lambda e: e.dma_start(out=mT[b][:, :, :], in_=MT[:, :, sl(tt, 128)].rearrange("c p t -> p c t")), writes=[mT[b]])
                S.dma("sp", lambda e: e.dma_start(out=xres[b][:, :], in_=xsrc[sl(tt, 128), :]), writes=[xres[b]])

            def mm_pe(tt):
                b = tt % 2
                for fs in range(4):
                    p = ps[fs]
                    for kc in range(16):
                        mm(p, p[:, :], mT[b][:, kc, :], wo[fs][:, kc, :], kc == 0, kc == 15, [mT[b], wo[fs]])

            def mm_stt(tt):
                b = tt % 2
                y = ys[b]
                for fs in range(4):
                    p = ps[fs]
                    S.op("dve", lambda e, p=p, fs=fs: e.scalar_tensor_tensor(y[:, sl(fs, 512)], xres[b][:, sl(fs, 512)], ALPHA, p[:, :], ALU.mult, ALU.add), reads=[p, xres[b]], writes=[y])

            load(0)
            load(1)
            mm_pe(0)
            mm_stt(0)
            for tt in range(16):
                b = tt % 2
                if tt + 1 < 16:
                    mm_pe(tt + 1)
                ln_tail(ys[b], G, Bv, sms[b], X1, X1T, tt, x1b[b], xTt[b], router)
                if tt + 1 < 16:
                    mm_stt(tt + 1)
                if tt + 2 < 16:
                    load(tt + 2)
            cnt = ar.alloc([32], F32)
            accs = [ar.alloc([32], F32) for _ in range(2)]
            cps = ps[6]
            for tt in range(16):
                mm(cps, cps[:, 0:32], ones[:, :], selS_all[:, tt, :], tt == 0, tt == 15, [ones, selS_all])
            S.op("dve", lambda e: e.tensor_copy(cnt[:, :], cps[:, 0:32]), reads=[cps], writes=[cnt])
            S.op("dve", lambda e: e.tensor_scalar(accs[0][:, :], cnt[:, :], 0.5, None, ALU.is_gt), reads=[cnt], writes=[accs[0]])
            NJ = T // BLK_ROWS
            for j in range(1, NJ):
                S.op("dve", lambda e, j=j: e.scalar_tensor_tensor(accs[j % 2][:, :], cnt[:, :], float(BLK_ROWS) * j + 0.5, accs[(j + 1) % 2][:, :], ALU.is_gt, ALU.add), reads=[cnt, accs[(j + 1) % 2]], writes=[accs[j % 2]])
            padded = ar.alloc([32], F32)
            S.op("dve", lambda e: e.tensor_scalar(padded[:, :], accs[(NJ - 1) % 2][:, :], float(BLK_ROWS), None, ALU.mult), reads=[accs[(NJ - 1) % 2]], writes=[padded])
            cs = [ar.alloc([32], F32) for _ in range(2)]
            S.op("dve", lambda e: e.tensor_copy(cs[0][:, :], padded[:, :]), reads=[padded], writes=[cs[0]])
            k = 0
            for dsh in (1, 2, 4, 8, 16):
                a_, b_ = cs[k % 2], cs[(k + 1) % 2]
                S.op("dve", lambda e, a_=a_, b_=b_, dsh=dsh: e.tensor_copy(b_[:, 0:dsh], a_[:, 0:dsh]), reads=[a_], writes=[b_])
                S.op("dve", lambda e, a_=a_, b_=b_, dsh=dsh: e.tensor_tensor(b_[:, dsh:32], a_[:, dsh:32], a_[:, 0:32 - dsh], ALU.add), reads=[a_], writes=[b_])
                k += 1
            pend = cs[k % 2]
            pstart = ar.alloc([32], F32)
            S.op("dve", lambda e: e.tensor_tensor(pstart[:, :], pend[:, :], padded[:, :], ALU.subtract), reads=[pend, padded], writes=[pstart])
            thr = ar.alloc([N_BLK], F32)
            S.dma("sp", lambda e: e.dma_start(out=thr[:, :], in_=cin["c_thr"][:, :]), writes=[thr])
            bacc = [ar.alloc([N_BLK], F32) for _ in range(2)]
            S.op("dve", lambda e: e.tensor_scalar(bacc[0][:, :], thr[:, :], pend[:, 0:1], None, ALU.is_ge), reads=[thr, pend], writes=[bacc[0]])
            for ex in range(1, 32):
                S.op("dve", lambda e, ex=ex: e.scalar_tensor_tensor(bacc[ex % 2][:, :], thr[:, :], pend[:, ex:ex + 1], bacc[(ex + 1) % 2][:, :], ALU.is_ge, ALU.add), reads=[thr, pend, bacc[(ex + 1) % 2]], writes=[bacc[ex % 2]])
            S.op("dve", lambda e: e.tensor_scalar(blkE[:, :], bacc[1][:, :], 31.0, None, ALU.min), reads=[bacc[1]], writes=[blkE])
            S.op("dve", lambda e: e.tensor_scalar(blkU[:, :], thr[:, :], pend[:, 31:32], OOB, ALU.is_ge, ALU.mult), reads=[thr, pend], writes=[blkU])
            dtmp = ar.alloc([32], F32)
            dprod = ar.alloc([32], F32)
            selB = ar.alloc([32], F32)
            dflt = ar.alloc([16, 2], F32)
            for tt in range(16):
                rp = ps[tt % 2]
                for t2 in range(tt):
                    mm(rp, rp[:, 0:32], ones[:, :], selS_all[:, t2, :], t2 == 0, False, [ones, selS_all])
                mm(rp, rp[:, 0:32], ustrict[:, :], selS_all[:, tt, :], tt == 0, True, [ustrict, selS_all])
                S.op("dve", lambda e, rp=rp: e.tensor_tensor(dtmp[:, :], rp[:, 0:32], pstart[:, :], ALU.add), reads=[rp, pstart], writes=[dtmp])
                S.op("dve", lambda e, tt=tt: e.tensor_tensor(dprod[:, :], dtmp[:, :], selA_all[:, tt, :], ALU.mult), reads=[dtmp, selA_all], writes=[dprod])
                S.op("dve", lambda e, tt=tt: e.tensor_reduce(dflt[:, tt, 0:1], dprod[:, :], AX.X, ALU.add), reads=[dprod], writes=[dflt])
                S.op("dve", lambda e, tt=tt: e.tensor_tensor(selB[:, :], selS_all[:, tt, :], selA_all[:, tt, :], ALU.subtract), reads=[selS_all, selA_all], writes=[selB])
                S.op("dve", lambda e: e.tensor_tensor(dprod[:, :], dtmp[:, :], selB[:, :], ALU.mult), reads=[dtmp, selB], writes=[dprod])
                S.op("dve", lambda e, tt=tt: e.tensor_reduce(dflt[:, tt, 1:2], dprod[:, :], AX.X, ALU.add), reads=[dprod], writes=[dflt])
            S.op("dve", lambda e: e.tensor_copy(dest_i[:, :, :], dflt[:, :, :]), reads=[dflt], writes=[dest_i])
            if debug:
                S.dma("sp", lambda e: e.dma_start(out=DBG_dest[:, :], in_=dest_i[:, :, :].rearrange("p a b -> p (a b)")), reads=[dest_i])
                S.dma("sp", lambda e: e.dma_start(out=DBG_blk[:, :], in_=blkE[:, :]), reads=[blkE])
                S.dma("sp", lambda e: e.dma_start(out=DBG_wab[:, :], in_=wab_all[:, :, :].rearrange("p a b -> p (a b)")), reads=[wab_all])

        def phase_X(L):
            S.barrier()
            ar.reset()
            xb = [ar.alloc([D], BF16) for _ in range(3)]
            for tt in range(16):
                b = xb[tt % 3]
                S.dma("sp", lambda e, b=b, tt=tt: e.dma_start(out=b[:, :], in_=X1B[sl(tt, 128), :]), writes=[b])
                for s_ in range(2):
                    S.dma("pool", lambda e, b=b, tt=tt, s_=s_: e.indirect_dma_start(
                        out=Xs[:, :], out_offset=bass.IndirectOffsetOnAxis(ap=dest_i[:, tt, s_:s_ + 1], axis=0),
                        in_=b[:, :], in_offset=None), reads=[b, dest_i])

        def phase_M(L):
            S.barrier()
            ar.reset()
            base = ar.alloc([12], F32)
            S.dma("sp", lambda e: e.dma_start(out=base[:, :], in_=cin["c_base"][:, :]), writes=[base])
            b1024 = ar.alloc([N_BLK], F32)
            b512 = ar.alloc([N_BLK], F32)
            S.op("dve", lambda e: e.tensor_scalar(b1024[:, :], blkE[:, :], 1024.0, float(L * N_EXP * 1024), ALU.mult, ALU.add), reads=[blkE], writes=[b1024])
            S.op("dve", lambda e: e.tensor_scalar(b512[:, :], blkE[:, :], 512.0, float(L * N_EXP * 512), ALU.mult, ALU.add), reads=[blkE], writes=[b512])
            S.op("dve", lambda e: e.tensor_tensor(b1024[:, :], b1024[:, :], blkU[:, :], ALU.add), reads=[blkU], writes=[b1024])
            S.op("dve", lambda e: e.tensor_tensor(b512[:, :], b512[:, :], blkU[:, :], ALU.add), reads=[blkU], writes=[b512])
            idf = ar.alloc([N_BLK, 12], F32)
            idi = ar.alloc([N_BLK, 12], I32)
            for j in range(12):
                srcb = b1024 if j < 8 else b512
                S.op("dve", lambda e, j=j, srcb=srcb: e.tensor_scalar(idf[:, :, j], srcb[:, :], base[:, j:j + 1], None, ALU.add), reads=[srcb, base], writes=[idf])
            S.op("dve", lambda e: e.tensor_copy(idi[:, :, :], idf[:, :, :]), reads=[idf], writes=[idi])
            NWB = 3
            wgu = [ar.alloc([8, 2048], BF16) for _ in range(NWB)]
            wdn = [ar.alloc([4, 2048], BF16) for _ in range(NWB)]
            xb = [ar.alloc([D], BF16) for _ in range(2)]
            xbT = [ar.alloc([16, 128], BF16) for _ in range(2)]
            sg = [ar.alloc([512], F32) for _ in range(2)]
            actb = [ar.alloc([512], BF16) for _ in range(2)]
            actT = [ar.alloc([4, 128], BF16) for _ in range(2)]
            ysb = [ar.alloc([D], F32) for _ in range(2)]
            RT = BLK_ROWS // 128
            order = []
            lo, hi = 0, N_BLK - 1
            while lo <= hi:
                order.append(lo)
                if hi != lo:
                    order.append(hi)
                lo += 1
                hi -= 1
            rows = [(bk * RT + rt, pos % NWB) for pos, bk in enumerate(order) for rt in range(RT)]
            NR = len(rows)

            def loadw(pos):
                bk = order[pos]
                i = pos % NWB
                for j in range(8):
                    S.dma("pool", lambda e, j=j: e.indirect_dma_start(out=wgu[i][:, j, :], out_offset=None, in_=w_gup[:, :],
                                                                     in_offset=bass.IndirectOffsetOnAxis(ap=idi[:, bk, j:j + 1], axis=0),
                                                                     bounds_check=RegConst(DEPTH * N_EXP * 128 * 8 - 1), oob_is_err=False), reads=[idi], writes=[wgu[i]])
                for j in range(4):
                    S.dma("pool", lambda e, j=j: e.indirect_dma_start(out=wdn[i][:, j, :], out_offset=None, in_=w_dnp[:, :],
                                                                     in_offset=bass.IndirectOffsetOnAxis(ap=idi[:, bk, 8 + j:9 + j], axis=0),
                                                                     bounds_check=RegConst(DEPTH * N_EXP * 128 * 4 - 1), oob_is_err=False), reads=[idi], writes=[wdn[i]])

            def loadx(n):
                S.dma("sp", lambda e: e.dma_start(out=xb[n % 2][:, :], in_=Xs[sl(rows[n][0], 128), :]), writes=[xb[n % 2]])

            def st1(n):
                x_, xt = xb[n % 2], xbT[n % 2]
                for half in range(2):
                    tp = ps[6 + half]
                    for j in range(8):
                        kc = half * 8 + j
                        S.op("pe", lambda e, tp=tp, j=j, kc=kc: e.transpose(tp.bf[:, sl(j, 128)], x_[:, sl(kc, 128)], ident[:, :]), reads=[x_, ident], writes=[tp])
                    if half == 0:
                        S.op("act", lambda e, tp=tp: e.copy(xt[:, 0:8, :], tp.bf[:, :].rearrange("p (a b) -> p a b", a=8)), reads=[tp], writes=[xt])
                    else:
                        S.op("dve", lambda e, tp=tp: e.tensor_copy(xt[:, 8:16, :], tp.bf[:, :].rearrange("p (a b) -> p a b", a=8)), reads=[tp], writes=[xt])

            def st2(n):
                i = rows[n][1]
                k = n % 2
                wg = wgu[i][:, :, :].rearrange("p a (b c) -> p (a b) c", b=2)
                xt = xbT[k]
                gp, up = ps[0], ps[1]
                for kc in range(16):
                    mm(gp, gp[:, :], xt[:, kc, :], wg[:, kc, 0:512], kc == 0, kc == 15, [xt, wgu[i]])
                for kc in range(16):
                    mm(up, up[:, :], xt[:, kc, :], wg[:, kc, 512:1024], kc == 0, kc == 15, [xt, wgu[i]])
                S.op("act", lambda e: e.activation(sg[k][:, :], gp[:, :], AF.Silu), reads=[gp], writes=[sg[k]])
                S.op("dve", lambda e: e.tensor_tensor(actb[k][:, :], up[:, :], sg[k][:, :], ALU.mult), reads=[up, sg[k]], writes=[actb[k]])

            def st3(n):
                k = n % 2
                tp = ps[2]
                for c in range(4):
                    S.op("pe", lambda e, c=c: e.transpose(tp.bf[:, sl(c, 128)], actb[k][:, sl(c, 128)], ident[:, :]), reads=[actb[k], ident], writes=[tp])
                S.op("act", lambda e: e.copy(actT[k][:, :, :], tp.bf[:, 0:512].rearrange("p (a b) -> p a b", a=4)), reads=[tp], writes=[actT[k]])

            def st4(n):
                r, i = rows[n]
                k = n % 2
                wd, at, yb = wdn[i], actT[k], ysb[k]
                for dg in range(4):
                    yp = nextps_m()
                    for c in range(4):
                        mm(yp, yp[:, :], at[:, c, :], wd[:, c, sl(dg, 512)], c == 0, c == 3, [at, wd])
                    if dg % 2 == 0:
                        S.op("act", lambda e, yp=yp, dg=dg: e.copy(yb[:, sl(dg, 512)], yp[:, :]), reads=[yp], writes=[yb])
                    else:
                        S.op("dve", lambda e, yp=yp, dg=dg: e.tensor_copy(yb[:, sl(dg, 512)], yp[:, :]), reads=[yp], writes=[yb])
                S.dma("sp", lambda e: e.dma_start(out=Ys[sl(r, 128), :], in_=yb[:, :]), reads=[yb])

            loadw(0)
            loadw(1)
            loadx(0)
            loadx(1)
            st1(0)
            for n in range(NR):
                st2(n)
                if n > 0:
                    st4(n - 1)
                if n % RT == 0 and n // RT + 2 < N_BLK:
                    loadw(n // RT + 2)
                if n + 1 < NR:
                    st1(n + 1)
                if n + 2 < NR:
                    loadx(n + 2)
                st3(n)
            st4(NR - 1)

        def phase_G(L, last):
            S.barrier()
            ar.reset()
            G = ar.alloc([D], F32)
            Bv = ar.alloc([D], F32)
            S.dma("sp", lambda e: e.dma_start(out=G[:, :], in_=ln_par[L, 2, :, :]), writes=[G])
            S.dma("sp", lambda e: e.dma_start(out=Bv[:, :], in_=ln_par[L, 3, :, :]), writes=[Bv])
            x1 = [ar.alloc([D], F32) for _ in range(2)]
            ff = [ar.alloc([D], F32) for _ in range(2)]
            fb = [ar.alloc([D], F32) for _ in range(2)]
            x1b = [ar.alloc([D], BF16) for _ in range(2)]
            xTt = [ar.alloc([16, 128], BF16) for _ in range(2)]
            sms = [ln_small() for _ in range(2)]

            def load(tt):
                b = tt % 2
                S.dma("sp", lambda e: e.dma_start(out=x1[b][:, :], in_=X1[sl(tt, 128), :]), writes=[x1[b]])
                S.dma("pool", lambda e: e.indirect_dma_start(out=ff[b][:, :], out_offset=None, in_=Ys[:, :], in_offset=bass.IndirectOffsetOnAxis(ap=dest_i[:, tt, 0:1], axis=0)), reads=[dest_i], writes=[ff[b]])
                S.dma("pool", lambda e: e.indirect_dma_start(out=fb[b][:, :], out_offset=None, in_=Ys[:, :], in_offset=bass.IndirectOffsetOnAxis(ap=dest_i[:, tt, 1:2], axis=0)), reads=[dest_i], writes=[fb[b]])

            load(0)
            for tt in range(16):
                if tt + 1 < 16:
                    load(tt + 1)
                b = tt % 2
                y = ff[b]
                S.op("act", lambda e, y=y, tt=tt: e.activation(y[:, :], y[:, :], AF.Identity, scale=wab_all[:, tt, 0:1]), reads=[wab_all], writes=[y])
                S.op("dve", lambda e, y=y, b=b, tt=tt: e.scalar_tensor_tensor(y[:, :], fb[b][:, :], wab_all[:, tt, 1:2], y[:, :], ALU.mult, ALU.add), reads=[fb[b], wab_all], writes=[y])
                S.op("dve", lambda e, y=y, b=b: e.scalar_tensor_tensor(y[:, :], x1[b][:, :], ALPHA, y[:, :], ALU.mult, ALU.add), reads=[x1[b]], writes=[y])
                ln_tail(y, G, Bv, sms[b], out if last else X2, None if last else X2T, tt, x1b[b], xTt[b])

        for L in range(n_layers):
            phase_A(L)
            phase_B(L)
            phase_C(L)
            phase_D(L)
            phase_E(L)
            phase_F(L)
            if no_indirect:
                continue
            phase_X(L)
            if stop_after == "X":
                continue
            phase_M(L)
            if stop_after == "M":
                continue
            phase_G(L, L == n_layers - 1)
        S.finish()
        S.emit()
    return nc, consts


_CACHE = {}


def _prep_shared(inp):
    f = lambda a: np.ascontiguousarray(np.asarray(a, dtype=np.float32))
    sh = {}
    for k in ["w_in", "p_moba", "p_ret", "p_mem", "w_mem_kv", "w_o"]:
        sh[k] = f(inp[k])
    g = f(inp["w_gate_up"]).reshape(DEPTH, N_EXP, 16, 128, 1024).transpose(0, 1, 3, 2, 4)
    sh["w_gup"] = np.ascontiguousarray(g).reshape(DEPTH * N_EXP * 128 * 8, 2048)
    dn = f(inp["w_down"]).reshape(DEPTH, N_EXP, 4, 128, D).transpose(0, 1, 3, 2, 4)
    sh["w_dnp"] = np.ascontiguousarray(dn).reshape(DEPTH * N_EXP * 128 * 4, 2048)
    lnp = np.stack([f(inp["ln1_g"]), f(inp["ln1_b"]), f(inp["ln2_g"]), f(inp["ln2_b"])], 1)
    sh["ln_par"] = np.ascontiguousarray(np.broadcast_to(lnp[:, :, None, :], (DEPTH, 4, 128, D)))
    sh["w_r"] = np.ascontiguousarray(np.concatenate([f(inp["w_group"]), f(inp["w_expert"])], -1))
    br = np.concatenate([f(inp["b_group"]), f(inp["b_expert"])], -1)
    sh["b_r"] = np.ascontiguousarray(np.broadcast_to(br[:, None, :], (DEPTH, 128, 36)))
    return sh


def kernel(**inputs):
    if "nc" not in _CACHE:
        _CACHE["nc"] = build()
    nc, consts = _CACHE["nc"]
    sh = _prep_shared(inputs)
    x = np.asarray(inputs["x"], dtype=np.float32)
    mem = np.asarray(inputs["mem"], dtype=np.float32)
    in_maps = []
    for b in range(8):
        m = dict(sh)
        m.update(consts)
        m["x"] = np.ascontiguousarray(x[b])
        m["xT"] = np.ascontiguousarray(x[b].T)
        m["memT"] = np.ascontiguousarray(mem[b].T)
        in_maps.append(m)
    res = run_bass_kernel_spmd(nc, in_maps, core_ids=list(range(8)))
    return np.stack([np.asarray(r["out"], dtype=np.float32) for r in res.results], 0)
```

```python
import math
import numpy as np
import ml_dtypes
import concourse.bass as bass
import concourse.mybir as mybir
from concourse.bass_utils import run_bass_kernel_spmd
from contextlib import ExitStack

F32 = mybir.dt.float32
BF16 = mybir.dt.bfloat16
I32 = mybir.dt.int32
AF = mybir.ActivationFunctionType
ALU = mybir.AluOpType
AX = mybir.AxisListType

T = 2048
D = 2048
DEPTH = 2
ALPHA = float((2 * DEPTH) ** 0.25)
LN_EPS = 1e-5
GN_EPS = 1e-6
NEG = -30000.0
N_EXP = 32
BLK_ROWS = 256
N_BLK = 48
OOB = float(2 ** 20)


class Buf:
    def __init__(self, ap, name=""):
        self.t = ap
        self.name = name
        self.w = []
        self.pr = []
        self.r = []
        self.bf = None

    def __getitem__(self, idx):
        return self.t[idx]


class _Rec:
    def __init__(self):
        self.call = None

    def __getattr__(self, name):
        def f(*a, **k):
            self.call = (name, a, k)
            return self
        return f


class RegConst:
    def __init__(self, v):
        self.v = v


_REG_CACHE = {}


def _resolve(e, k):
    out = {}
    for key, val in k.items():
        if isinstance(val, RegConst):
            ck = (id(e), val.v)
            if ck not in _REG_CACHE:
                _REG_CACHE[ck] = e.to_reg(val.v)
            val = _REG_CACHE[ck]
        out[key] = val
    return out


def _record(fn):
    r = _Rec()
    fn(r)
    return r.call


class EngState:
    def __init__(self, name):
        self.name = name
        self.cnt = 0
        self.ops = []
        self.seen = {}


class Sched:
    N_DMA_SEMS = 16

    def __init__(self, nc, stack):
        self.nc = nc
        self.sems = {}
        self.engs = {}
        for name in ["pe", "dve", "act", "pool", "sp"]:
            self.sems[name] = stack.enter_context(nc.semaphore("s_" + name))
            self.engs[name] = EngState(name)
        self.dma_sems = {}
        self.dma_next = {}
        self.dma_uses = {}
        for q in ["sp", "pool"]:
            lst = []
            for i in range(self.N_DMA_SEMS):
                key = "d_%s_%d" % (q, i)
                self.sems[key] = stack.enter_context(nc.semaphore(key))
                lst.append(key)
                self.dma_uses[key] = 0
            self.dma_sems[q] = lst
            self.dma_next[q] = 0

    def _wait(self, eng, deps):
        best = {}
        for d in deps:
            if d is None:
                continue
            k, v = d
            if best.get(k, 0) < v:
                best[k] = v
        for k, v in best.items():
            if k == eng.name and eng.name == "pe":
                continue
            if eng.seen.get(k, 0) >= v:
                continue
            eng.seen[k] = v
            sem = self.sems[k]
            eng.ops.append(lambda e, sem=sem, v=v: e.wait_ge(sem, v))

    @staticmethod
    def _deps(reads, writes, is_dma=False):
        deps = []
        for b in reads:
            deps.extend(b.w)
        for b in writes:
            deps.extend(b.r)
            if is_dma:
                deps.extend(t for t in b.w if not t[0].startswith("d_"))
                deps.extend(b.pr)
            else:
                deps.extend(b.w)
        return deps

    @staticmethod
    def _commit(tok, reads, writes, is_dma=False):
        for b in writes:
            if is_dma and not b.r and all(t[0].startswith("d_") for t in b.w):
                b.w = b.w + [tok]
            else:
                b.pr = list(b.r)
                b.w = [tok]
            b.r = []
        for b in reads:
            if b not in writes:
                b.r.append(tok)

    def op(self, engname, fn, reads=(), writes=()):
        eng = self.engs[engname]
        self._wait(eng, self._deps(reads, writes))
        eng.cnt += 1
        tok = (eng.name, eng.cnt)
        sem = self.sems[eng.name]
        name, a, k = _record(fn)
        eng.ops.append(lambda e, name=name, a=a, k=k, sem=sem: getattr(e, name)(*a, **k).then_inc(sem, 1))
        self._commit(tok, reads, writes)
        return tok

    def dma(self, q, fn, reads=(), writes=()):
        eng = self.engs[q]
        i = self.dma_next[q]
        self.dma_next[q] = (i + 1) % self.N_DMA_SEMS
        key = self.dma_sems[q][i]
        deps = self._deps(reads, writes, True)
        if self.dma_uses[key] > 0:
            deps.append((key, 16 * self.dma_uses[key]))
        self._wait(eng, deps)
        self.dma_uses[key] += 1
        tok = (key, 16 * self.dma_uses[key])
        sem = self.sems[key]
        name, a, k = _record(fn)
        eng.ops.append(lambda e, name=name, a=a, k=k, sem=sem: getattr(e, name)(*a, **_resolve(e, k)).then_inc(sem, 16))
        self._commit(tok, reads, writes, True)
        return tok

    def raw_dma(self, q, fn, reads=(), writes=()):
        eng = self.engs[q]
        i = self.dma_next[q]
        self.dma_next[q] = (i + 1) % self.N_DMA_SEMS
        key = self.dma_sems[q][i]
        deps = self._deps(reads, writes)
        if self.dma_uses[key] > 0:
            deps.append((key, 16 * self.dma_uses[key]))
        self._wait(eng, deps)
        self.dma_uses[key] += 1
        tok = (key, 16 * self.dma_uses[key])
        sem = self.sems[key]
        eng.ops.append(lambda e, fn=fn, sem=sem: fn(e).then_inc(sem, 16))
        self._commit(tok, reads, writes)
        return tok

    def _all_tokens(self):
        deps = []
        for key, n in self.dma_uses.items():
            if n > 0:
                deps.append((key, 16 * n))
        for name in ["pe", "dve", "act", "pool"]:
            if self.engs[name].cnt > 0:
                deps.append((name, self.engs[name].cnt))
        return deps

    def barrier(self):
        deps = self._all_tokens()
        for name in ["pe", "dve", "act", "pool", "sp"]:
            self._wait(self.engs[name], deps)

    def finish(self):
        self._wait(self.engs["sp"], self._all_tokens())

    def emit(self):
        nc = self.nc
        engs = self.engs
        with nc.Block() as block:
            @block.tensor
            def _(e):
                for f in engs["pe"].ops:
                    f(e)

            @block.vector
            def _(e):
                for f in engs["dve"].ops:
                    f(e)

            @block.scalar
            def _(e):
                for f in engs["act"].ops:
                    f(e)

            @block.gpsimd
            def _(e):
                for f in engs["pool"].ops:
                    f(e)

            @block.sync
            def _(e):
                for f in engs["sp"].ops:
                    f(e)


class Arena:
    def __init__(self, t, nbytes):
        self.t = t
        self.nbytes = nbytes
        self.off = 0

    def reset(self):
        self.off = 0

    def alloc(self, free_shape, dtype, parts=128):
        n = 1
        for s in free_shape:
            n *= s
        esz = 4 if dtype in (F32, I32) else 2
        nb = (n * esz + 63) // 64 * 64
        assert self.off + nb <= self.nbytes, ("arena overflow", self.off, nb)
        v = self.t[0:parts, self.off // 2:(self.off + n * esz) // 2]
        self.off += nb
        if dtype in (F32, I32):
            v = v.bitcast(dtype)
        if len(free_shape) == 2:
            v = v.rearrange("p (a b) -> p a b", a=free_shape[0])
        elif len(free_shape) == 3:
            v = v.rearrange("p (a b c) -> p a b c", a=free_shape[0], b=free_shape[1])
        return Buf(v)


def _consts():
    c = {}
    bf = ml_dtypes.bfloat16
    c["c_ident"] = np.eye(128, dtype=np.float32).astype(bf)
    c["c_identf"] = np.eye(128, dtype=np.float32)
    c["c_ones"] = np.ones((128, 128), np.float32).astype(bf)
    half = 128
    inv = (10000.0 ** (-np.linspace(0.0, 1.0, half, dtype=np.float32))).astype(np.float32)
    pos = np.arange(T, dtype=np.float32)
    ang = (pos[None, :] * inv[:, None]).astype(np.float32)
    cos = np.cos(ang).astype(np.float32)
    sin = np.sin(ang).astype(np.float32)
    c["c_rot"] = np.stack([cos, sin, cos / 16.0, sin / 16.0], 0).astype(np.float32)
    nh = 4
    log_g = np.log1p(-np.power(2.0, -5.0 - np.arange(nh, dtype=np.float64)))
    idx = np.arange(128, dtype=np.float64)
    diff = idx[None, :] - idx[:, None]
    decT = np.where(diff >= 0, np.exp(log_g[:, None, None] * np.maximum(diff, 0.0)[None]), 0.0)
    c["c_decT"] = np.ascontiguousarray(decT.transpose(1, 0, 2)).astype(np.float32)
    xi = np.exp(log_g[:, None] * (idx + 1.0)[None, :])
    xi_t = np.tile(xi, (1, T // 128))
    c["c_xi"] = np.ascontiguousarray(np.broadcast_to(xi_t[:, None, :], (nh, 128, T))).astype(np.float32)
    zeta = np.exp(log_g[:, None] * (127.0 - idx)[None, :])
    c["c_zeta"] = np.ascontiguousarray(zeta.T).astype(np.float32)
    cd = np.exp(log_g * 128.0)
    qt = np.arange(16)[:, None]
    n = np.arange(8)[None, :]
    past = (n < (qt // 2)).astype(np.float32).reshape(1, 128)
    own = (n == (qt // 2)).astype(np.float32).reshape(1, 128)
    c["c_moba"] = np.ascontiguousarray(np.stack([
        np.broadcast_to((past - 1.0) * 1e30, (128, 128)),
        np.broadcast_to(past, (128, 128)),
        np.broadcast_to(own, (128, 128))], 0)).astype(np.float32)
    e8 = np.zeros((128, 1024), np.float32)
    for i in range(8):
        e8[i, i * 128:(i + 1) * 128] = 1.0
    c["c_e8"] = e8.astype(bf)
    k = np.arange(128)[:, None, None]
    j = np.arange(4)[None, :, None]
    q = np.arange(512)[None, None, :]
    c["c_cm"] = np.where((128 * j + k) > q, NEG, 0.0).astype(np.float32).astype(bf)
    pp = np.arange(128)
    c["c_ustrict"] = (pp[:, None] < pp[None, :]).astype(np.float32).astype(bf)
    c["c_thr"] = np.ascontiguousarray(np.broadcast_to((float(BLK_ROWS) * np.arange(N_BLK, dtype=np.float32))[None, :], (128, N_BLK)))
    c["c_base"] = np.concatenate([(pp[:, None] * 8 + np.arange(8)[None, :]), (pp[:, None] * 4 + np.arange(4)[None, :])], 1).astype(np.float32)
    return c, [float(v) for v in cd]


def build(n_layers=DEPTH, debug=False, no_indirect=False, stop_after=None):
    _REG_CACHE.clear()
    nc = bass.Bass("TRN2", target_bir_lowering=False)
    consts, CD = _consts()

    def din(name, shape, dt=F32):
        return nc.dram_tensor(name, list(shape), dt, kind="ExternalInput").ap()

    def dscr(name, shape, dt):
        return nc.dram_tensor(name, list(shape), dt, kind=("ExternalOutput" if debug else "Internal")).ap()

    x_in = din("x", [T, D])
    xT_in = din("xT", [D, T])
    memT_in = din("memT", [D, 256])
    w_in = din("w_in", [DEPTH, D, 16384])
    p_moba = din("p_moba", [DEPTH, 1024, D])
    p_ret = din("p_ret", [DEPTH, 2048, D])
    p_mem = din("p_mem", [DEPTH, 1024, D])
    w_mem_kv = din("w_mem_kv", [DEPTH, D, 2048])
    w_o = din("w_o", [DEPTH, D, D])
    ln_par = din("ln_par", [DEPTH, 4, 128, D])
    w_r = din("w_r", [DEPTH, D, 36])
    b_r = din("b_r", [DEPTH, 128, 36])
    w_gup = din("w_gup", [DEPTH * N_EXP * 128 * 8, 2048])
    w_dnp = din("w_dnp", [DEPTH * N_EXP * 128 * 4, 2048])
    cin = {}
    for k, v in consts.items():
        cin[k] = din(k, v.shape, BF16 if v.dtype == ml_dtypes.bfloat16 else F32)
    out = nc.dram_tensor("out", [T, D], F32, kind="ExternalOutput").ap()

    QaT = dscr("QaT", [8, 128, T], BF16)
    KaT = dscr("KaT", [8, 128, T], BF16)
    Va = dscr("Va", [T, 1024], BF16)
    RqT = dscr("RqT", [8, 128, T], BF16)
    RkT = dscr("RkT", [8, 128, T], BF16)
    Rv = dscr("Rv", [T, 2048], BF16)
    RgT = dscr("RgT", [16, 128, T], BF16)
    MqT = dscr("MqT", [8, 128, T], BF16)
    GT = dscr("GT", [48, 128, T], BF16)
    YT = dscr("YT", [32, 128, T], BF16)
    MT = dscr("MT", [16, 128, T], BF16)
    X1 = dscr("X1", [T, D], F32)
    X1T = dscr("X1T", [16, 128, T], BF16)
    X2 = dscr("X2", [T, D], F32)
    X2T = dscr("X2T", [16, 128, T], BF16)
    X1B = dscr("X1B", [T, D], BF16)
    Xs = dscr("Xs", [N_BLK * BLK_ROWS, D], BF16)
    Ys = dscr("Ys", [N_BLK * BLK_ROWS, D], F32)

    if debug:
        DBG_dest = nc.dram_tensor("DBG_dest", [128, 32], I32, kind="ExternalOutput").ap()
        DBG_blk = nc.dram_tensor("DBG_blk", [128, N_BLK], F32, kind="ExternalOutput").ap()
        DBG_wab = nc.dram_tensor("DBG_wab", [128, 32], F32, kind="ExternalOutput").ap()
    with ExitStack() as st:
        S = Sched(nc, st)
        ARENA_BYTES = 198 * 1024
        arena_t = st.enter_context(nc.sbuf_tensor("arena", [128, ARENA_BYTES // 2], BF16))
        ar = Arena(arena_t, ARENA_BYTES)
        ident = Buf(st.enter_context(nc.sbuf_tensor("ident", [128, 128], BF16)))
        identf = Buf(st.enter_context(nc.sbuf_tensor("identf", [128, 128], F32)))
        ones = Buf(st.enter_context(nc.sbuf_tensor("ones", [128, 128], BF16)))
        def pers(name, shape, dt):
            return Buf(st.enter_context(nc.sbuf_tensor(name, shape, dt)))
        ustrict = pers("ustrict", [128, 128], BF16)
        selS_all = pers("selS_all", [128, 16, 32], BF16)
        selA_all = pers("selA_all", [128, 16, 32], BF16)
        wab_all = pers("wab_all", [128, 16, 2], F32)
        dest_i = pers("dest_i", [128, 16, 2], I32)
        blkE = pers("blkE", [128, N_BLK], F32)
        blkU = pers("blkU", [128, N_BLK], F32)
        ps = []
        for i in range(8):
            b = Buf(st.enter_context(nc.psum_tensor("ps%d" % i, [128, 512], F32)))
            b.bf = b.t[:, :].bitcast(BF16)
            ps.append(b)
        psn = [0]

        psm = [0]

        def nextps_m():
            p = ps[3 + (psm[0] % 3)]
            psm[0] += 1
            return p

        def nextps():
            p = ps[psn[0] % 8]
            psn[0] += 1
            return p

        S.dma("sp", lambda e: e.dma_start(out=ident[:, :], in_=cin["c_ident"][:, :]), writes=[ident])
        S.dma("sp", lambda e: e.dma_start(out=identf[:, :], in_=cin["c_identf"][:, :]), writes=[identf])
        S.dma("sp", lambda e: e.dma_start(out=ones[:, :], in_=cin["c_ones"][:, :]), writes=[ones])
        S.dma("sp", lambda e: e.dma_start(out=ustrict[:, :], in_=cin["c_ustrict"][:, :]), writes=[ustrict])

        def sl(i, n):
            return slice(i * n, (i + 1) * n)

        def mm(pt, o, l, r, start, stop, reads):
            S.op("pe", lambda e: e.matmul(o, l, r, start=start, stop=stop), reads=reads, writes=[pt])

        def load_w512(wbuf, wsrc2d, col0, nk=16):
            src = wsrc2d[:, col0:col0 + 512].rearrange("(kc p) f -> p kc f", p=128)
            S.dma("pool", lambda e: e.dma_start(out=wbuf[:, 0:nk, :], in_=src), writes=[wbuf])

        def load_actT(actT, L):
            if L == 0:
                src = xT_in.rearrange("(kc p) t -> p kc t", p=128)
                for kc in range(16):
                    S.dma("pool", lambda e, kc=kc: e.dma_start(out=actT[:, kc, :], in_=src[:, kc, :]), writes=[actT])
            else:
                S.dma("sp", lambda e: e.dma_start(out=actT[:, :, :], in_=X2T.rearrange("c p t -> p c t")), writes=[actT])

        def phase_A(L):
            S.barrier()
            ar.reset()
            actT = ar.alloc([16, T], BF16)
            load_actT(actT, L)
            rot = ar.alloc([4, T], F32)
            S.dma("sp", lambda e: e.dma_start(out=rot[:, :, :], in_=cin["c_rot"].rearrange("c p t -> p c t")), writes=[rot])
            wb = [ar.alloc([16, 512], BF16) for _ in range(2)]
            ot = [ar.alloc([T], BF16) for _ in range(4)]
            tm = [ar.alloc([512], BF16) for _ in range(3)]
            tmp = [ar.alloc([512], F32) for _ in range(4)]
            W = w_in[L]
            cnt = {"wb": 0, "ot": 0, "tm": 0}

            def proj_chunk(wbuf, c, tg):
                p = nextps()
                for kc in range(16):
                    mm(p, p[:, :], wbuf[:, kc, sl(c, 128)], actT[:, kc, sl(tg, 512)], kc == 0, kc == 15, [wbuf, actT])
                return p

            def tform(col0, ncols, kind, dest, dchunk0):
                for g in range(ncols // 512):
                    wbuf = wb[cnt["wb"] % 2]
                    cnt["wb"] += 1
                    load_w512(wbuf, W, col0 + g * 512)
                    if kind == "rot" or kind == "rotk":
                        ci, si = (0, 1) if kind == "rot" else (2, 3)
                        for pr in range(2):
                            o1 = ot[cnt["ot"] % 4]
                            o2 = ot[(cnt["ot"] + 1) % 4]
                            cnt["ot"] += 2
                            for tg in range(4):
                                p1 = proj_chunk(wbuf, 2 * pr, tg)
                                p2 = proj_chunk(wbuf, 2 * pr + 1, tg)
                                ts = sl(tg, 512)
                                S.op("dve", lambda e, p1=p1, ts=ts: e.tensor_tensor(tmp[0][:, :], p1[:, :], rot[:, ci, ts], ALU.mult), reads=[p1, rot], writes=[tmp[0]])
                                S.op("dve", lambda e, p2=p2, ts=ts: e.tensor_tensor(tmp[1][:, :], p2[:, :], rot[:, si, ts], ALU.mult), reads=[p2, rot], writes=[tmp[1]])
                                S.op("dve", lambda e, p1=p1, ts=ts: e.tensor_tensor(tmp[2][:, :], p1[:, :], rot[:, si, ts], ALU.mult), reads=[p1, rot], writes=[tmp[2]])
                                S.op("dve", lambda e, p2=p2, ts=ts: e.tensor_tensor(tmp[3][:, :], p2[:, :], rot[:, ci, ts], ALU.mult), reads=[p2, rot], writes=[tmp[3]])
                                S.op("pool", lambda e, o1=o1, ts=ts: e.tensor_tensor(o1[:, ts], tmp[0][:, :], tmp[1][:, :], ALU.subtract), reads=[tmp[0], tmp[1]], writes=[o1])
                                S.op("pool", lambda e, o2=o2, ts=ts: e.tensor_tensor(o2[:, ts], tmp[2][:, :], tmp[3][:, :], ALU.add), reads=[tmp[2], tmp[3]], writes=[o2])
                            ch = dchunk0 + g * 4 + pr * 2
                            S.dma("sp", lambda e, o1=o1, ch=ch: e.dma_start(out=dest[ch, :, :], in_=o1[:, :]), reads=[o1])
                            S.dma("sp", lambda e, o2=o2, ch=ch: e.dma_start(out=dest[ch + 1, :, :], in_=o2[:, :]), reads=[o2])
                    else:
                        for c in range(4):
                            o = ot[cnt["ot"] % 4]
                            cnt["ot"] += 1
                            for tg in range(4):
                                p = proj_chunk(wbuf, c, tg)
                                ts = sl(tg, 512)
                                if kind == "copy":
                                    if tg % 2 == 0:
                                        S.op("act", lambda e, p=p, o=o, ts=ts: e.copy(o[:, ts], p[:, :]), reads=[p], writes=[o])
                                    else:
                                        S.op("dve", lambda e, p=p, o=o, ts=ts: e.tensor_copy(o[:, ts], p[:, :]), reads=[p], writes=[o])
                                else:
                                    fn = AF.Silu if kind == "silu" else AF.Sigmoid
                                    S.op("act", lambda e, p=p, o=o, ts=ts, fn=fn: e.activation(o[:, ts], p[:, :], fn), reads=[p], writes=[o])
                            ch = dchunk0 + g * 4 + c
                            S.dma("sp", lambda e, o=o, ch=ch: e.dma_start(out=dest[ch, :, :], in_=o[:, :]), reads=[o])

            def tokmaj(col0, ncols, dest):
                for g in range(ncols // 512):
                    wbuf = wb[cnt["wb"] % 2]
                    cnt["wb"] += 1
                    load_w512(wbuf, W, col0 + g * 512)
                    for tt in range(16):
                        p = nextps()
                        for kc in range(16):
                            mm(p, p[:, :], actT[:, kc, sl(tt, 128)], wbuf[:, kc, :], kc == 0, kc == 15, [wbuf, actT])
                        o = tm[cnt["tm"] % 3]
                        cnt["tm"] += 1
                        if tt % 2 == 0:
                            S.op("act", lambda e, p=p, o=o: e.copy(o[:, :], p[:, :]), reads=[p], writes=[o])
                        else:
                            S.op("dve", lambda e, p=p, o=o: e.tensor_copy(o[:, :], p[:, :]), reads=[p], writes=[o])
                        S.dma("sp", lambda e, o=o, tt=tt, g=g: e.dma_start(out=dest[sl(tt, 128), sl(g, 512)], in_=o[:, :]), reads=[o])

            tform(0, 1024, "copy", QaT, 0)
            tform(1024, 1024, "copy", KaT, 0)
            tokmaj(2048, 1024, Va)
            tform(3072, 1024, "rot", RqT, 0)
            tform(4096, 1024, "rotk", RkT, 0)
            tokmaj(5120, 2048, Rv)
            tform(9216, 1024, "copy", MqT, 0)
            tform(7168, 2048, "silu", RgT, 0)
            tform(10240, 6144, "sigmoid", GT, 0)

        def phase_B(L):
            S.barrier()
            ar.reset()
            NB3 = 3
            QT = [ar.alloc([T], BF16) for _ in range(NB3)]
            KT = [ar.alloc([T], BF16) for _ in range(NB3)]
            V = [ar.alloc([16, 128], BF16) for _ in range(NB3)]
            mob = ar.alloc([3, 128], F32)
            e8 = ar.alloc([1024], BF16)
            cm = ar.alloc([4, 512], BF16)
            S.dma("sp", lambda e: e.dma_start(out=mob[:, :, :], in_=cin["c_moba"].rearrange("c p t -> p c t")), writes=[mob])
            S.dma("sp", lambda e: e.dma_start(out=e8[:, :], in_=cin["c_e8"][:, :]), writes=[e8])
            S.dma("sp", lambda e: e.dma_start(out=cm[:, :, :], in_=cin["c_cm"][:, :, :]), writes=[cm])
            km = [ar.alloc([8], F32) for _ in range(2)]
            kmb = [ar.alloc([8], BF16) for _ in range(2)]
            gm = ar.alloc([16, 8], F32)
            mx = ar.alloc([16, 8], F32)
            sel = ar.alloc([16, 8], F32)
            mbb = ar.alloc([128], BF16)
            MbT = [ar.alloc([T], BF16) for _ in range(2)]
            for m_ in MbT:
                S.op("pool", lambda e, m_=m_: e.memset(m_[:, :], 0.0), writes=[m_])
            PT = [ar.alloc([512], BF16) for _ in range(3)]
            rden = [ar.alloc([512], F32) for _ in range(2)]
            yo = [ar.alloc([T], BF16) for _ in range(2)]
            Vsrc = Va.rearrange("(tt p) f -> p tt f", p=128)

            def load(h):
                b = h % NB3
                S.dma("sp", lambda e: e.dma_start(out=QT[b][:, :], in_=QaT[h, :, :]), writes=[QT[b]])
                S.dma("sp", lambda e: e.dma_start(out=KT[b][:, :], in_=KaT[h, :, :]), writes=[KT[b]])
                S.dma("sp", lambda e: e.dma_start(out=V[b][:, :, :], in_=Vsrc[:, :, sl(h, 128)]), writes=[V[b]])

            def setup_a(h):
                k = KT[h % NB3]
                S.op("dve", lambda e: e.tensor_reduce(km[h % 2][:, :], k[:, :].rearrange("p (n j) -> p n j", n=8), AX.X, ALU.add), reads=[k], writes=[km[h % 2]])
                S.op("act", lambda e: e.mul(kmb[h % 2][:, :], km[h % 2][:, :], 1.0 / 256.0), reads=[km[h % 2]], writes=[kmb[h % 2]])

            def setup_b(h):
                q = QT[h % NB3]
                gp = ps[6]
                for qt in range(16):
                    mm(gp, gp[:, sl(qt, 8)], q[:, sl(qt, 128)], kmb[h % 2][:, :], True, True, [q, kmb[h % 2]])
                S.op("dve", lambda e: e.tensor_tensor(gm[:, :, :], gp[:, 0:128].rearrange("p (a b) -> p a b", a=16), mob[:, 0, :].rearrange("p (a b) -> p a b", a=16), ALU.add), reads=[gp, mob], writes=[gm])
                for qt in range(16):
                    S.op("dve", lambda e, qt=qt: e.max(mx[:, qt, :], gm[:, qt, :]), reads=[gm], writes=[mx])
                for qt in range(16):
                    S.op("dve", lambda e, qt=qt: e.tensor_scalar(sel[:, qt, :], gm[:, qt, :], mx[:, qt, 2:3], None, ALU.is_ge), reads=[gm, mx], writes=[sel])
                selv = sel[:, :, :].rearrange("p a b -> p (a b)")
                S.op("dve", lambda e: e.tensor_tensor(selv, selv, mob[:, 1, :], ALU.mult), reads=[sel, mob], writes=[sel])
                S.op("dve", lambda e: e.tensor_tensor(selv, selv, mob[:, 2, :], ALU.add), reads=[sel, mob], writes=[sel])
                S.op("dve", lambda e: e.tensor_scalar(mbb[:, :], selv, -1.0, -NEG, ALU.add, ALU.mult), reads=[sel], writes=[mbb])

            def setup_c(h):
                M = MbT[h % 2]
                for half in range(2):
                    tp = ps[6 + half]
                    for j in range(8):
                        qt = half * 8 + j
                        S.op("pe", lambda e, tp=tp, j=j, qt=qt: e.transpose(tp.bf[0:8, sl(j, 128)], mbb[:, sl(qt, 8)], ident[:, :]), reads=[mbb, ident], writes=[tp])
                    S.op("act", lambda e, tp=tp, half=half: e.copy(M[0:8, sl(half, 1024)], tp.bf[0:8, 0:1024]), reads=[tp], writes=[M])

            cnt = {"it": 0, "npt": 0}

            def main(h, mid):
                q, k, v, M, y = QT[h % NB3], KT[h % NB3], V[h % NB3], MbT[h % 2], yo[h % 2]
                items = [(g, kt) for g in range(4) for kt in range(4 * g + 4)]
                banks = {}
                for g in range(4):
                    banks[g] = (ps[2 + cnt["it"] % 2], ps[4 + cnt["it"] % 2])
                    cnt["it"] += 1
                sp_pt = {}

                def emit_S(i):
                    g, kt = items[i]
                    Sp = ps[cnt["npt"] % 2]
                    pt = PT[cnt["npt"] % 3]
                    cnt["npt"] += 1
                    sp_pt[i] = pt
                    diag = kt >= 4 * g
                    mm(Sp, Sp[:, :], k[:, sl(kt, 128)], q[:, sl(g, 512)], True, False, [k, q])
                    mm(Sp, Sp[:, :], e8[:, sl(kt // 2, 128)], M[:, sl(g, 512)], False, not diag, [e8, M])
                    if diag:
                        mm(Sp, Sp[:, :], ident[:, :], cm[:, kt - 4 * g, :], False, True, [ident, cm])
                    S.op("act", lambda e: e.activation(pt[:, :], Sp[:, :], AF.Exp, scale=128.0 ** -0.5), reads=[Sp], writes=[pt])

                def emit_PV(i):
                    g, kt = items[i]
                    Op, Dp = banks[g]
                    pt = sp_pt[i]
                    nkt = 4 * g + 4
                    mm(Op, Op[:, :], v[:, kt, :], pt[:, :], kt == 0, kt == nkt - 1, [v, pt])
                    mm(Dp, Dp[:, :], ones[:, :], pt[:, :], kt == 0, kt == nkt - 1, [ones, pt])
                    if kt == nkt - 1:
                        rd = rden[g % 2]
                        S.op("dve", lambda e: e.reciprocal(rd[:, :], Dp[:, :]), reads=[Dp], writes=[rd])
                        S.op("dve", lambda e: e.tensor_tensor(y[:, sl(g, 512)], Op[:, :], rd[:, :], ALU.mult), reads=[Op, rd], writes=[y])
                        if g == 0 and mid is not None:
                            mid()

                emit_S(0)
                for i in range(len(items)):
                    if i + 1 < len(items):
                        emit_S(i + 1)
                    emit_PV(i)
                S.dma("sp", lambda e: e.dma_start(out=YT[h, :, :], in_=y[:, :]), reads=[y])

            load(0)
            load(1)
            setup_a(0)
            setup_b(0)
            setup_c(0)
            setup_a(1)
            for h in range(8):
                if h + 2 < 8:
                    load(h + 2)
                nxt = (lambda h=h: setup_b(h + 1)) if h + 1 < 8 else None
                main(h, nxt)
                if h + 1 < 8:
                    setup_c(h + 1)
                if h + 2 < 8:
                    setup_a(h + 2)

        def phase_C(L):
            S.barrier()
            ar.reset()
            Rq = [ar.alloc([2, T], BF16) for _ in range(2)]
            Rk = [ar.alloc([2, T], BF16) for _ in range(2)]
            Rvt = [ar.alloc([16, 512], BF16) for _ in range(2)]
            Rg = [ar.alloc([4, T], BF16) for _ in range(2)]
            xi = [ar.alloc([T], F32) for _ in range(2)]
            decT = ar.alloc([4, 128], F32)
            zeta = ar.alloc([4], F32)
            S.dma("sp", lambda e: e.dma_start(out=decT[:, :, :], in_=cin["c_decT"][:, :, :]), writes=[decT])
            S.dma("sp", lambda e: e.dma_start(out=zeta[:, :], in_=cin["c_zeta"][:, :]), writes=[zeta])
            Qxi = ar.alloc([2, T], BF16)
            Kz = ar.alloc([16, 256], BF16)
            state = ar.alloc([2, 512], F32)
            stateb = ar.alloc([2, 512], BF16)
            onb = [ar.alloc([512], BF16) for _ in range(2)]
            STb = [ar.alloc([128], BF16) for _ in range(2)]
            st6 = ar.alloc([6], F32)
            mv = ar.alloc([2], F32)
            lnv = ar.alloc([1], F32)
            rstd = ar.alloc([1], F32)
            nmr = ar.alloc([1], F32)
            yr = [ar.alloc([4, T], BF16) for _ in range(2)]
            Rvsrc = Rv.rearrange("(tt p) f -> p tt f", p=128)

            def load(h):
                b = h % 2
                S.dma("sp", lambda e: e.dma_start(out=Rq[b][:, :, :], in_=RqT[2 * h:2 * h + 2].rearrange("c p t -> p c t")), writes=[Rq[b]])
                S.dma("sp", lambda e: e.dma_start(out=Rk[b][:, :, :], in_=RkT[2 * h:2 * h + 2].rearrange("c p t -> p c t")), writes=[Rk[b]])
                S.dma("sp", lambda e: e.dma_start(out=Rvt[b][:, :, :], in_=Rvsrc[:, :, sl(h, 512)]), writes=[Rvt[b]])
                S.dma("sp", lambda e: e.dma_start(out=Rg[b][:, :, :], in_=RgT[4 * h:4 * h + 4].rearrange("c p t -> p c t")), writes=[Rg[b]])
                S.dma("sp", lambda e: e.dma_start(out=xi[b][:, :], in_=cin["c_xi"][h, :, :]), writes=[xi[b]])

            load(0)
            for h in range(4):
                if h + 1 < 4:
                    load(h + 1)
                b = h % 2
                rq, rk, rv, rg, x_i, y = Rq[b], Rk[b], Rvt[b], Rg[b], xi[b], yr[b]
                for dc in range(2):
                    S.op("dve", lambda e, dc=dc: e.tensor_tensor(Qxi[:, dc, :], rq[:, dc, :], x_i[:, :], ALU.mult), reads=[rq, x_i], writes=[Qxi])
                for n in range(16):
                    tp = ps[5 + n % 2]
                    for dc in range(2):
                        S.op("pe", lambda e, tp=tp, dc=dc, n=n: e.transpose(tp.bf[:, sl(dc, 128)], rk[:, dc, sl(n, 128)], ident[:, :]), reads=[rk, ident], writes=[tp])
                    S.op("act", lambda e, tp=tp, n=n: e.activation(Kz[:, n, :], tp.bf[:, 0:256], AF.Identity, scale=zeta[:, h:h + 1]), reads=[tp, zeta], writes=[Kz])
                def emit_ST(n):
                    ns = sl(n, 128)
                    STp = ps[0]
                    for dc in range(2):
                        mm(STp, STp[:, 0:128], rk[:, dc, ns], rq[:, dc, ns], dc == 0, dc == 1, [rk, rq])
                    sb = STb[n % 2]
                    S.op("dve", lambda e: e.tensor_tensor(sb[:, :], STp[:, 0:128], decT[:, h, :], ALU.mult), reads=[STp, decT], writes=[sb])

                def emit_O(n):
                    ns = sl(n, 128)
                    sb = STb[n % 2]
                    Op = ps[1 + n % 2]
                    mm(Op, Op[:, :], sb[:, :], rv[:, n, :], True, n == 0, [sb, rv])
                    if n > 0:
                        for dc in range(2):
                            mm(Op, Op[:, :], Qxi[:, dc, ns], stateb[:, dc, :], False, dc == 1, [Qxi, stateb])
                    if n < 15:
                        for dc in range(2):
                            kv = ps[3 + dc]
                            mm(kv, kv[:, :], Kz[:, n, sl(dc, 128)], rv[:, n, :], True, True, [Kz, rv])
                            if n == 0:
                                S.op("dve", lambda e, kv=kv, dc=dc: e.tensor_copy(state[:, dc, :], kv[:, :]), reads=[kv], writes=[state])
                            else:
                                S.op("dve", lambda e, kv=kv, dc=dc: e.scalar_tensor_tensor(state[:, dc, :], state[:, dc, :], CD[h], kv[:, :], ALU.mult, ALU.add), reads=[kv, state], writes=[state])
                        S.op("act", lambda e: e.copy(stateb[:, :, :], state[:, :, :]), reads=[state], writes=[stateb])
                    S.op("dve", lambda e: e.bn_stats(st6[:, :], Op[:, :]), reads=[Op], writes=[st6])
                    S.op("dve", lambda e: e.bn_aggr(mv[:, :], st6[:, :]), reads=[st6], writes=[mv])
                    S.op("act", lambda e: e.activation(lnv[:, :], mv[:, 1:2], AF.Ln, bias=GN_EPS), reads=[mv], writes=[lnv])
                    S.op("act", lambda e: e.activation(rstd[:, :], lnv[:, :], AF.Exp, scale=-0.5), reads=[lnv], writes=[rstd])
                    S.op("dve", lambda e: e.scalar_tensor_tensor(nmr[:, :], mv[:, 0:1], -1.0, rstd[:, :], ALU.mult, ALU.mult), reads=[mv, rstd], writes=[nmr])
                    ob = onb[n % 2]
                    S.op("act", lambda e: e.activation(ob[:, :], Op[:, :], AF.Identity, bias=nmr[:, 0:1], scale=rstd[:, 0:1]), reads=[Op, nmr, rstd], writes=[ob])

                def emit_T(n):
                    ns = sl(n, 128)
                    ob = onb[n % 2]
                    tp = ps[7]
                    for ec in range(4):
                        S.op("pe", lambda e, ec=ec: e.transpose(tp.bf[:, sl(ec, 128)], ob[:, sl(ec, 128)], ident[:, :]), reads=[ob, ident], writes=[tp])
                    S.op("dve", lambda e: e.tensor_tensor(y[:, :, ns], tp.bf[:, 0:512].rearrange("p (a b) -> p a b", a=4), rg[:, :, ns], ALU.mult), reads=[tp, rg], writes=[y])

                emit_ST(0)
                for n in range(16):
                    if n + 1 < 16:
                        emit_ST(n + 1)
                    emit_O(n)
                    if n > 0:
                        emit_T(n - 1)
                emit_T(15)
                S.dma("sp", lambda e, y=y, h=h: e.dma_start(out=YT[8 + 4 * h:12 + 4 * h].rearrange("c p t -> p c t"), in_=y[:, :, :]), reads=[y])

        def phase_D(L):
            S.barrier()
            ar.reset()
            memT = ar.alloc([16, 256], BF16)
            S.dma("pool", lambda e: e.dma_start(out=memT[:, :, :], in_=memT_in.rearrange("(kc p) m -> p kc m", p=128)), writes=[memT])
            wb = [ar.alloc([16, 512], BF16) for _ in range(2)]
            kmT = ar.alloc([8, 256], BF16)
            vm = ar.alloc([2, 1024], BF16)
            Mq = [ar.alloc([2, T], BF16) for _ in range(2)]
            PT = [ar.alloc([512], BF16) for _ in range(4)]
            rden = [ar.alloc([512], F32) for _ in range(2)]
            ym = [ar.alloc([2, T], BF16) for _ in range(2)]
            W = w_mem_kv[L]
            for g in range(4):
                wbuf = wb[g % 2]
                load_w512(wbuf, W, g * 512)
                if g < 2:
                    for c in range(4):
                        p = nextps()
                        for kc in range(16):
                            mm(p, p[:, 0:256], wbuf[:, kc, sl(c, 128)], memT[:, kc, :], kc == 0, kc == 15, [wbuf, memT])
                        S.op("act", lambda e, p=p, g=g, c=c: e.copy(kmT[:, g * 4 + c, :], p[:, 0:256]), reads=[p], writes=[kmT])
                else:
                    for mt in range(2):
                        p = nextps()
                        for kc in range(16):
                            mm(p, p[:, :], memT[:, kc, sl(mt, 128)], wbuf[:, kc, :], kc == 0, kc == 15, [wbuf, memT])
                        S.op("dve", lambda e, p=p, g=g, mt=mt: e.tensor_copy(vm[:, mt, sl(g - 2, 512)], p[:, :]), reads=[p], writes=[vm])

            def load(h):
                S.dma("sp", lambda e: e.dma_start(out=Mq[h % 2][:, :, :], in_=MqT[2 * h:2 * h + 2].rearrange("c p t -> p c t")), writes=[Mq[h % 2]])

            load(0)
            npt = 0
            for h in range(4):
                if h + 1 < 4:
                    load(h + 1)
                mq, y = Mq[h % 2], ym[h % 2]
                for g in range(4):
                    pts = []
                    for mt in range(2):
                        Sp = ps[mt]
                        for dc in range(2):
                            mm(Sp, Sp[:, :], kmT[:, 2 * h + dc, sl(mt, 128)], mq[:, dc, sl(g, 512)], dc == 0, dc == 1, [kmT, mq])
                        pt = PT[npt % 4]
                        npt += 1
                        S.op("act", lambda e, Sp=Sp, pt=pt: e.activation(pt[:, :], Sp[:, :], AF.Exp, scale=1.0 / 16.0), reads=[Sp], writes=[pt])
                        pts.append(pt)
                    Dp = ps[4 + g % 2]
                    for mt in range(2):
                        mm(Dp, Dp[:, :], ones[:, :], pts[mt][:, :], mt == 0, mt == 1, [ones, pts[mt]])
                    rd = rden[g % 2]
                    S.op("dve", lambda e, rd=rd, Dp=Dp: e.reciprocal(rd[:, :], Dp[:, :]), reads=[Dp], writes=[rd])
                    for dc in range(2):
                        Op = ps[2 + dc]
                        for mt in range(2):
                            mm(Op, Op[:, :], vm[:, mt, sl(2 * h + dc, 128)], pts[mt][:, :], mt == 0, mt == 1, [vm, pts[mt]])
                        S.op("dve", lambda e, rd=rd, Op=Op, y=y, g=g, dc=dc: e.tensor_tensor(y[:, dc, sl(g, 512)], Op[:, :], rd[:, :], ALU.mult), reads=[Op, rd], writes=[y])
                S.dma("sp", lambda e, y=y, h=h: e.dma_start(out=YT[24 + 2 * h:26 + 2 * h].rearrange("c p t -> p c t"), in_=y[:, :, :]), reads=[y])

        def phase_E(L):
            S.barrier()
            ar.reset()
            yT = ar.alloc([32, T], BF16)
            for c0 in range(0, 32, 8):
                S.dma("sp", lambda e, c0=c0: e.dma_start(out=yT[:, c0:c0 + 8, :], in_=YT[c0:c0 + 8].rearrange("c p t -> p c t")), writes=[yT])
            wbE = [ar.alloc([32, 256], BF16) for _ in range(2)]
            sg = [ar.alloc([3, 2, 512], BF16) for _ in range(2)]
            tmp = [ar.alloc([512], F32) for _ in range(6)]
            mo = [ar.alloc([2, T], BF16) for _ in range(1)]
            it = 0

            def loadw(fg):
                wbuf = wbE[fg % 2]
                cs = slice(fg * 256, fg * 256 + 256)
                S.dma("pool", lambda e: e.dma_start(out=wbuf[:, 0:8, :], in_=p_moba[L][:, cs].rearrange("(kc p) f -> p kc f", p=128)), writes=[wbuf])
                S.dma("pool", lambda e: e.dma_start(out=wbuf[:, 8:24, :], in_=p_ret[L][:, cs].rearrange("(kc p) f -> p kc f", p=128)), writes=[wbuf])
                S.dma("pool", lambda e: e.dma_start(out=wbuf[:, 24:32, :], in_=p_mem[L][:, cs].rearrange("(kc p) f -> p kc f", p=128)), writes=[wbuf])

            loadw(0)
            for fg in range(8):
                if fg + 1 < 8:
                    loadw(fg + 1)
                wbuf = wbE[fg % 2]
                mout = mo[0]
                for tg in range(4):
                    ts = sl(tg, 512)
                    sgt = sg[it % 2]
                    it += 1
                    for br in range(3):
                        S.dma("sp", lambda e, br=br: e.dma_start(out=sgt[:, br, :, :], in_=GT[br * 16 + fg * 2:br * 16 + fg * 2 + 2, :, ts].rearrange("c p t -> p c t")), writes=[sgt])
                    for c in range(2):
                        zs = []
                        for (k0, k1) in ((0, 8), (8, 24), (24, 32)):
                            p = nextps()
                            for kc in range(k0, k1):
                                mm(p, p[:, :], wbuf[:, kc, sl(c, 128)], yT[:, kc, ts], kc == k0, kc == k1 - 1, [wbuf, yT])
                            zs.append(p)
                        t3 = tmp[(c % 2) * 3:(c % 2) * 3 + 3]
                        for br in range(3):
                            S.op("dve", lambda e, br=br: e.tensor_tensor(t3[br][:, :], zs[br][:, :], sgt[:, br, c, :], ALU.mult), reads=[zs[br], sgt], writes=[t3[br]])
                        S.op("pool", lambda e: e.tensor_tensor(t3[0][:, :], t3[0][:, :], t3[1][:, :], ALU.add), reads=[t3[1]], writes=[t3[0]])
                        S.op("pool", lambda e: e.tensor_tensor(mout[:, c, ts], t3[0][:, :], t3[2][:, :], ALU.add), reads=[t3[0], t3[2]], writes=[mout])
                S.dma("sp", lambda e: e.dma_start(out=MT[fg * 2:fg * 2 + 2].rearrange("c p t -> p c t"), in_=mout[:, :, :]), reads=[mout])

        def ln_tail(y, G, Bv, sm, x_dst, xT_dst, tt, x1b, xTt, router=None, part=None):
            if part != "q":
                ln_p(y, G, sm)
            if part != "p":
                ln_q(y, Bv, x_dst, xT_dst, tt, x1b, xTt, router)

        def ln_p(y, G, sm):
            st = sm["st"]
            for fs in range(4):
                S.op("dve", lambda e, fs=fs: e.bn_stats(st[:, fs, :], y[:, sl(fs, 512)]), reads=[y], writes=[st])
            S.op("dve", lambda e: e.bn_aggr(sm["mv"][:, :], st[:, :, :].rearrange("p a b -> p (a b)")), reads=[st], writes=[sm["mv"]])
            S.op("act", lambda e: e.activation(sm["lnv"][:, :], sm["mv"][:, 1:2], AF.Ln, bias=LN_EPS), reads=[sm["mv"]], writes=[sm["lnv"]])
            S.op("act", lambda e: e.activation(sm["rstd"][:, :], sm["lnv"][:, :], AF.Exp, scale=-0.5), reads=[sm["lnv"]], writes=[sm["rstd"]])
            S.op("dve", lambda e: e.scalar_tensor_tensor(sm["nmr"][:, :], sm["mv"][:, 0:1], -1.0, sm["rstd"][:, :], ALU.mult, ALU.mult), reads=[sm["mv"], sm["rstd"]], writes=[sm["nmr"]])
            S.op("act", lambda e: e.activation(y[:, :], y[:, :], AF.Identity, bias=sm["nmr"][:, 0:1], scale=sm["rstd"][:, 0:1]), reads=[sm["nmr"], sm["rstd"]], writes=[y])
            S.op("pool", lambda e: e.tensor_tensor(y[:, :], y[:, :], G[:, :], ALU.mult), reads=[G], writes=[y])

        def ln_q(y, Bv, x_dst, xT_dst, tt, x1b, xTt, router):
            S.op("dve", lambda e: e.tensor_tensor(y[:, :], y[:, :], Bv[:, :], ALU.add), reads=[Bv], writes=[y])
            S.dma("sp", lambda e: e.dma_start(out=x_dst[sl(tt, 128), :], in_=y[:, :]), reads=[y])
            if xT_dst is None:
                return
            S.op("act", lambda e: e.copy(x1b[:, :], y[:, :]), reads=[y], writes=[x1b])
            if router is not None:
                S.dma("sp", lambda e: e.dma_start(out=X1B[sl(tt, 128), :], in_=x1b[:, :]), reads=[x1b])
            for half in range(2):
                tp = ps[4 + half]
                for j in range(8):
                    kc = half * 8 + j
                    S.op("pe", lambda e, tp=tp, j=j, kc=kc: e.transpose(tp.bf[:, sl(j, 128)], x1b[:, sl(kc, 128)], ident[:, :]), reads=[x1b, ident], writes=[tp])
                if half == 0:
                    S.op("act", lambda e, tp=tp: e.copy(xTt[:, 0:8, :], tp.bf[:, :].rearrange("p (a b) -> p a b", a=8)), reads=[tp], writes=[xTt])
                else:
                    S.op("dve", lambda e, tp=tp: e.tensor_copy(xTt[:, 8:16, :], tp.bf[:, :].rearrange("p (a b) -> p a b", a=8)), reads=[tp], writes=[xTt])
            S.dma("sp", lambda e: e.dma_start(out=xT_dst[:, :, sl(tt, 128)].rearrange("c p t -> p c t"), in_=xTt[:, :, :]), reads=[xTt])
            if router is not None:
                router(xTt, tt)

        def ln_small():
            return {"st": ar.alloc([4, 6], F32), "mv": ar.alloc([2], F32), "lnv": ar.alloc([1], F32), "rstd": ar.alloc([1], F32), "nmr": ar.alloc([1], F32)}

        def phase_F(L):
            S.barrier()
            ar.reset()
            wo = [ar.alloc([16, 512], BF16) for _ in range(4)]
            for g in range(4):
                src = w_o[L][:, sl(g, 512)].rearrange("(kc p) f -> p kc f", p=128)
                S.dma("pool", lambda e, src=src, g=g: e.dma_start(out=wo[g][:, :, :], in_=src), writes=[wo[g]])
            G = ar.alloc([D], F32)
            Bv = ar.alloc([D], F32)
            S.dma("sp", lambda e: e.dma_start(out=G[:, :], in_=ln_par[L, 0, :, :]), writes=[G])
            S.dma("sp", lambda e: e.dma_start(out=Bv[:, :], in_=ln_par[L, 1, :, :]), writes=[Bv])
            wr = ar.alloc([16, 36], BF16)
            S.dma("pool", lambda e: e.dma_start(out=wr[:, :, :], in_=w_r[L].rearrange("(kc p) f -> p kc f", p=128)), writes=[wr])
            br = ar.alloc([36], F32)
            S.dma("sp", lambda e: e.dma_start(out=br[:, :], in_=b_r[L, :, :]), writes=[br])
            mT = [ar.alloc([16, 128], BF16) for _ in range(2)]
            xres = [ar.alloc([D], F32) for _ in range(2)]
            ys = [ar.alloc([D], F32) for _ in range(2)]
            x1b = [ar.alloc([D], BF16) for _ in range(2)]
            xTt = [ar.alloc([16, 128], BF16) for _ in range(2)]
            sms = [ln_small() for _ in range(2)]
            lg = ar.alloc([36], F32)
            r_ = {k: ar.alloc([n], F32) for k, n in (("gmax", 1), ("ngmax", 1), ("ge", 4), ("gsum", 1), ("gmask", 4), ("pen", 4),
                                                        ("em", 32), ("mx", 8), ("sel", 32), ("nv1", 1), ("ex", 32), ("e2", 1), ("den", 1), ("rr", 1), ("W", 32))}
            xsrc = x_in if L == 0 else X2

            def router(xt, tt):
                lp = ps[6]
                for kc in range(16):
                    mm(lp, lp[:, 0:36], xt[:, kc, :], wr[:, kc, :], kc == 0, kc == 15, [xt, wr])
                R = r_
                S.op("dve", lambda e: e.tensor_tensor(lg[:, :], lp[:, 0:36], br[:, :], ALU.add), reads=[lp, br], writes=[lg])
                S.op("dve", lambda e: e.tensor_reduce(R["gmax"][:, :], lg[:, 0:4], AX.X, ALU.max), reads=[lg], writes=[R["gmax"]])
                S.op("dve", lambda e: e.tensor_scalar(R["ngmax"][:, :], R["gmax"][:, :], -1.0, None, ALU.mult), reads=[R["gmax"]], writes=[R["ngmax"]])
                S.op("act", lambda e: e.activation(R["ge"][:, :], lg[:, 0:4], AF.Exp, bias=R["ngmax"][:, 0:1], accum_out=R["gsum"][:, 0:1]), reads=[lg, R["ngmax"]], writes=[R["ge"], R["gsum"]])
                S.op("dve", lambda e: e.tensor_scalar(R["gmask"][:, :], lg[:, 0:4], R["gmax"][:, 0:1], None, ALU.is_ge), reads=[lg, R["gmax"]], writes=[R["gmask"]])
                S.op("dve", lambda e: e.tensor_scalar(R["pen"][:, :], R["gmask"][:, :], -1.0, 1e30, ALU.add, ALU.mult), reads=[R["gmask"]], writes=[R["pen"]])
                for g in range(4):
                    S.op("dve", lambda e, g=g: e.tensor_scalar(R["em"][:, sl(g, 8)], lg[:, 4 + g * 8:12 + g * 8], R["pen"][:, g:g + 1], None, ALU.add), reads=[lg, R["pen"]], writes=[R["em"]])
                S.op("dve", lambda e: e.max(R["mx"][:, :], R["em"][:, :]), reads=[R["em"]], writes=[R["mx"]])
                S.op("dve", lambda e: e.tensor_scalar(R["sel"][:, :], R["em"][:, :], R["mx"][:, 1:2], None, ALU.is_ge), reads=[R["em"], R["mx"]], writes=[R["sel"]])
                S.op("dve", lambda e: e.tensor_scalar(R["nv1"][:, :], R["mx"][:, 0:1], -1.0, None, ALU.mult), reads=[R["mx"]], writes=[R["nv1"]])
                S.op("act", lambda e: e.activation(R["ex"][:, :], R["em"][:, :], AF.Exp, bias=R["nv1"][:, 0:1]), reads=[R["em"], R["nv1"]], writes=[R["ex"]])
                S.op("act", lambda e: e.activation(R["e2"][:, :], R["mx"][:, 1:2], AF.Exp, bias=R["nv1"][:, 0:1]), reads=[R["mx"], R["nv1"]], writes=[R["e2"]])
                S.op("dve", lambda e: e.scalar_tensor_tensor(R["den"][:, :], R["e2"][:, :], 1.0, R["gsum"][:, :], ALU.add, ALU.mult), reads=[R["e2"], R["gsum"]], writes=[R["den"]])
                S.op("dve", lambda e: e.reciprocal(R["rr"][:, :], R["den"][:, :]), reads=[R["den"]], writes=[R["rr"]])
                S.op("act", lambda e: e.copy(selS_all[:, tt, :], R["sel"][:, :]), reads=[R["sel"]], writes=[selS_all])
                S.op("dve", lambda e: e.tensor_scalar(selA_all[:, tt, :], R["em"][:, :], R["mx"][:, 0:1], None, ALU.is_ge), reads=[R["em"], R["mx"]], writes=[selA_all])
                S.op("act", lambda e: e.copy(wab_all[:, tt, 0:1], R["rr"][:, :]), reads=[R["rr"]], writes=[wab_all])
                S.op("dve", lambda e: e.tensor_tensor(wab_all[:, tt, 1:2], R["rr"][:, :], R["e2"][:, :], ALU.mult), reads=[R["rr"], R["e2"]], writes=[wab_all])

            def load(tt):
                b = tt % 2
                S.dma("sp", lambda e: e.dma_start(out=mT[b][:, :, :], in_=MT[:, :, sl(tt, 128)].rearrange("c p t -> p c t")), writes=[mT[b]])
                S.dma("sp", lambda e: e.dma_start(out=xres[b][:, :], in_=xsrc[sl(tt, 128), :]), writes=[xres[b]])

            def mm_pe(tt):
                b = tt % 2
                for fs in range(4):
                    p = ps[fs]
                    for kc in range(16):
                        mm(p, p[:, :], mT[b][:, kc, :], wo[fs][:, kc, :], kc == 0, kc == 15, [mT[b], wo[fs]])

            def mm_stt(tt):
                b = tt % 2
                y = ys[b]
                for fs in range(4):
                    p = ps[fs]
                    S.op("dve", lambda e, p=p, fs=fs: e.scalar_tensor_tensor(y[:, sl(fs, 512)], xres[b][:, sl(fs, 512)], ALPHA, p[:, :], ALU.mult, ALU.add), reads=[p, xres[b]], writes=[y])

            load(0)
            load(1)
            mm_pe(0)
            mm_stt(0)
            for tt in range(16):
                b = tt % 2
                if tt + 1 < 16:
                    mm_pe(tt + 1)
                ln_tail(ys[b], G, Bv, sms[b], X1, X1T, tt, x1b[b], xTt[b], router, part="p")
                if tt > 0:
                    pb = (tt - 1) % 2
                    ln_tail(ys[pb], G, Bv, sms[pb], X1, X1T, tt - 1, x1b[pb], xTt[pb], router, part="q")
                if tt + 1 < 16:
                    mm_stt(tt + 1)
                if tt + 2 < 16:
                    load(tt + 2)
            ln_tail(ys[1], G, Bv, sms[1], X1, X1T, 15, x1b[1], xTt[1], router, part="q")
            cnt = ar.alloc([32], F32)
            accs = [ar.alloc([32], F32) for _ in range(2)]
            cps = ps[6]
            for tt in range(16):
                mm(cps, cps[:, 0:32], ones[:, :], selS_all[:, tt, :], tt == 0, tt == 15, [ones, selS_all])
            S.op("dve", lambda e: e.tensor_copy(cnt[:, :], cps[:, 0:32]), reads=[cps], writes=[cnt])
            S.op("dve", lambda e: e.tensor_scalar(accs[0][:, :], cnt[:, :], 0.5, None, ALU.is_gt), reads=[cnt], writes=[accs[0]])
            NJ = T // BLK_ROWS
            for j in range(1, NJ):
                S.op("dve", lambda e, j=j: e.scalar_tensor_tensor(accs[j % 2][:, :], cnt[:, :], float(BLK_ROWS) * j + 0.5, accs[(j + 1) % 2][:, :], ALU.is_gt, ALU.add), reads=[cnt, accs[(j + 1) % 2]], writes=[accs[j % 2]])
            padded = ar.alloc([32], F32)
            S.op("dve", lambda e: e.tensor_scalar(padded[:, :], accs[(NJ - 1) % 2][:, :], float(BLK_ROWS), None, ALU.mult), reads=[accs[(NJ - 1) % 2]], writes=[padded])
            cs = [ar.alloc([32], F32) for _ in range(2)]
            S.op("dve", lambda e: e.tensor_copy(cs[0][:, :], padded[:, :]), reads=[padded], writes=[cs[0]])
            k = 0
            for dsh in (1, 2, 4, 8, 16):
                a_, b_ = cs[k % 2], cs[(k + 1) % 2]
                S.op("dve", lambda e, a_=a_, b_=b_, dsh=dsh: e.tensor_copy(b_[:, 0:dsh], a_[:, 0:dsh]), reads=[a_], writes=[b_])
                S.op("dve", lambda e, a_=a_, b_=b_, dsh=dsh: e.tensor_tensor(b_[:, dsh:32], a_[:, dsh:32], a_[:, 0:32 - dsh], ALU.add), reads=[a_], writes=[b_])
                k += 1
            pend = cs[k % 2]
            pstart = ar.alloc([32], F32)
            S.op("dve", lambda e: e.tensor_tensor(pstart[:, :], pend[:, :], padded[:, :], ALU.subtract), reads=[pend, padded], writes=[pstart])
            thr = ar.alloc([N_BLK], F32)
            S.dma("sp", lambda e: e.dma_start(out=thr[:, :], in_=cin["c_thr"][:, :]), writes=[thr])
            bacc = [ar.alloc([N_BLK], F32) for _ in range(2)]
            S.op("dve", lambda e: e.tensor_scalar(bacc[0][:, :], thr[:, :], pend[:, 0:1], None, ALU.is_ge), reads=[thr, pend], writes=[bacc[0]])
            for ex in range(1, 32):
                S.op("dve", lambda e, ex=ex: e.scalar_tensor_tensor(bacc[ex % 2][:, :], thr[:, :], pend[:, ex:ex + 1], bacc[(ex + 1) % 2][:, :], ALU.is_ge, ALU.add), reads=[thr, pend, bacc[(ex + 1) % 2]], writes=[bacc[ex % 2]])
            S.op("dve", lambda e: e.tensor_scalar(blkE[:, :], bacc[1][:, :], 31.0, None, ALU.min), reads=[bacc[1]], writes=[blkE])
            S.op("dve", lambda e: e.tensor_scalar(blkU[:, :], thr[:, :], pend[:, 31:32], OOB, ALU.is_ge, ALU.mult), reads=[thr, pend], writes=[blkU])
            dtmp = ar.alloc([32], F32)
            dprod = ar.alloc([32], F32)
            selB = ar.alloc([32], F32)
            dflt = ar.alloc([16, 2], F32)
            for tt in range(16):
                rp = ps[tt % 2]
                for t2 in range(tt):
                    mm(rp, rp[:, 0:32], ones[:, :], selS_all[:, t2, :], t2 == 0, False, [ones, selS_all])
                mm(rp, rp[:, 0:32], ustrict[:, :], selS_all[:, tt, :], tt == 0, True, [ustrict, selS_all])
                S.op("dve", lambda e, rp=rp: e.tensor_tensor(dtmp[:, :], rp[:, 0:32], pstart[:, :], ALU.add), reads=[rp, pstart], writes=[dtmp])
                S.op("dve", lambda e, tt=tt: e.tensor_tensor(dprod[:, :], dtmp[:, :], selA_all[:, tt, :], ALU.mult), reads=[dtmp, selA_all], writes=[dprod])
                S.op("dve", lambda e, tt=tt: e.tensor_reduce(dflt[:, tt, 0:1], dprod[:, :], AX.X, ALU.add), reads=[dprod], writes=[dflt])
                S.op("dve", lambda e, tt=tt: e.tensor_tensor(selB[:, :], selS_all[:, tt, :], selA_all[:, tt, :], ALU.subtract), reads=[selS_all, selA_all], writes=[selB])
                S.op("dve", lambda e: e.tensor_tensor(dprod[:, :], dtmp[:, :], selB[:, :], ALU.mult), reads=[dtmp, selB], writes=[dprod])
                S.op("dve", lambda e, tt=tt: e.tensor_reduce(dflt[:, tt, 1:2], dprod[:, :], AX.X, ALU.add), reads=[dprod], writes=[dflt])
            S.op("dve", lambda e: e.tensor_copy(dest_i[:, :, :], dflt[:, :, :]), reads=[dflt], writes=[dest_i])
            if debug:
                S.dma("sp", lambda e: e.dma_start(out=DBG_dest[:, :], in_=dest_i[:, :, :].rearrange("p a b -> p (a b)")), reads=[dest_i])
                S.dma("sp", lambda e: e.dma_start(out=DBG_blk[:, :], in_=blkE[:, :]), reads=[blkE])
                S.dma("sp", lambda e: e.dma_start(out=DBG_wab[:, :], in_=wab_all[:, :, :].rearrange("p a b -> p (a b)")), reads=[wab_all])

        def phase_X(L):
            S.barrier()
            ar.reset()
            xb = [ar.alloc([D], BF16) for _ in range(3)]
            for tt in range(16):
                b = xb[tt % 3]
                S.dma("sp", lambda e, b=b, tt=tt: e.dma_start(out=b[:, :], in_=X1B[sl(tt, 128), :]), writes=[b])
                for s_ in range(2):
                    S.dma("pool", lambda e, b=b, tt=tt, s_=s_: e.indirect_dma_start(
                        out=Xs[:, :], out_offset=bass.IndirectOffsetOnAxis(ap=dest_i[:, tt, s_:s_ + 1], axis=0),
                        in_=b[:, :], in_offset=None), reads=[b, dest_i])

        def phase_M(L):
            S.barrier()
            ar.reset()
            base = ar.alloc([12], F32)
            S.dma("sp", lambda e: e.dma_start(out=base[:, :], in_=cin["c_base"][:, :]), writes=[base])
            b1024 = ar.alloc([N_BLK], F32)
            b512 = ar.alloc([N_BLK], F32)
            S.op("dve", lambda e: e.tensor_scalar(b1024[:, :], blkE[:, :], 1024.0, float(L * N_EXP * 1024), ALU.mult, ALU.add), reads=[blkE], writes=[b1024])
            S.op("dve", lambda e: e.tensor_scalar(b512[:, :], blkE[:, :], 512.0, float(L * N_EXP * 512), ALU.mult, ALU.add), reads=[blkE], writes=[b512])
            S.op("dve", lambda e: e.tensor_tensor(b1024[:, :], b1024[:, :], blkU[:, :], ALU.add), reads=[blkU], writes=[b1024])
            S.op("dve", lambda e: e.tensor_tensor(b512[:, :], b512[:, :], blkU[:, :], ALU.add), reads=[blkU], writes=[b512])
            idf = ar.alloc([N_BLK, 12], F32)
            idi = ar.alloc([N_BLK, 12], I32)
            for j in range(12):
                srcb = b1024 if j < 8 else b512
                S.op("dve", lambda e, j=j, srcb=srcb: e.tensor_scalar(idf[:, :, j], srcb[:, :], base[:, j:j + 1], None, ALU.add), reads=[srcb, base], writes=[idf])
            S.op("dve", lambda e: e.tensor_copy(idi[:, :, :], idf[:, :, :]), reads=[idf], writes=[idi])
            NWB = 3
            wgu = [ar.alloc([8, 2048], BF16) for _ in range(NWB)]
            wdn = [ar.alloc([4, 2048], BF16) for _ in range(NWB)]
            xb = [ar.alloc([D], BF16) for _ in range(2)]
            xbT = [ar.alloc([16, 128], BF16) for _ in range(2)]
            sg = [ar.alloc([512], F32) for _ in range(2)]
            actb = [ar.alloc([512], BF16) for _ in range(2)]
            actT = [ar.alloc([4, 128], BF16) for _ in range(2)]
            ysb = [ar.alloc([D], F32) for _ in range(2)]
            RT = BLK_ROWS // 128
            order = []
            lo, hi = 0, N_BLK - 1
            while lo <= hi:
                for _ in range(2):
                    if lo <= hi:
                        order.append(lo)
                        lo += 1
                if lo <= hi:
                    order.append(hi)
                    hi -= 1
            assert sorted(order) == list(range(N_BLK))
            rows = [(bk * RT + rt, pos % NWB) for pos, bk in enumerate(order) for rt in range(RT)]
            NR = len(rows)

            def loadw(pos):
                bk = order[pos]
                i = pos % NWB
                for j in range(8):
                    S.dma("pool", lambda e, j=j: e.indirect_dma_start(out=wgu[i][:, j, :], out_offset=None, in_=w_gup[:, :],
                                                                     in_offset=bass.IndirectOffsetOnAxis(ap=idi[:, bk, j:j + 1], axis=0),
                                                                     bounds_check=RegConst(DEPTH * N_EXP * 128 * 8 - 1), oob_is_err=False), reads=[idi], writes=[wgu[i]])
                for j in range(4):
                    S.dma("pool", lambda e, j=j: e.indirect_dma_start(out=wdn[i][:, j, :], out_offset=None, in_=w_dnp[:, :],
                                                                     in_offset=bass.IndirectOffsetOnAxis(ap=idi[:, bk, 8 + j:9 + j], axis=0),
                                                                     bounds_check=RegConst(DEPTH * N_EXP * 128 * 4 - 1), oob_is_err=False), reads=[idi], writes=[wdn[i]])

            def loadx(n):
                S.dma("sp", lambda e: e.dma_start(out=xb[n % 2][:, :], in_=Xs[sl(rows[n][0], 128), :]), writes=[xb[n % 2]])

            def st1(n):
                x_, xt = xb[n % 2], xbT[n % 2]
                for half in range(2):
                    tp = ps[6 + half]
                    for j in range(8):
                        kc = half * 8 + j
                        S.op("pe", lambda e, tp=tp, j=j, kc=kc: e.transpose(tp.bf[:, sl(j, 128)], x_[:, sl(kc, 128)], ident[:, :]), reads=[x_, ident], writes=[tp])
                    if half == 0:
                        S.op("act", lambda e, tp=tp: e.copy(xt[:, 0:8, :], tp.bf[:, :].rearrange("p (a b) -> p a b", a=8)), reads=[tp], writes=[xt])
                    else:
                        S.op("dve", lambda e, tp=tp: e.tensor_copy(xt[:, 8:16, :], tp.bf[:, :].rearrange("p (a b) -> p a b", a=8)), reads=[tp], writes=[xt])

            def st2(n):
                i = rows[n][1]
                k = n % 2
                wg = wgu[i][:, :, :].rearrange("p a (b c) -> p (a b) c", b=2)
                xt = xbT[k]
                gp, up = ps[0], ps[1]
                for kc in range(16):
                    mm(gp, gp[:, :], xt[:, kc, :], wg[:, kc, 0:512], kc == 0, kc == 15, [xt, wgu[i]])
                for kc in range(16):
                    mm(up, up[:, :], xt[:, kc, :], wg[:, kc, 512:1024], kc == 0, kc == 15, [xt, wgu[i]])
                S.op("act", lambda e: e.activation(sg[k][:, :], gp[:, :], AF.Silu), reads=[gp], writes=[sg[k]])
                S.op("dve", lambda e: e.tensor_tensor(actb[k][:, :], up[:, :], sg[k][:, :], ALU.mult), reads=[up, sg[k]], writes=[actb[k]])

            def st3(n):
                k = n % 2
                tp = ps[2]
                for c in range(4):
                    S.op("pe", lambda e, c=c: e.transpose(tp.bf[:, sl(c, 128)], actb[k][:, sl(c, 128)], ident[:, :]), reads=[actb[k], ident], writes=[tp])
                S.op("act", lambda e: e.copy(actT[k][:, :, :], tp.bf[:, 0:512].rearrange("p (a b) -> p a b", a=4)), reads=[tp], writes=[actT[k]])

            def st4(n):
                r, i = rows[n]
                k = n % 2
                wd, at, yb = wdn[i], actT[k], ysb[k]
                for dg in range(4):
                    yp = nextps_m()
                    for c in range(4):
                        mm(yp, yp[:, :], at[:, c, :], wd[:, c, sl(dg, 512)], c == 0, c == 3, [at, wd])
                    if dg % 2 == 0:
                        S.op("act", lambda e, yp=yp, dg=dg: e.copy(yb[:, sl(dg, 512)], yp[:, :]), reads=[yp], writes=[yb])
                    else:
                        S.op("dve", lambda e, yp=yp, dg=dg: e.tensor_copy(yb[:, sl(dg, 512)], yp[:, :]), reads=[yp], writes=[yb])
                S.dma("sp", lambda e: e.dma_start(out=Ys[sl(r, 128), :], in_=yb[:, :]), reads=[yb])

            loadw(0)
            loadw(1)
            loadx(0)
            loadx(1)
            st1(0)
            for n in range(NR):
                st2(n)
                if n > 0:
                    st4(n - 1)
                if n % RT == 0 and n // RT + 2 < N_BLK:
                    loadw(n // RT + 2)
                if n + 1 < NR:
                    st1(n + 1)
                if n + 2 < NR:
                    loadx(n + 2)
                st3(n)
            st4(NR - 1)

        def phase_G(L, last):
            S.barrier()
            ar.reset()
            G = ar.alloc([D], F32)
            Bv = ar.alloc([D], F32)
            S.dma("sp", lambda e: e.dma_start(out=G[:, :], in_=ln_par[L, 2, :, :]), writes=[G])
            S.dma("sp", lambda e: e.dma_start(out=Bv[:, :], in_=ln_par[L, 3, :, :]), writes=[Bv])
            x1 = [ar.alloc([D], F32) for _ in range(2)]
            ff = [ar.alloc([D], F32) for _ in range(2)]
            fb = [ar.alloc([D], F32) for _ in range(2)]
            x1b = [ar.alloc([D], BF16) for _ in range(2)]
            xTt = [ar.alloc([16, 128], BF16) for _ in range(2)]
            sms = [ln_small() for _ in range(2)]

            def load(tt):
                b = tt % 2
                S.dma("sp", lambda e: e.dma_start(out=x1[b][:, :], in_=X1[sl(tt, 128), :]), writes=[x1[b]])
                S.dma("pool", lambda e: e.indirect_dma_start(out=ff[b][:, :], out_offset=None, in_=Ys[:, :], in_offset=bass.IndirectOffsetOnAxis(ap=dest_i[:, tt, 0:1], axis=0)), reads=[dest_i], writes=[ff[b]])
                S.dma("pool", lambda e: e.indirect_dma_start(out=fb[b][:, :], out_offset=None, in_=Ys[:, :], in_offset=bass.IndirectOffsetOnAxis(ap=dest_i[:, tt, 1:2], axis=0)), reads=[dest_i], writes=[fb[b]])

            dst_x = out if last else X2
            dst_xT = None if last else X2T

            def P(tt):
                b = tt % 2
                y = ff[b]
                S.op("act", lambda e: e.activation(y[:, :], y[:, :], AF.Identity, scale=wab_all[:, tt, 0:1]), reads=[wab_all], writes=[y])
                S.op("dve", lambda e: e.scalar_tensor_tensor(y[:, :], fb[b][:, :], wab_all[:, tt, 1:2], y[:, :], ALU.mult, ALU.add), reads=[fb[b], wab_all], writes=[y])
                S.op("dve", lambda e: e.scalar_tensor_tensor(y[:, :], x1[b][:, :], ALPHA, y[:, :], ALU.mult, ALU.add), reads=[x1[b]], writes=[y])
                ln_tail(y, G, Bv, sms[b], dst_x, dst_xT, tt, x1b[b], xTt[b], part="p")

            def Q(tt):
                b = tt % 2
                ln_tail(ff[b], G, Bv, sms[b], dst_x, dst_xT, tt, x1b[b], xTt[b], part="q")

            load(0)
            load(1)
            P(0)
            for tt in range(16):
                if tt + 1 < 16:
                    P(tt + 1)
                Q(tt)
                if tt + 2 < 16:
                    load(tt + 2)

        for L in range(n_layers):
            phase_A(L)
            phase_B(L)
            phase_C(L)
            phase_D(L)
            phase_E(L)
            phase_F(L)
            if no_indirect:
                continue
            phase_X(L)
            if stop_after == "X":
                continue
            phase_M(L)
            if stop_after == "M":
                continue
            phase_G(L, L == n_layers - 1)
        S.finish()
        S.emit()
    return nc, consts


_CACHE = {}


def _prep_shared(inp):
    f = lambda a: np.ascontiguousarray(np.asarray(a, dtype=np.float32))
    sh = {}
    for k in ["w_in", "p_moba", "p_ret", "p_mem", "w_mem_kv", "w_o"]:
        sh[k] = f(inp[k])
    g = f(inp["w_gate_up"]).reshape(DEPTH, N_EXP, 16, 128, 1024).transpose(0, 1, 3, 2, 4)
    sh["w_gup"] = np.ascontiguousarray(g).reshape(DEPTH * N_EXP * 128 * 8, 2048)
    dn = f(inp["w_down"]).reshape(DEPTH, N_EXP, 4, 128, D).transpose(0, 1, 3, 2, 4)
    sh["w_dnp"] = np.ascontiguousarray(dn).reshape(DEPTH * N_EXP * 128 * 4, 2048)
    lnp = np.stack([f(inp["ln1_g"]), f(inp["ln1_b"]), f(inp["ln2_g"]), f(inp["ln2_b"])], 1)
    sh["ln_par"] = np.ascontiguousarray(np.broadcast_to(lnp[:, :, None, :], (DEPTH, 4, 128, D)))
    sh["w_r"] = np.ascontiguousarray(np.concatenate([f(inp["w_group"]), f(inp["w_expert"])], -1))
    br = np.concatenate([f(inp["b_group"]), f(inp["b_expert"])], -1)
    sh["b_r"] = np.ascontiguousarray(np.broadcast_to(br[:, None, :], (DEPTH, 128, 36)))
    return sh


def kernel(**inputs):
    if "nc" not in _CACHE:
        _CACHE["nc"] = build()
    nc, consts = _CACHE["nc"]
    sh = _prep_shared(inputs)
    x = np.asarray(inputs["x"], dtype=np.float32)
    mem = np.asarray(inputs["mem"], dtype=np.float32)
    in_maps = []
    for b in range(8):
        m = dict(sh)
        m.update(consts)
        m["x"] = np.ascontiguousarray(x[b])
        m["xT"] = np.ascontiguousarray(x[b].T)
        m["memT"] = np.ascontiguousarray(mem[b].T)
        in_maps.append(m)
    res = run_bass_kernel_spmd(nc, in_maps, core_ids=list(range(8)))
    return np.stack([np.asarray(r["out"], dtype=np.float32) for r in res.results], 0)
```

```python
import math
import numpy as np
import ml_dtypes
import concourse.bass as bass
import concourse.mybir as mybir
from concourse.bass_utils import run_bass_kernel_spmd
from contextlib import ExitStack

F32 = mybir.dt.float32
BF16 = mybir.dt.bfloat16
I32 = mybir.dt.int32
AF = mybir.ActivationFunctionType
ALU = mybir.AluOpType
AX = mybir.AxisListType

T = 2048
D = 2048
DEPTH = 2
ALPHA = float((2 * DEPTH) ** 0.25)
LN_EPS = 1e-5
GN_EPS = 1e-6
NEG = -30000.0
N_EXP = 32
BLK_ROWS = 256
N_BLK = 48
OOB = float(2 ** 20)


class Buf:
    def __init__(self, ap, name=""):
        self.t = ap
        self.name = name
        self.w = []
        self.pr = []
        self.r = []
        self.bf = None

    def __getitem__(self, idx):
        return self.t[idx]


class _Rec:
    def __init__(self):
        self.call = None

    def __getattr__(self, name):
        def f(*a, **k):
            self.call = (name, a, k)
            return self
        return f


class RegConst:
    def __init__(self, v):
        self.v = v


_REG_CACHE = {}


def _resolve(e, k):
    out = {}
    for key, val in k.items():
        if isinstance(val, RegConst):
            ck = (id(e), val.v)
            if ck not in _REG_CACHE:
                _REG_CACHE[ck] = e.to_reg(val.v)
            val = _REG_CACHE[ck]
        out[key] = val
    return out


def _record(fn):
    r = _Rec()
    fn(r)
    return r.call


class EngState:
    def __init__(self, name):
        self.name = name
        self.cnt = 0
        self.ops = []
        self.seen = {}


class Sched:
    N_DMA_SEMS = 16

    def __init__(self, nc, stack):
        self.nc = nc
        self.sems = {}
        self.engs = {}
        for name in ["pe", "dve", "act", "pool", "sp"]:
            self.sems[name] = stack.enter_context(nc.semaphore("s_" + name))
            self.engs[name] = EngState(name)
        self.dma_sems = {}
        self.dma_next = {}
        self.dma_uses = {}
        for q in ["sp", "pool"]:
            lst = []
            for i in range(self.N_DMA_SEMS):
                key = "d_%s_%d" % (q, i)
                self.sems[key] = stack.enter_context(nc.semaphore(key))
                lst.append(key)
                self.dma_uses[key] = 0
            self.dma_sems[q] = lst
            self.dma_next[q] = 0

    def _wait(self, eng, deps):
        best = {}
        for d in deps:
            if d is None:
                continue
            k, v = d
            if best.get(k, 0) < v:
                best[k] = v
        for k, v in best.items():
            if k == eng.name and eng.name == "pe":
                continue
            if eng.seen.get(k, 0) >= v:
                continue
            eng.seen[k] = v
            sem = self.sems[k]
            eng.ops.append(lambda e, sem=sem, v=v: e.wait_ge(sem, v))

    @staticmethod
    def _deps(reads, writes, is_dma=False):
        deps = []
        for b in reads:
            deps.extend(b.w)
        for b in writes:
            deps.extend(b.r)
            if is_dma:
                deps.extend(t for t in b.w if not t[0].startswith("d_"))
                deps.extend(b.pr)
            else:
                deps.extend(b.w)
        return deps

    @staticmethod
    def _commit(tok, reads, writes, is_dma=False):
        for b in writes:
            if is_dma and not b.r and all(t[0].startswith("d_") for t in b.w):
                b.w = b.w + [tok]
            else:
                b.pr = list(b.r)
                b.w = [tok]
            b.r = []
        for b in reads:
            if b not in writes:
                b.r.append(tok)

    def op(self, engname, fn, reads=(), writes=()):
        eng = self.engs[engname]
        self._wait(eng, self._deps(reads, writes))
        eng.cnt += 1
        tok = (eng.name, eng.cnt)
        sem = self.sems[eng.name]
        name, a, k = _record(fn)
        eng.ops.append(lambda e, name=name, a=a, k=k, sem=sem: getattr(e, name)(*a, **k).then_inc(sem, 1))
        self._commit(tok, reads, writes)
        return tok

    def dma(self, q, fn, reads=(), writes=()):
        eng = self.engs[q]
        i = self.dma_next[q]
        self.dma_next[q] = (i + 1) % self.N_DMA_SEMS
        key = self.dma_sems[q][i]
        deps = self._deps(reads, writes, True)
        if self.dma_uses[key] > 0:
            deps.append((key, 16 * self.dma_uses[key]))
        self._wait(eng, deps)
        self.dma_uses[key] += 1
        tok = (key, 16 * self.dma_uses[key])
        sem = self.sems[key]
        name, a, k = _record(fn)
        eng.ops.append(lambda e, name=name, a=a, k=k, sem=sem: getattr(e, name)(*a, **_resolve(e, k)).then_inc(sem, 16))
        self._commit(tok, reads, writes, True)
        return tok

    def raw_dma(self, q, fn, reads=(), writes=()):
        eng = self.engs[q]
        i = self.dma_next[q]
        self.dma_next[q] = (i + 1) % self.N_DMA_SEMS
        key = self.dma_sems[q][i]
        deps = self._deps(reads, writes)
        if self.dma_uses[key] > 0:
            deps.append((key, 16 * self.dma_uses[key]))
        self._wait(eng, deps)
        self.dma_uses[key] += 1
        tok = (key, 16 * self.dma_uses[key])
        sem = self.sems[key]
        eng.ops.append(lambda e, fn=fn, sem=sem: fn(e).then_inc(sem, 16))
        self._commit(tok, reads, writes)
        return tok

    def _all_tokens(self):
        deps = []
        for key, n in self.dma_uses.items():
            if n > 0:
                deps.append((key, 16 * n))
        for name in ["pe", "dve", "act", "pool"]:
            if self.engs[name].cnt > 0:
                deps.append((name, self.engs[name].cnt))
        return deps

    def barrier(self):
        deps = self._all_tokens()
        for name in ["pe", "dve", "act", "pool", "sp"]:
            self._wait(self.engs[name], deps)

    def finish(self):
        self._wait(self.engs["sp"], self._all_tokens())

    def emit(self):
        nc = self.nc
        engs = self.engs
        with nc.Block() as block:
            @block.tensor
            def _(e):
                for f in engs["pe"].ops:
                    f(e)

            @block.vector
            def _(e):
                for f in engs["dve"].ops:
                    f(e)

            @block.scalar
            def _(e):
                for f in engs["act"].ops:
                    f(e)

            @block.gpsimd
            def _(e):
                for f in engs["pool"].ops:
                    f(e)

            @block.sync
            def _(e):
                for f in engs["sp"].ops:
                    f(e)


class Arena:
    def __init__(self, t, nbytes):
        self.t = t
        self.nbytes = nbytes
        self.off = 0

    def reset(self):
        self.off = 0

    def alloc(self, free_shape, dtype, parts=128):
        n = 1
        for s in free_shape:
            n *= s
        esz = 4 if dtype in (F32, I32) else 2
        nb = (n * esz + 63) // 64 * 64
        assert self.off + nb <= self.nbytes, ("arena overflow", self.off, nb)
        v = self.t[0:parts, self.off // 2:(self.off + n * esz) // 2]
        self.off += nb
        if dtype in (F32, I32):
            v = v.bitcast(dtype)
        if len(free_shape) == 2:
            v = v.rearrange("p (a b) -> p a b", a=free_shape[0])
        elif len(free_shape) == 3:
            v = v.rearrange("p (a b c) -> p a b c", a=free_shape[0], b=free_shape[1])
        return Buf(v)


def _consts():
    c = {}
    bf = ml_dtypes.bfloat16
    c["c_ident"] = np.eye(128, dtype=np.float32).astype(bf)
    c["c_identf"] = np.eye(128, dtype=np.float32)
    c["c_ones"] = np.ones((128, 128), np.float32).astype(bf)
    half = 128
    inv = (10000.0 ** (-np.linspace(0.0, 1.0, half, dtype=np.float32))).astype(np.float32)
    pos = np.arange(T, dtype=np.float32)
    ang = (pos[None, :] * inv[:, None]).astype(np.float32)
    cos = np.cos(ang).astype(np.float32)
    sin = np.sin(ang).astype(np.float32)
    c["c_rot"] = np.stack([cos, sin, cos / 16.0, sin / 16.0], 0).astype(np.float32)
    nh = 4
    log_g = np.log1p(-np.power(2.0, -5.0 - np.arange(nh, dtype=np.float64)))
    idx = np.arange(128, dtype=np.float64)
    diff = idx[None, :] - idx[:, None]
    decT = np.where(diff >= 0, np.exp(log_g[:, None, None] * np.maximum(diff, 0.0)[None]), 0.0)
    c["c_decT"] = np.ascontiguousarray(decT.transpose(1, 0, 2)).astype(np.float32)
    xi = np.exp(log_g[:, None] * (idx + 1.0)[None, :])
    xi_t = np.tile(xi, (1, T // 128))
    c["c_xi"] = np.ascontiguousarray(np.broadcast_to(xi_t[:, None, :], (nh, 128, T))).astype(np.float32)
    zeta = np.exp(log_g[:, None] * (127.0 - idx)[None, :])
    c["c_zeta"] = np.ascontiguousarray(zeta.T).astype(np.float32)
    cd = np.exp(log_g * 128.0)
    qt = np.arange(16)[:, None]
    n = np.arange(8)[None, :]
    past = (n < (qt // 2)).astype(np.float32).reshape(1, 128)
    own = (n == (qt // 2)).astype(np.float32).reshape(1, 128)
    c["c_moba"] = np.ascontiguousarray(np.stack([
        np.broadcast_to((past - 1.0) * 1e30, (128, 128)),
        np.broadcast_to(past, (128, 128)),
        np.broadcast_to(own, (128, 128))], 0)).astype(np.float32)
    e8 = np.zeros((128, 1024), np.float32)
    for i in range(8):
        e8[i, i * 128:(i + 1) * 128] = 1.0
    c["c_e8"] = e8.astype(bf)
    k = np.arange(128)[:, None, None]
    j = np.arange(4)[None, :, None]
    q = np.arange(512)[None, None, :]
    c["c_cm"] = np.where((128 * j + k) > q, NEG, 0.0).astype(np.float32).astype(bf)
    pp = np.arange(128)
    c["c_ustrict"] = (pp[:, None] < pp[None, :]).astype(np.float32).astype(bf)
    c["c_thr"] = np.ascontiguousarray(np.broadcast_to((float(BLK_ROWS) * np.arange(N_BLK, dtype=np.float32))[None, :], (128, N_BLK)))
    c["c_base"] = np.concatenate([(pp[:, None] * 8 + np.arange(8)[None, :]), (pp[:, None] * 4 + np.arange(4)[None, :])], 1).astype(np.float32)
    return c, [float(v) for v in cd]


def build(n_layers=DEPTH, debug=False, no_indirect=False, stop_after=None):
    _REG_CACHE.clear()
    nc = bass.Bass("TRN2", target_bir_lowering=False)
    consts, CD = _consts()

    def din(name, shape, dt=F32):
        return nc.dram_tensor(name, list(shape), dt, kind="ExternalInput").ap()

    def dscr(name, shape, dt):
        return nc.dram_tensor(name, list(shape), dt, kind=("ExternalOutput" if debug else "Internal")).ap()

    x_in = din("x", [T, D])
    xT_in = din("xT", [D, T])
    memT_in = din("memT", [D, 256])
    w_in = din("w_in", [DEPTH, D, 16384])
    p_moba = din("p_moba", [DEPTH, 1024, D])
    p_ret = din("p_ret", [DEPTH, 2048, D])
    p_mem = din("p_mem", [DEPTH, 1024, D])
    w_mem_kv = din("w_mem_kv", [DEPTH, D, 2048])
    w_o = din("w_o", [DEPTH, D, D])
    ln_par = din("ln_par", [DEPTH, 4, 128, D])
    w_r = din("w_r", [DEPTH, D, 36])
    b_r = din("b_r", [DEPTH, 128, 36])
    w_gup = din("w_gup", [DEPTH * N_EXP * 128 * 8, 2048])
    w_dnp = din("w_dnp", [DEPTH * N_EXP * 128 * 4, 2048])
    cin = {}
    for k, v in consts.items():
        cin[k] = din(k, v.shape, BF16 if v.dtype == ml_dtypes.bfloat16 else F32)
    out = nc.dram_tensor("out", [T, D], F32, kind="ExternalOutput").ap()

    QaT = dscr("QaT", [8, 128, T], BF16)
    KaT = dscr("KaT", [8, 128, T], BF16)
    Va = dscr("Va", [T, 1024], BF16)
    RqT = dscr("RqT", [8, 128, T], BF16)
    RkT = dscr("RkT", [8, 128, T], BF16)
    Rv = dscr("Rv", [T, 2048], BF16)
    RgT = dscr("RgT", [16, 128, T], BF16)
    MqT = dscr("MqT", [8, 128, T], BF16)
    GT = dscr("GT", [48, 128, T], BF16)
    YT = dscr("YT", [32, 128, T], BF16)
    MT = dscr("MT", [16, 128, T], BF16)
    X1 = dscr("X1", [T, D], F32)
    X1T = dscr("X1T", [16, 128, T], BF16)
    X2 = dscr("X2", [T, D], F32)
    X2T = dscr("X2T", [16, 128, T], BF16)
    X1B = dscr("X1B", [T, D], BF16)
    Xs = dscr("Xs", [N_BLK * BLK_ROWS, D], BF16)
    Ys = dscr("Ys", [N_BLK * BLK_ROWS, D], F32)

    if debug:
        DBG_dest = nc.dram_tensor("DBG_dest", [128, 32], I32, kind="ExternalOutput").ap()
        DBG_blk = nc.dram_tensor("DBG_blk", [128, N_BLK], F32, kind="ExternalOutput").ap()
        DBG_wab = nc.dram_tensor("DBG_wab", [128, 32], F32, kind="ExternalOutput").ap()
    with ExitStack() as st:
        S = Sched(nc, st)
        ARENA_BYTES = 198 * 1024
        arena_t = st.enter_context(nc.sbuf_tensor("arena", [128, ARENA_BYTES // 2], BF16))
        ar = Arena(arena_t, ARENA_BYTES)
        ident = Buf(st.enter_context(nc.sbuf_tensor("ident", [128, 128], BF16)))
        identf = Buf(st.enter_context(nc.sbuf_tensor("identf", [128, 128], F32)))
        ones = Buf(st.enter_context(nc.sbuf_tensor("ones", [128, 128], BF16)))
        def pers(name, shape, dt):
            return Buf(st.enter_context(nc.sbuf_tensor(name, shape, dt)))
        ustrict = pers("ustrict", [128, 128], BF16)
        selS_all = pers("selS_all", [128, 16, 32], BF16)
        selA_all = pers("selA_all", [128, 16, 32], BF16)
        wab_all = pers("wab_all", [128, 16, 2], F32)
        dest_i = pers("dest_i", [128, 16, 2], I32)
        blkE = pers("blkE", [128, N_BLK], F32)
        blkU = pers("blkU", [128, N_BLK], F32)
        ps = []
        for i in range(8):
            b = Buf(st.enter_context(nc.psum_tensor("ps%d" % i, [128, 512], F32)))
            b.bf = b.t[:, :].bitcast(BF16)
            ps.append(b)
        psn = [0]

        psm = [0]

        def nextps_m():
            p = ps[3 + (psm[0] % 3)]
            psm[0] += 1
            return p

        def nextps():
            p = ps[psn[0] % 8]
            psn[0] += 1
            return p

        S.dma("sp", lambda e: e.dma_start(out=ident[:, :], in_=cin["c_ident"][:, :]), writes=[ident])
        S.dma("sp", lambda e: e.dma_start(out=identf[:, :], in_=cin["c_identf"][:, :]), writes=[identf])
        S.dma("sp", lambda e: e.dma_start(out=ones[:, :], in_=cin["c_ones"][:, :]), writes=[ones])
        S.dma("sp", lambda e: e.dma_start(out=ustrict[:, :], in_=cin["c_ustrict"][:, :]), writes=[ustrict])

        def sl(i, n):
            return slice(i * n, (i + 1) * n)

        def mm(pt, o, l, r, start, stop, reads):
            S.op("pe", lambda e: e.matmul(o, l, r, start=start, stop=stop), reads=reads, writes=[pt])

        def load_w512(wbuf, wsrc2d, col0, nk=16):
            src = wsrc2d[:, col0:col0 + 512].rearrange("(kc p) f -> p kc f", p=128)
            S.dma("pool", lambda e: e.dma_start(out=wbuf[:, 0:nk, :], in_=src), writes=[wbuf])

        def load_actT(actT, L):
            if L == 0:
                src = xT_in.rearrange("(kc p) t -> p kc t", p=128)
                for kc in range(16):
                    S.dma("pool", lambda e, kc=kc: e.dma_start(out=actT[:, kc, :], in_=src[:, kc, :]), writes=[actT])
            else:
                S.dma("sp", lambda e: e.dma_start(out=actT[:, :, :], in_=X2T.rearrange("c p t -> p c t")), writes=[actT])

        def phase_A(L):
            S.barrier()
            ar.reset()
            actT = ar.alloc([16, T], BF16)
            load_actT(actT, L)
            rot = ar.alloc([4, T], F32)
            S.dma("sp", lambda e: e.dma_start(out=rot[:, :, :], in_=cin["c_rot"].rearrange("c p t -> p c t")), writes=[rot])
            wb = [ar.alloc([16, 512], BF16) for _ in range(2)]
            ot = [ar.alloc([T], BF16) for _ in range(4)]
            tm = [ar.alloc([512], BF16) for _ in range(3)]
            tmp = [ar.alloc([512], F32) for _ in range(4)]
            W = w_in[L]
            cnt = {"wb": 0, "ot": 0, "tm": 0}

            def proj_chunk(wbuf, c, tg):
                p = nextps()
                for kc in range(16):
                    mm(p, p[:, :], wbuf[:, kc, sl(c, 128)], actT[:, kc, sl(tg, 512)], kc == 0, kc == 15, [wbuf, actT])
                return p

            def tform(col0, ncols, kind, dest, dchunk0):
                for g in range(ncols // 512):
                    wbuf = wb[cnt["wb"] % 2]
                    cnt["wb"] += 1
                    load_w512(wbuf, W, col0 + g * 512)
                    if kind == "rot" or kind == "rotk":
                        ci, si = (0, 1) if kind == "rot" else (2, 3)
                        for pr in range(2):
                            o1 = ot[cnt["ot"] % 4]
                            o2 = ot[(cnt["ot"] + 1) % 4]
                            cnt["ot"] += 2
                            for tg in range(4):
                                p1 = proj_chunk(wbuf, 2 * pr, tg)
                                p2 = proj_chunk(wbuf, 2 * pr + 1, tg)
                                ts = sl(tg, 512)
                                S.op("dve", lambda e, p1=p1, ts=ts: e.tensor_tensor(tmp[0][:, :], p1[:, :], rot[:, ci, ts], ALU.mult), reads=[p1, rot], writes=[tmp[0]])
                                S.op("dve", lambda e, p2=p2, ts=ts: e.tensor_tensor(tmp[1][:, :], p2[:, :], rot[:, si, ts], ALU.mult), reads=[p2, rot], writes=[tmp[1]])
                                S.op("dve", lambda e, p1=p1, ts=ts: e.tensor_tensor(tmp[2][:, :], p1[:, :], rot[:, si, ts], ALU.mult), reads=[p1, rot], writes=[tmp[2]])
                                S.op("dve", lambda e, p2=p2, ts=ts: e.tensor_tensor(tmp[3][:, :], p2[:, :], rot[:, ci, ts], ALU.mult), reads=[p2, rot], writes=[tmp[3]])
                                S.op("pool", lambda e, o1=o1, ts=ts: e.tensor_tensor(o1[:, ts], tmp[0][:, :], tmp[1][:, :], ALU.subtract), reads=[tmp[0], tmp[1]], writes=[o1])
                                S.op("pool", lambda e, o2=o2, ts=ts: e.tensor_tensor(o2[:, ts], tmp[2][:, :], tmp[3][:, :], ALU.add), reads=[tmp[2], tmp[3]], writes=[o2])
                            ch = dchunk0 + g * 4 + pr * 2
                            S.dma("sp", lambda e, o1=o1, ch=ch: e.dma_start(out=dest[ch, :, :], in_=o1[:, :]), reads=[o1])
                            S.dma("sp", lambda e, o2=o2, ch=ch: e.dma_start(out=dest[ch + 1, :, :], in_=o2[:, :]), reads=[o2])
                    else:
                        for c in range(4):
                            o = ot[cnt["ot"] % 4]
                            cnt["ot"] += 1
                            for tg in range(4):
                                p = proj_chunk(wbuf, c, tg)
                                ts = sl(tg, 512)
                                if kind == "copy":
                                    if tg % 2 == 0:
                                        S.op("act", lambda e, p=p, o=o, ts=ts: e.copy(o[:, ts], p[:, :]), reads=[p], writes=[o])
                                    else:
                                        S.op("dve", lambda e, p=p, o=o, ts=ts: e.tensor_copy(o[:, ts], p[:, :]), reads=[p], writes=[o])
                                else:
                                    fn = AF.Silu if kind == "silu" else AF.Sigmoid
                                    S.op("act", lambda e, p=p, o=o, ts=ts, fn=fn: e.activation(o[:, ts], p[:, :], fn), reads=[p], writes=[o])
                            ch = dchunk0 + g * 4 + c
                            S.dma("sp", lambda e, o=o, ch=ch: e.dma_start(out=dest[ch, :, :], in_=o[:, :]), reads=[o])

            def tokmaj(col0, ncols, dest):
                for g in range(ncols // 512):
                    wbuf = wb[cnt["wb"] % 2]
                    cnt["wb"] += 1
                    load_w512(wbuf, W, col0 + g * 512)
                    for tt in range(16):
                        p = nextps()
                        for kc in range(16):
                            mm(p, p[:, :], actT[:, kc, sl(tt, 128)], wbuf[:, kc, :], kc == 0, kc == 15, [wbuf, actT])
                        o = tm[cnt["tm"] % 3]
                        cnt["tm"] += 1
                        if tt % 2 == 0:
                            S.op("act", lambda e, p=p, o=o: e.copy(o[:, :], p[:, :]), reads=[p], writes=[o])
                        else:
                            S.op("dve", lambda e, p=p, o=o: e.tensor_copy(o[:, :], p[:, :]), reads=[p], writes=[o])
                        S.dma("sp", lambda e, o=o, tt=tt, g=g: e.dma_start(out=dest[sl(tt, 128), sl(g, 512)], in_=o[:, :]), reads=[o])

            tform(0, 1024, "copy", QaT, 0)
            tform(1024, 1024, "copy", KaT, 0)
            tokmaj(2048, 1024, Va)
            tform(3072, 1024, "rot", RqT, 0)
            tform(4096, 1024, "rotk", RkT, 0)
            tokmaj(5120, 2048, Rv)
            tform(9216, 1024, "copy", MqT, 0)
            tform(7168, 2048, "silu", RgT, 0)
            tform(10240, 6144, "sigmoid", GT, 0)

        def phase_B(L):
            S.barrier()
            ar.reset()
            NB3 = 3
            QT = [ar.alloc([T], BF16) for _ in range(NB3)]
            KT = [ar.alloc([T], BF16) for _ in range(NB3)]
            V = [ar.alloc([16, 128], BF16) for _ in range(NB3)]
            mob = ar.alloc([3, 128], F32)
            e8 = ar.alloc([1024], BF16)
            cm = ar.alloc([4, 512], BF16)
            S.dma("sp", lambda e: e.dma_start(out=mob[:, :, :], in_=cin["c_moba"].rearrange("c p t -> p c t")), writes=[mob])
            S.dma("sp", lambda e: e.dma_start(out=e8[:, :], in_=cin["c_e8"][:, :]), writes=[e8])
            S.dma("sp", lambda e: e.dma_start(out=cm[:, :, :], in_=cin["c_cm"][:, :, :]), writes=[cm])
            km = [ar.alloc([8], F32) for _ in range(2)]
            kmb = [ar.alloc([8], BF16) for _ in range(2)]
            gm = ar.alloc([16, 8], F32)
            mx = ar.alloc([16, 8], F32)
            sel = ar.alloc([16, 8], F32)
            mbb = ar.alloc([128], BF16)
            MbT = [ar.alloc([T], BF16) for _ in range(2)]
            for m_ in MbT:
                S.op("pool", lambda e, m_=m_: e.memset(m_[:, :], 0.0), writes=[m_])
            PT = [ar.alloc([512], BF16) for _ in range(3)]
            rden = [ar.alloc([512], F32) for _ in range(2)]
            yo = [ar.alloc([T], BF16) for _ in range(2)]
            Vsrc = Va.rearrange("(tt p) f -> p tt f", p=128)

            def load(h):
                b = h % NB3
                S.dma("sp", lambda e: e.dma_start(out=QT[b][:, :], in_=QaT[h, :, :]), writes=[QT[b]])
                S.dma("sp", lambda e: e.dma_start(out=KT[b][:, :], in_=KaT[h, :, :]), writes=[KT[b]])
                S.dma("sp", lambda e: e.dma_start(out=V[b][:, :, :], in_=Vsrc[:, :, sl(h, 128)]), writes=[V[b]])

            def setup_a(h):
                k = KT[h % NB3]
                S.op("dve", lambda e: e.tensor_reduce(km[h % 2][:, :], k[:, :].rearrange("p (n j) -> p n j", n=8), AX.X, ALU.add), reads=[k], writes=[km[h % 2]])
                S.op("act", lambda e: e.mul(kmb[h % 2][:, :], km[h % 2][:, :], 1.0 / 256.0), reads=[km[h % 2]], writes=[kmb[h % 2]])

            def setup_b(h):
                q = QT[h % NB3]
                gp = ps[6]
                for qt in range(16):
                    mm(gp, gp[:, sl(qt, 8)], q[:, sl(qt, 128)], kmb[h % 2][:, :], True, True, [q, kmb[h % 2]])
                S.op("dve", lambda e: e.tensor_tensor(gm[:, :, :], gp[:, 0:128].rearrange("p (a b) -> p a b", a=16), mob[:, 0, :].rearrange("p (a b) -> p a b", a=16), ALU.add), reads=[gp, mob], writes=[gm])
                for qt in range(16):
                    S.op("dve", lambda e, qt=qt: e.max(mx[:, qt, :], gm[:, qt, :]), reads=[gm], writes=[mx])
                for qt in range(16):
                    S.op("dve", lambda e, qt=qt: e.tensor_scalar(sel[:, qt, :], gm[:, qt, :], mx[:, qt, 2:3], None, ALU.is_ge), reads=[gm, mx], writes=[sel])
                selv = sel[:, :, :].rearrange("p a b -> p (a b)")
                S.op("dve", lambda e: e.tensor_tensor(selv, selv, mob[:, 1, :], ALU.mult), reads=[sel, mob], writes=[sel])
                S.op("dve", lambda e: e.tensor_tensor(selv, selv, mob[:, 2, :], ALU.add), reads=[sel, mob], writes=[sel])
                S.op("dve", lambda e: e.tensor_scalar(mbb[:, :], selv, -1.0, -NEG, ALU.add, ALU.mult), reads=[sel], writes=[mbb])

            def setup_c(h):
                M = MbT[h % 2]
                for half in range(2):
                    tp = ps[6 + half]
                    for j in range(8):
                        qt = half * 8 + j
                        S.op("pe", lambda e, tp=tp, j=j, qt=qt: e.transpose(tp.bf[0:8, sl(j, 128)], mbb[:, sl(qt, 8)], ident[:, :]), reads=[mbb, ident], writes=[tp])
                    S.op("act", lambda e, tp=tp, half=half: e.copy(M[0:8, sl(half, 1024)], tp.bf[0:8, 0:1024]), reads=[tp], writes=[M])

            cnt = {"it": 0, "npt": 0}

            def main(h, mid):
                q, k, v, M, y = QT[h % NB3], KT[h % NB3], V[h % NB3], MbT[h % 2], yo[h % 2]
                items = [(g, kt) for g in range(4) for kt in range(4 * g + 4)]
                banks = {}
                for g in range(4):
                    banks[g] = (ps[2 + cnt["it"] % 2], ps[4 + cnt["it"] % 2])
                    cnt["it"] += 1
                sp_pt = {}

                def emit_S(i):
                    g, kt = items[i]
                    Sp = ps[cnt["npt"] % 2]
                    pt = PT[cnt["npt"] % 3]
                    cnt["npt"] += 1
                    sp_pt[i] = pt
                    diag = kt >= 4 * g
                    mm(Sp, Sp[:, :], k[:, sl(kt, 128)], q[:, sl(g, 512)], True, False, [k, q])
                    mm(Sp, Sp[:, :], e8[:, sl(kt // 2, 128)], M[:, sl(g, 512)], False, not diag, [e8, M])
                    if diag:
                        mm(Sp, Sp[:, :], ident[:, :], cm[:, kt - 4 * g, :], False, True, [ident, cm])
                    S.op("act", lambda e: e.activation(pt[:, :], Sp[:, :], AF.Exp, scale=128.0 ** -0.5), reads=[Sp], writes=[pt])

                def emit_PV(i):
                    g, kt = items[i]
                    Op, Dp = banks[g]
                    pt = sp_pt[i]
                    nkt = 4 * g + 4
                    mm(Op, Op[:, :], v[:, kt, :], pt[:, :], kt == 0, kt == nkt - 1, [v, pt])
                    mm(Dp, Dp[:, :], ones[:, :], pt[:, :], kt == 0, kt == nkt - 1, [ones, pt])
                    if kt == nkt - 1:
                        rd = rden[g % 2]
                        S.op("dve", lambda e: e.reciprocal(rd[:, :], Dp[:, :]), reads=[Dp], writes=[rd])
                        S.op("dve", lambda e: e.tensor_tensor(y[:, sl(g, 512)], Op[:, :], rd[:, :], ALU.mult), reads=[Op, rd], writes=[y])
                        if g == 0 and mid is not None:
                            mid()

                emit_S(0)
                for i in range(len(items)):
                    if i + 1 < len(items):
                        emit_S(i + 1)
                    emit_PV(i)
                S.dma("sp", lambda e: e.dma_start(out=YT[h, :, :], in_=y[:, :]), reads=[y])

            load(0)
            load(1)
            setup_a(0)
            setup_b(0)
            setup_c(0)
            setup_a(1)
            for h in range(8):
                if h + 2 < 8:
                    load(h + 2)
                nxt = (lambda h=h: setup_b(h + 1)) if h + 1 < 8 else None
                main(h, nxt)
                if h + 1 < 8:
                    setup_c(h + 1)
                if h + 2 < 8:
                    setup_a(h + 2)

        def phase_C(L):
            S.barrier()
            ar.reset()
            Rq = [ar.alloc([2, T], BF16) for _ in range(2)]
            Rk = [ar.alloc([2, T], BF16) for _ in range(2)]
            Rvt = [ar.alloc([16, 512], BF16) for _ in range(2)]
            Rg = [ar.alloc([4, T], BF16) for _ in range(2)]
            xi = [ar.alloc([T], F32) for _ in range(2)]
            decT = ar.alloc([4, 128], F32)
            zeta = ar.alloc([4], F32)
            S.dma("sp", lambda e: e.dma_start(out=decT[:, :, :], in_=cin["c_decT"][:, :, :]), writes=[decT])
            S.dma("sp", lambda e: e.dma_start(out=zeta[:, :], in_=cin["c_zeta"][:, :]), writes=[zeta])
            Qxi = ar.alloc([2, T], BF16)
            Kz = ar.alloc([16, 256], BF16)
            state = ar.alloc([2, 512], F32)
            stateb = ar.alloc([2, 512], BF16)
            onb = [ar.alloc([512], BF16) for _ in range(2)]
            STb = [ar.alloc([128], BF16) for _ in range(2)]
            st6 = ar.alloc([6], F32)
            mv = ar.alloc([2], F32)
            lnv = ar.alloc([1], F32)
            rstd = ar.alloc([1], F32)
            nmr = ar.alloc([1], F32)
            yr = [ar.alloc([4, T], BF16) for _ in range(2)]
            Rvsrc = Rv.rearrange("(tt p) f -> p tt f", p=128)

            def load(h):
                b = h % 2
                S.dma("sp", lambda e: e.dma_start(out=Rq[b][:, :, :], in_=RqT[2 * h:2 * h + 2].rearrange("c p t -> p c t")), writes=[Rq[b]])
                S.dma("sp", lambda e: e.dma_start(out=Rk[b][:, :, :], in_=RkT[2 * h:2 * h + 2].rearrange("c p t -> p c t")), writes=[Rk[b]])
                S.dma("sp", lambda e: e.dma_start(out=Rvt[b][:, :, :], in_=Rvsrc[:, :, sl(h, 512)]), writes=[Rvt[b]])
                S.dma("sp", lambda e: e.dma_start(out=Rg[b][:, :, :], in_=RgT[4 * h:4 * h + 4].rearrange("c p t -> p c t")), writes=[Rg[b]])
                S.dma("sp", lambda e: e.dma_start(out=xi[b][:, :], in_=cin["c_xi"][h, :, :]), writes=[xi[b]])

            load(0)
            for h in range(4):
                if h + 1 < 4:
                    load(h + 1)
                b = h % 2
                rq, rk, rv, rg, x_i, y = Rq[b], Rk[b], Rvt[b], Rg[b], xi[b], yr[b]
                for dc in range(2):
                    S.op("dve", lambda e, dc=dc: e.tensor_tensor(Qxi[:, dc, :], rq[:, dc, :], x_i[:, :], ALU.mult), reads=[rq, x_i], writes=[Qxi])
                for n in range(16):
                    tp = ps[5 + n % 2]
                    for dc in range(2):
                        S.op("pe", lambda e, tp=tp, dc=dc, n=n: e.transpose(tp.bf[:, sl(dc, 128)], rk[:, dc, sl(n, 128)], ident[:, :]), reads=[rk, ident], writes=[tp])
                    S.op("act", lambda e, tp=tp, n=n: e.activation(Kz[:, n, :], tp.bf[:, 0:256], AF.Identity, scale=zeta[:, h:h + 1]), reads=[tp, zeta], writes=[Kz])
                def emit_ST(n):
                    ns = sl(n, 128)
                    STp = ps[0]
                    for dc in range(2):
                        mm(STp, STp[:, 0:128], rk[:, dc, ns], rq[:, dc, ns], dc == 0, dc == 1, [rk, rq])
                    sb = STb[n % 2]
                    S.op("dve", lambda e: e.tensor_tensor(sb[:, :], STp[:, 0:128], decT[:, h, :], ALU.mult), reads=[STp, decT], writes=[sb])

                def emit_O(n):
                    ns = sl(n, 128)
                    sb = STb[n % 2]
                    Op = ps[1 + n % 2]
                    mm(Op, Op[:, :], sb[:, :], rv[:, n, :], True, n == 0, [sb, rv])
                    if n > 0:
                        for dc in range(2):
                            mm(Op, Op[:, :], Qxi[:, dc, ns], stateb[:, dc, :], False, dc == 1, [Qxi, stateb])
                    if n < 15:
                        for dc in range(2):
                            kv = ps[3 + dc]
                            mm(kv, kv[:, :], Kz[:, n, sl(dc, 128)], rv[:, n, :], True, True, [Kz, rv])
                            if n == 0:
                                S.op("dve", lambda e, kv=kv, dc=dc: e.tensor_copy(state[:, dc, :], kv[:, :]), reads=[kv], writes=[state])
                            else:
                                S.op("dve", lambda e, kv=kv, dc=dc: e.scalar_tensor_tensor(state[:, dc, :], state[:, dc, :], CD[h], kv[:, :], ALU.mult, ALU.add), reads=[kv, state], writes=[state])
                        S.op("act", lambda e: e.copy(stateb[:, :, :], state[:, :, :]), reads=[state], writes=[stateb])
                    S.op("dve", lambda e: e.bn_stats(st6[:, :], Op[:, :]), reads=[Op], writes=[st6])
                    S.op("dve", lambda e: e.bn_aggr(mv[:, :], st6[:, :]), reads=[st6], writes=[mv])
                    S.op("act", lambda e: e.activation(lnv[:, :], mv[:, 1:2], AF.Ln, bias=GN_EPS), reads=[mv], writes=[lnv])
                    S.op("act", lambda e: e.activation(rstd[:, :], lnv[:, :], AF.Exp, scale=-0.5), reads=[lnv], writes=[rstd])
                    S.op("dve", lambda e: e.scalar_tensor_tensor(nmr[:, :], mv[:, 0:1], -1.0, rstd[:, :], ALU.mult, ALU.mult), reads=[mv, rstd], writes=[nmr])
                    ob = onb[n % 2]
                    S.op("act", lambda e: e.activation(ob[:, :], Op[:, :], AF.Identity, bias=nmr[:, 0:1], scale=rstd[:, 0:1]), reads=[Op, nmr, rstd], writes=[ob])

                def emit_T(n):
                    ns = sl(n, 128)
                    ob = onb[n % 2]
                    tp = ps[7]
                    for ec in range(4):
                        S.op("pe", lambda e, ec=ec: e.transpose(tp.bf[:, sl(ec, 128)], ob[:, sl(ec, 128)], ident[:, :]), reads=[ob, ident], writes=[tp])
                    S.op("dve", lambda e: e.tensor_tensor(y[:, :, ns], tp.bf[:, 0:512].rearrange("p (a b) -> p a b", a=4), rg[:, :, ns], ALU.mult), reads=[tp, rg], writes=[y])

                emit_ST(0)
                for n in range(16):
                    if n + 1 < 16:
                        emit_ST(n + 1)
                    emit_O(n)
                    if n > 0:
                        emit_T(n - 1)
                emit_T(15)
                S.dma("sp", lambda e, y=y, h=h: e.dma_start(out=YT[8 + 4 * h:12 + 4 * h].rearrange("c p t -> p c t"), in_=y[:, :, :]), reads=[y])

        def phase_D(L):
            S.barrier()
            ar.reset()
            memT = ar.alloc([16, 256], BF16)
            S.dma("pool", lambda e: e.dma_start(out=memT[:, :, :], in_=memT_in.rearrange("(kc p) m -> p kc m", p=128)), writes=[memT])
            wb = [ar.alloc([16, 512], BF16) for _ in range(2)]
            kmT = ar.alloc([8, 256], BF16)
            vm = ar.alloc([2, 1024], BF16)
            Mq = [ar.alloc([2, T], BF16) for _ in range(2)]
            PT = [ar.alloc([512], BF16) for _ in range(4)]
            rden = [ar.alloc([512], F32) for _ in range(2)]
            ym = [ar.alloc([2, T], BF16) for _ in range(2)]
            W = w_mem_kv[L]
            for g in range(4):
                wbuf = wb[g % 2]
                load_w512(wbuf, W, g * 512)
                if g < 2:
                    for c in range(4):
                        p = nextps()
                        for kc in range(16):
                            mm(p, p[:, 0:256], wbuf[:, kc, sl(c, 128)], memT[:, kc, :], kc == 0, kc == 15, [wbuf, memT])
                        S.op("act", lambda e, p=p, g=g, c=c: e.copy(kmT[:, g * 4 + c, :], p[:, 0:256]), reads=[p], writes=[kmT])
                else:
                    for mt in range(2):
                        p = nextps()
                        for kc in range(16):
                            mm(p, p[:, :], memT[:, kc, sl(mt, 128)], wbuf[:, kc, :], kc == 0, kc == 15, [wbuf, memT])
                        S.op("dve", lambda e, p=p, g=g, mt=mt: e.tensor_copy(vm[:, mt, sl(g - 2, 512)], p[:, :]), reads=[p], writes=[vm])

            def load(h):
                S.dma("sp", lambda e: e.dma_start(out=Mq[h % 2][:, :, :], in_=MqT[2 * h:2 * h + 2].rearrange("c p t -> p c t")), writes=[Mq[h % 2]])

            load(0)
            npt = 0
            for h in range(4):
                if h + 1 < 4:
                    load(h + 1)
                mq, y = Mq[h % 2], ym[h % 2]
                for g in range(4):
                    pts = []
                    for mt in range(2):
                        Sp = ps[mt]
                        for dc in range(2):
                            mm(Sp, Sp[:, :], kmT[:, 2 * h + dc, sl(mt, 128)], mq[:, dc, sl(g, 512)], dc == 0, dc == 1, [kmT, mq])
                        pt = PT[npt % 4]
                        npt += 1
                        S.op("act", lambda e, Sp=Sp, pt=pt: e.activation(pt[:, :], Sp[:, :], AF.Exp, scale=1.0 / 16.0), reads=[Sp], writes=[pt])
                        pts.append(pt)
                    Dp = ps[4 + g % 2]
                    for mt in range(2):
                        mm(Dp, Dp[:, :], ones[:, :], pts[mt][:, :], mt == 0, mt == 1, [ones, pts[mt]])
                    rd = rden[g % 2]
                    S.op("dve", lambda e, rd=rd, Dp=Dp: e.reciprocal(rd[:, :], Dp[:, :]), reads=[Dp], writes=[rd])
                    for dc in range(2):
                        Op = ps[2 + dc]
                        for mt in range(2):
                            mm(Op, Op[:, :], vm[:, mt, sl(2 * h + dc, 128)], pts[mt][:, :], mt == 0, mt == 1, [vm, pts[mt]])
                        S.op("dve", lambda e, rd=rd, Op=Op, y=y, g=g, dc=dc: e.tensor_tensor(y[:, dc, sl(g, 512)], Op[:, :], rd[:, :], ALU.mult), reads=[Op, rd], writes=[y])
                S.dma("sp", lambda e, y=y, h=h: e.dma_start(out=YT[24 + 2 * h:26 + 2 * h].rearrange("c p t -> p c t"), in_=y[:, :, :]), reads=[y])

        def phase_E(L):
            S.barrier()
            ar.reset()
            yT = ar.alloc([32, T], BF16)
            for c0 in range(0, 32, 8):
                S.dma("sp", lambda e, c0=c0: e.dma_start(out=yT[:, c0:c0 + 8, :], in_=YT[c0:c0 + 8].rearrange("c p t -> p c t")), writes=[yT])
            wbE = [ar.alloc([32, 256], BF16) for _ in range(2)]
            sg = [ar.alloc([3, 2, 512], BF16) for _ in range(2)]
            tmp = [ar.alloc([512], F32) for _ in range(6)]
            mo = [ar.alloc([2, T], BF16) for _ in range(1)]
            it = 0

            def loadw(fg):
                wbuf = wbE[fg % 2]
                cs = slice(fg * 256, fg * 256 + 256)
                S.dma("pool", lambda e: e.dma_start(out=wbuf[:, 0:8, :], in_=p_moba[L][:, cs].rearrange("(kc p) f -> p kc f", p=128)), writes=[wbuf])
                S.dma("pool", lambda e: e.dma_start(out=wbuf[:, 8:24, :], in_=p_ret[L][:, cs].rearrange("(kc p) f -> p kc f", p=128)), writes=[wbuf])
                S.dma("pool", lambda e: e.dma_start(out=wbuf[:, 24:32, :], in_=p_mem[L][:, cs].rearrange("(kc p) f -> p kc f", p=128)), writes=[wbuf])

            loadw(0)
            for fg in range(8):
                if fg + 1 < 8:
                    loadw(fg + 1)
                wbuf = wbE[fg % 2]
                mout = mo[0]
                for tg in range(4):
                    ts = sl(tg, 512)
                    sgt = sg[it % 2]
                    it += 1
                    for br in range(3):
                        S.dma("sp", lambda e, br=br: e.dma_start(out=sgt[:, br, :, :], in_=GT[br * 16 + fg * 2:br * 16 + fg * 2 + 2, :, ts].rearrange("c p t -> p c t")), writes=[sgt])
                    for c in range(2):
                        zs = []
                        for (k0, k1) in ((0, 8), (8, 24), (24, 32)):
                            p = nextps()
                            for kc in range(k0, k1):
                                mm(p, p[:, :], wbuf[:, kc, sl(c, 128)], yT[:, kc, ts], kc == k0, kc == k1 - 1, [wbuf, yT])
                            zs.append(p)
                        t3 = tmp[(c % 2) * 3:(c % 2) * 3 + 3]
                        for br in range(3):
                            S.op("dve", lambda e, br=br: e.tensor_tensor(t3[br][:, :], zs[br][:, :], sgt[:, br, c, :], ALU.mult), reads=[zs[br], sgt], writes=[t3[br]])
                        S.op("pool", lambda e: e.tensor_tensor(t3[0][:, :], t3[0][:, :], t3[1][:, :], ALU.add), reads=[t3[1]], writes=[t3[0]])
                        S.op("pool", lambda e: e.tensor_tensor(mout[:, c, ts], t3[0][:, :], t3[2][:, :], ALU.add), reads=[t3[0], t3[2]], writes=[mout])
                S.dma("sp", lambda e: e.dma_start(out=MT[fg * 2:fg * 2 + 2].rearrange("c p t -> p c t"), in_=mout[:, :, :]), reads=[mout])

        def ln_tail(y, G, Bv, sm, x_dst, xT_dst, tt, x1b, xTt, router=None, part=None):
            if part != "q":
                ln_p(y, G, sm)
            if part != "p":
                ln_q(y, Bv, x_dst, xT_dst, tt, x1b, xTt, router)

        def ln_p(y, G, sm):
            st = sm["st"]
            for fs in range(4):
                S.op("dve", lambda e, fs=fs: e.bn_stats(st[:, fs, :], y[:, sl(fs, 512)]), reads=[y], writes=[st])
            S.op("dve", lambda e: e.bn_aggr(sm["mv"][:, :], st[:, :, :].rearrange("p a b -> p (a b)")), reads=[st], writes=[sm["mv"]])
            S.op("act", lambda e: e.activation(sm["lnv"][:, :], sm["mv"][:, 1:2], AF.Ln, bias=LN_EPS), reads=[sm["mv"]], writes=[sm["lnv"]])
            S.op("act", lambda e: e.activation(sm["rstd"][:, :], sm["lnv"][:, :], AF.Exp, scale=-0.5), reads=[sm["lnv"]], writes=[sm["rstd"]])
            S.op("dve", lambda e: e.scalar_tensor_tensor(sm["nmr"][:, :], sm["mv"][:, 0:1], -1.0, sm["rstd"][:, :], ALU.mult, ALU.mult), reads=[sm["mv"], sm["rstd"]], writes=[sm["nmr"]])
            S.op("act", lambda e: e.activation(y[:, :], y[:, :], AF.Identity, bias=sm["nmr"][:, 0:1], scale=sm["rstd"][:, 0:1]), reads=[sm["nmr"], sm["rstd"]], writes=[y])
            S.op("pool", lambda e: e.tensor_tensor(y[:, :], y[:, :], G[:, :], ALU.mult), reads=[G], writes=[y])

        def ln_q(y, Bv, x_dst, xT_dst, tt, x1b, xTt, router):
            S.op("dve", lambda e: e.tensor_tensor(y[:, :], y[:, :], Bv[:, :], ALU.add), reads=[Bv], writes=[y])
            S.dma("sp", lambda e: e.dma_start(out=x_dst[sl(tt, 128), :], in_=y[:, :]), reads=[y])
            if xT_dst is None:
                return
            S.op("act", lambda e: e.copy(x1b[:, :], y[:, :]), reads=[y], writes=[x1b])
            if router is not None:
                S.dma("sp", lambda e: e.dma_start(out=X1B[sl(tt, 128), :], in_=x1b[:, :]), reads=[x1b])
            for half in range(2):
                tp = ps[4 + half]
                for j in range(8):
                    kc = half * 8 + j
                    S.op("pe", lambda e, tp=tp, j=j, kc=kc: e.transpose(tp.bf[:, sl(j, 128)], x1b[:, sl(kc, 128)], ident[:, :]), reads=[x1b, ident], writes=[tp])
                if half == 0:
                    S.op("act", lambda e, tp=tp: e.copy(xTt[:, 0:8, :], tp.bf[:, :].rearrange("p (a b) -> p a b", a=8)), reads=[tp], writes=[xTt])
                else:
                    S.op("dve", lambda e, tp=tp: e.tensor_copy(xTt[:, 8:16, :], tp.bf[:, :].rearrange("p (a b) -> p a b", a=8)), reads=[tp], writes=[xTt])
            S.dma("sp", lambda e: e.dma_start(out=xT_dst[:, :, sl(tt, 128)].rearrange("c p t -> p c t"), in_=xTt[:, :, :]), reads=[xTt])
            if router is not None:
                router(xTt, tt)

        def ln_small():
            return {"st": ar.alloc([4, 6], F32), "mv": ar.alloc([2], F32), "lnv": ar.alloc([1], F32), "rstd": ar.alloc([1], F32), "nmr": ar.alloc([1], F32)}

        def phase_F(L):
            S.barrier()
            ar.reset()
            wo = [ar.alloc([16, 512], BF16) for _ in range(4)]
            for g in range(4):
                src = w_o[L][:, sl(g, 512)].rearrange("(kc p) f -> p kc f", p=128)
                S.dma("pool", lambda e, src=src, g=g: e.dma_start(out=wo[g][:, :, :], in_=src), writes=[wo[g]])
            G = ar.alloc([D], F32)
            Bv = ar.alloc([D], F32)
            S.dma("sp", lambda e: e.dma_start(out=G[:, :], in_=ln_par[L, 0, :, :]), writes=[G])
            S.dma("sp", lambda e: e.dma_start(out=Bv[:, :], in_=ln_par[L, 1, :, :]), writes=[Bv])
            wr = ar.alloc([16, 36], BF16)
            S.dma("pool", lambda e: e.dma_start(out=wr[:, :, :], in_=w_r[L].rearrange("(kc p) f -> p kc f", p=128)), writes=[wr])
            br = ar.alloc([36], F32)
            S.dma("sp", lambda e: e.dma_start(out=br[:, :], in_=b_r[L, :, :]), writes=[br])
            mT = [ar.alloc([16, 128], BF16) for _ in range(2)]
            xres = [ar.alloc([D], F32) for _ in range(2)]
            ys = [ar.alloc([D], F32) for _ in range(2)]
            x1b = [ar.alloc([D], BF16) for _ in range(2)]
            xTt = [ar.alloc([16, 128], BF16) for _ in range(2)]
            sms = [ln_small() for _ in range(2)]
            lg = ar.alloc([36], F32)
            r_ = {k: ar.alloc([n], F32) for k, n in (("gmax", 1), ("ngmax", 1), ("ge", 4), ("gsum", 1), ("gmask", 4), ("pen", 4),
                                                        ("em", 32), ("mx", 8), ("sel", 32), ("nv1", 1), ("ex", 32), ("e2", 1), ("den", 1), ("rr", 1), ("W", 32))}
            xsrc = x_in if L == 0 else X2

            def router(xt, tt):
                lp = ps[6]
                for kc in range(16):
                    mm(lp, lp[:, 0:36], xt[:, kc, :], wr[:, kc, :], kc == 0, kc == 15, [xt, wr])
                R = r_
                S.op("dve", lambda e: e.tensor_tensor(lg[:, :], lp[:, 0:36], br[:, :], ALU.add), reads=[lp, br], writes=[lg])
                S.op("dve", lambda e: e.tensor_reduce(R["gmax"][:, :], lg[:, 0:4], AX.X, ALU.max), reads=[lg], writes=[R["gmax"]])
                S.op("dve", lambda e: e.tensor_scalar(R["ngmax"][:, :], R["gmax"][:, :], -1.0, None, ALU.mult), reads=[R["gmax"]], writes=[R["ngmax"]])
                S.op("act", lambda e: e.activation(R["ge"][:, :], lg[:, 0:4], AF.Exp, bias=R["ngmax"][:, 0:1], accum_out=R["gsum"][:, 0:1]), reads=[lg, R["ngmax"]], writes=[R["ge"], R["gsum"]])
                S.op("dve", lambda e: e.tensor_scalar(R["gmask"][:, :], lg[:, 0:4], R["gmax"][:, 0:1], None, ALU.is_ge), reads=[lg, R["gmax"]], writes=[R["gmask"]])
                S.op("dve", lambda e: e.tensor_scalar(R["pen"][:, :], R["gmask"][:, :], -1.0, 1e30, ALU.add, ALU.mult), reads=[R["gmask"]], writes=[R["pen"]])
                for g in range(4):
                    S.op("dve", lambda e, g=g: e.tensor_scalar(R["em"][:, sl(g, 8)], lg[:, 4 + g * 8:12 + g * 8], R["pen"][:, g:g + 1], None, ALU.add), reads=[lg, R["pen"]], writes=[R["em"]])
                S.op("dve", lambda e: e.max(R["mx"][:, :], R["em"][:, :]), reads=[R["em"]], writes=[R["mx"]])
                S.op("dve", lambda e: e.tensor_scalar(R["sel"][:, :], R["em"][:, :], R["mx"][:, 1:2], None, ALU.is_ge), reads=[R["em"], R["mx"]], writes=[R["sel"]])
                S.op("dve", lambda e: e.tensor_scalar(R["nv1"][:, :], R["mx"][:, 0:1], -1.0, None, ALU.mult), reads=[R["mx"]], writes=[R["nv1"]])
                S.op("act", lambda e: e.activation(R["ex"][:, :], R["em"][:, :], AF.Exp, bias=R["nv1"][:, 0:1]), reads=[R["em"], R["nv1"]], writes=[R["ex"]])
                S.op("act", lambda e: e.activation(R["e2"][:, :], R["mx"][:, 1:2], AF.Exp, bias=R["nv1"][:, 0:1]), reads=[R["mx"], R["nv1"]], writes=[R["e2"]])
                S.op("dve", lambda e: e.scalar_tensor_tensor(R["den"][:, :], R["e2"][:, :], 1.0, R["gsum"][:, :], ALU.add, ALU.mult), reads=[R["e2"], R["gsum"]], writes=[R["den"]])
                S.op("dve", lambda e: e.reciprocal(R["rr"][:, :], R["den"][:, :]), reads=[R["den"]], writes=[R["rr"]])
                S.op("act", lambda e: e.copy(selS_all[:, tt, :], R["sel"][:, :]), reads=[R["sel"]], writes=[selS_all])
                S.op("dve", lambda e: e.tensor_scalar(selA_all[:, tt, :], R["em"][:, :], R["mx"][:, 0:1], None, ALU.is_ge), reads=[R["em"], R["mx"]], writes=[selA_all])
                S.op("act", lambda e: e.copy(wab_all[:, tt, 0:1], R["rr"][:, :]), reads=[R["rr"]], writes=[wab_all])
                S.op("dve", lambda e: e.tensor_tensor(wab_all[:, tt, 1:2], R["rr"][:, :], R["e2"][:, :], ALU.mult), reads=[R["rr"], R["e2"]], writes=[wab_all])

            def load(tt):
                b = tt % 2
                S.dma("sp", lambda e: e.dma_start(out=mT[b][:, :, :], in_=MT[:, :, sl(tt, 128)].rearrange("c p t -> p c t")), writes=[mT[b]])
                S.dma("sp", lambda e: e.dma_start(out=xres[b][:, :], in_=xsrc[sl(tt, 128), :]), writes=[xres[b]])

            def mm_pe(tt):
                b = tt % 2
                for fs in range(4):
                    p = ps[fs]
                    for kc in range(16):
                        mm(p, p[:, :], mT[b][:, kc, :], wo[fs][:, kc, :], kc == 0, kc == 15, [mT[b], wo[fs]])

            def mm_stt(tt):
                b = tt % 2
                y = ys[b]
                for fs in range(4):
                    p = ps[fs]
                    S.op("dve", lambda e, p=p, fs=fs: e.scalar_tensor_tensor(y[:, sl(fs, 512)], xres[b][:, sl(fs, 512)], ALPHA, p[:, :], ALU.mult, ALU.add), reads=[p, xres[b]], writes=[y])

            load(0)
            load(1)
            mm_pe(0)
            mm_stt(0)
            for tt in range(16):
                b = tt % 2
                if tt + 1 < 16:
                    mm_pe(tt + 1)
                ln_tail(ys[b], G, Bv, sms[b], X1, X1T, tt, x1b[b], xTt[b], router, part="p")
                if tt > 0:
                    pb = (tt - 1) % 2
                    ln_tail(ys[pb], G, Bv, sms[pb], X1, X1T, tt - 1, x1b[pb], xTt[pb], router, part="q")
                if tt + 1 < 16:
                    mm_stt(tt + 1)
                if tt + 2 < 16:
                    load(tt + 2)
            ln_tail(ys[1], G, Bv, sms[1], X1, X1T, 15, x1b[1], xTt[1], router, part="q")
            cnt = ar.alloc([32], F32)
            accs = [ar.alloc([32], F32) for _ in range(2)]
            cps = ps[6]
            for tt in range(16):
                mm(cps, cps[:, 0:32], ones[:, :], selS_all[:, tt, :], tt == 0, tt == 15, [ones, selS_all])
            S.op("dve", lambda e: e.tensor_copy(cnt[:, :], cps[:, 0:32]), reads=[cps], writes=[cnt])
            S.op("dve", lambda e: e.tensor_scalar(accs[0][:, :], cnt[:, :], 0.5, None, ALU.is_gt), reads=[cnt], writes=[accs[0]])
            NJ = T // BLK_ROWS
            for j in range(1, NJ):
                S.op("dve", lambda e, j=j: e.scalar_tensor_tensor(accs[j % 2][:, :], cnt[:, :], float(BLK_ROWS) * j + 0.5, accs[(j + 1) % 2][:, :], ALU.is_gt, ALU.add), reads=[cnt, accs[(j + 1) % 2]], writes=[accs[j % 2]])
            padded = ar.alloc([32], F32)
            S.op("dve", lambda e: e.tensor_scalar(padded[:, :], accs[(NJ - 1) % 2][:, :], float(BLK_ROWS), None, ALU.mult), reads=[accs[(NJ - 1) % 2]], writes=[padded])
            cs = [ar.alloc([32], F32) for _ in range(2)]
            S.op("dve", lambda e: e.tensor_copy(cs[0][:, :], padded[:, :]), reads=[padded], writes=[cs[0]])
            k = 0
            for dsh in (1, 2, 4, 8, 16):
                a_, b_ = cs[k % 2], cs[(k + 1) % 2]
                S.op("dve", lambda e, a_=a_, b_=b_, dsh=dsh: e.tensor_copy(b_[:, 0:dsh], a_[:, 0:dsh]), reads=[a_], writes=[b_])
                S.op("dve", lambda e, a_=a_, b_=b_, dsh=dsh: e.tensor_tensor(b_[:, dsh:32], a_[:, dsh:32], a_[:, 0:32 - dsh], ALU.add), reads=[a_], writes=[b_])
                k += 1
            pend = cs[k % 2]
            pstart = ar.alloc([32], F32)
            S.op("dve", lambda e: e.tensor_tensor(pstart[:, :], pend[:, :], padded[:, :], ALU.subtract), reads=[pend, padded], writes=[pstart])
            thr = ar.alloc([N_BLK], F32)
            S.dma("sp", lambda e: e.dma_start(out=thr[:, :], in_=cin["c_thr"][:, :]), writes=[thr])
            bacc = [ar.alloc([N_BLK], F32) for _ in range(2)]
            S.op("dve", lambda e: e.tensor_scalar(bacc[0][:, :], thr[:, :], pend[:, 0:1], None, ALU.is_ge), reads=[thr, pend], writes=[bacc[0]])
            for ex in range(1, 32):
                S.op("dve", lambda e, ex=ex: e.scalar_tensor_tensor(bacc[ex % 2][:, :], thr[:, :], pend[:, ex:ex + 1], bacc[(ex + 1) % 2][:, :], ALU.is_ge, ALU.add), reads=[thr, pend, bacc[(ex + 1) % 2]], writes=[bacc[ex % 2]])
            S.op("dve", lambda e: e.tensor_scalar(blkE[:, :], bacc[1][:, :], 31.0, None, ALU.min), reads=[bacc[1]], writes=[blkE])
            S.op("dve", lambda e: e.tensor_scalar(blkU[:, :], thr[:, :], pend[:, 31:32], OOB, ALU.is_ge, ALU.mult), reads=[thr, pend], writes=[blkU])
            dtmp = ar.alloc([32], F32)
            dprod = ar.alloc([32], F32)
            selB = ar.alloc([32], F32)
            dflt = ar.alloc([16, 2], F32)
            for tt in range(16):
                rp = ps[tt % 2]
                for t2 in range(tt):
                    mm(rp, rp[:, 0:32], ones[:, :], selS_all[:, t2, :], t2 == 0, False, [ones, selS_all])
                mm(rp, rp[:, 0:32], ustrict[:, :], selS_all[:, tt, :], tt == 0, True, [ustrict, selS_all])
                S.op("dve", lambda e, rp=rp: e.tensor_tensor(dtmp[:, :], rp[:, 0:32], pstart[:, :], ALU.add), reads=[rp, pstart], writes=[dtmp])
                S.op("dve", lambda e, tt=tt: e.tensor_tensor(dprod[:, :], dtmp[:, :], selA_all[:, tt, :], ALU.mult), reads=[dtmp, selA_all], writes=[dprod])
                S.op("dve", lambda e, tt=tt: e.tensor_reduce(dflt[:, tt, 0:1], dprod[:, :], AX.X, ALU.add), reads=[dprod], writes=[dflt])
                S.op("dve", lambda e, tt=tt: e.tensor_tensor(selB[:, :], selS_all[:, tt, :], selA_all[:, tt, :], ALU.subtract), reads=[selS_all, selA_all], writes=[selB])
                S.op("dve", lambda e: e.tensor_tensor(dprod[:, :], dtmp[:, :], selB[:, :], ALU.mult), reads=[dtmp, selB], writes=[dprod])
                S.op("dve", lambda e, tt=tt: e.tensor_reduce(dflt[:, tt, 1:2], dprod[:, :], AX.X, ALU.add), reads=[dprod], writes=[dflt])
            S.op("dve", lambda e: e.tensor_copy(dest_i[:, :, :], dflt[:, :, :]), reads=[dflt], writes=[dest_i])
            if debug:
                S.dma("sp", lambda e: e.dma_start(out=DBG_dest[:, :], in_=dest_i[:, :, :].rearrange("p a b -> p (a b)")), reads=[dest_i])
                S.dma("sp", lambda e: e.dma_start(out=DBG_blk[:, :], in_=blkE[:, :]), reads=[blkE])
                S.dma("sp", lambda e: e.dma_start(out=DBG_wab[:, :], in_=wab_all[:, :, :].rearrange("p a b -> p (a b)")), reads=[wab_all])

        def phase_X(L):
            S.barrier()
            ar.reset()
            xb = [ar.alloc([D], BF16) for _ in range(3)]
            for tt in range(16):
                b = xb[tt % 3]
                S.dma("sp", lambda e, b=b, tt=tt: e.dma_start(out=b[:, :], in_=X1B[sl(tt, 128), :]), writes=[b])
                for s_ in range(2):
                    S.dma("pool", lambda e, b=b, tt=tt, s_=s_: e.indirect_dma_start(
                        out=Xs[:, :], out_offset=bass.IndirectOffsetOnAxis(ap=dest_i[:, tt, s_:s_ + 1], axis=0),
                        in_=b[:, :], in_offset=None), reads=[b, dest_i])

        def phase_M(L):
            S.barrier()
            ar.reset()
            base = ar.alloc([12], F32)
            S.dma("sp", lambda e: e.dma_start(out=base[:, :], in_=cin["c_base"][:, :]), writes=[base])
            b1024 = ar.alloc([N_BLK], F32)
            b512 = ar.alloc([N_BLK], F32)
            S.op("dve", lambda e: e.tensor_scalar(b1024[:, :], blkE[:, :], 1024.0, float(L * N_EXP * 1024), ALU.mult, ALU.add), reads=[blkE], writes=[b1024])
            S.op("dve", lambda e: e.tensor_scalar(b512[:, :], blkE[:, :], 512.0, float(L * N_EXP * 512), ALU.mult, ALU.add), reads=[blkE], writes=[b512])
            S.op("dve", lambda e: e.tensor_tensor(b1024[:, :], b1024[:, :], blkU[:, :], ALU.add), reads=[blkU], writes=[b1024])
            S.op("dve", lambda e: e.tensor_tensor(b512[:, :], b512[:, :], blkU[:, :], ALU.add), reads=[blkU], writes=[b512])
            idf = ar.alloc([N_BLK, 12], F32)
            idi = ar.alloc([N_BLK, 12], I32)
            for j in range(12):
                srcb = b1024 if j < 8 else b512
                S.op("dve", lambda e, j=j, srcb=srcb: e.tensor_scalar(idf[:, :, j], srcb[:, :], base[:, j:j + 1], None, ALU.add), reads=[srcb, base], writes=[idf])
            S.op("dve", lambda e: e.tensor_copy(idi[:, :, :], idf[:, :, :]), reads=[idf], writes=[idi])
            NWB = 3
            wgu = [ar.alloc([8, 2048], BF16) for _ in range(NWB)]
            wdn = [ar.alloc([4, 2048], BF16) for _ in range(NWB)]
            xb = [ar.alloc([D], BF16) for _ in range(2)]
            xbT = [ar.alloc([16, 128], BF16) for _ in range(2)]
            sg = [ar.alloc([512], F32) for _ in range(2)]
            actb = [ar.alloc([512], BF16) for _ in range(2)]
            actT = [ar.alloc([4, 128], BF16) for _ in range(2)]
            ysb = [ar.alloc([D], F32) for _ in range(2)]
            RT = BLK_ROWS // 128
            order = []
            lo, hi = 0, N_BLK - 1
            while lo <= hi:
                for _ in range(2):
                    if lo <= hi:
                        order.append(lo)
                        lo += 1
                if lo <= hi:
                    order.append(hi)
                    hi -= 1
            assert sorted(order) == list(range(N_BLK))
            rows = [(bk * RT + rt, pos % NWB) for pos, bk in enumerate(order) for rt in range(RT)]
            NR = len(rows)

            def loadw(pos):
                bk = order[pos]
                i = pos % NWB
                for j in range(8):
                    S.dma("pool", lambda e, j=j: e.indirect_dma_start(out=wgu[i][:, j, :], out_offset=None, in_=w_gup[:, :],
                                                                     in_offset=bass.IndirectOffsetOnAxis(ap=idi[:, bk, j:j + 1], axis=0),
                                                                     bounds_check=RegConst(DEPTH * N_EXP * 128 * 8 - 1), oob_is_err=False), reads=[idi], writes=[wgu[i]])
                for j in range(4):
                    S.dma("pool", lambda e, j=j: e.indirect_dma_start(out=wdn[i][:, j, :], out_offset=None, in_=w_dnp[:, :],
                                                                     in_offset=bass.IndirectOffsetOnAxis(ap=idi[:, bk, 8 + j:9 + j], axis=0),
                                                                     bounds_check=RegConst(DEPTH * N_EXP * 128 * 4 - 1), oob_is_err=False), reads=[idi], writes=[wdn[i]])

            def loadx(n):
                S.dma("sp", lambda e: e.dma_start(out=xb[n % 2][:, :], in_=Xs[sl(rows[n][0], 128), :]), writes=[xb[n % 2]])

            def st1(n):
                x_, xt = xb[n % 2], xbT[n % 2]
                for half in range(2):
                    tp = ps[6 + half]
                    for j in range(8):
                        kc = half * 8 + j
                        S.op("pe", lambda e, tp=tp, j=j, kc=kc: e.transpose(tp.bf[:, sl(j, 128)], x_[:, sl(kc, 128)], ident[:, :]), reads=[x_, ident], writes=[tp])
                    if half == 0:
                        S.op("act", lambda e, tp=tp: e.copy(xt[:, 0:8, :], tp.bf[:, :].rearrange("p (a b) -> p a b", a=8)), reads=[tp], writes=[xt])
                    else:
                        S.op("dve", lambda e, tp=tp: e.tensor_copy(xt[:, 8:16, :], tp.bf[:, :].rearrange("p (a b) -> p a b", a=8)), reads=[tp], writes=[xt])

            def st2(n):
                i = rows[n][1]
                k = n % 2
                wg = wgu[i][:, :, :].rearrange("p a (b c) -> p (a b) c", b=2)
                xt = xbT[k]
                gp, up = ps[0], ps[1]
                for kc in range(16):
                    mm(gp, gp[:, :], xt[:, kc, :], wg[:, kc, 0:512], kc == 0, kc == 15, [xt, wgu[i]])
                for kc in range(16):
                    mm(up, up[:, :], xt[:, kc, :], wg[:, kc, 512:1024], kc == 0, kc == 15, [xt, wgu[i]])
                S.op("act", lambda e: e.activation(sg[k][:, :], gp[:, :], AF.Silu), reads=[gp], writes=[sg[k]])
                S.op("dve", lambda e: e.tensor_tensor(actb[k][:, :], up[:, :], sg[k][:, :], ALU.mult), reads=[up, sg[k]], writes=[actb[k]])

            def st3(n):
                k = n % 2
                tp = ps[2]
                for c in range(4):
                    S.op("pe", lambda e, c=c: e.transpose(tp.bf[:, sl(c, 128)], actb[k][:, sl(c, 128)], ident[:, :]), reads=[actb[k], ident], writes=[tp])
                S.op("act", lambda e: e.copy(actT[k][:, :, :], tp.bf[:, 0:512].rearrange("p (a b) -> p a b", a=4)), reads=[tp], writes=[actT[k]])

            def st4(n):
                r, i = rows[n]
                k = n % 2
                wd, at, yb = wdn[i], actT[k], ysb[k]
                for dg in range(4):
                    yp = nextps_m()
                    for c in range(4):
                        mm(yp, yp[:, :], at[:, c, :], wd[:, c, sl(dg, 512)], c == 0, c == 3, [at, wd])
                    if dg % 2 == 0:
                        S.op("act", lambda e, yp=yp, dg=dg: e.copy(yb[:, sl(dg, 512)], yp[:, :]), reads=[yp], writes=[yb])
                    else:
                        S.op("dve", lambda e, yp=yp, dg=dg: e.tensor_copy(yb[:, sl(dg, 512)], yp[:, :]), reads=[yp], writes=[yb])
                S.dma("sp", lambda e: e.dma_start(out=Ys[sl(r, 128), :], in_=yb[:, :]), reads=[yb])

            loadw(0)
            loadw(1)
            loadx(0)
            loadx(1)
            st1(0)
            for n in range(NR):
                st2(n)
                if n > 0:
                    st4(n - 1)
                if n % RT == 0 and n // RT + 2 < N_BLK:
                    loadw(n // RT + 2)
                if n + 1 < NR:
                    st1(n + 1)
                if n + 2 < NR:
                    loadx(n + 2)
                st3(n)
            st4(NR - 1)

        def phase_G(L, last):
            S.barrier()
            ar.reset()
            G = ar.alloc([D], F32)
            Bv = ar.alloc([D], F32)
            S.dma("sp", lambda e: e.dma_start(out=G[:, :], in_=ln_par[L, 2, :, :]), writes=[G])
            S.dma("sp", lambda e: e.dma_start(out=Bv[:, :], in_=ln_par[L, 3, :, :]), writes=[Bv])
            x1 = [ar.alloc([D], F32) for _ in range(3)]
            ff = [ar.alloc([D], F32) for _ in range(3)]
            fb = [ar.alloc([D], F32) for _ in range(3)]
            x1b = [ar.alloc([D], BF16) for _ in range(2)]
            xTt = [ar.alloc([16, 128], BF16) for _ in range(2)]
            sms = [ln_small() for _ in range(2)]

            def load(tt):
                b = tt % 3
                S.dma("sp", lambda e: e.dma_start(out=x1[b][:, :], in_=X1[sl(tt, 128), :]), writes=[x1[b]])
                S.dma("pool", lambda e: e.indirect_dma_start(out=ff[b][:, :], out_offset=None, in_=Ys[:, :], in_offset=bass.IndirectOffsetOnAxis(ap=dest_i[:, tt, 0:1], axis=0)), reads=[dest_i], writes=[ff[b]])
                S.dma("pool", lambda e: e.indirect_dma_start(out=fb[b][:, :], out_offset=None, in_=Ys[:, :], in_offset=bass.IndirectOffsetOnAxis(ap=dest_i[:, tt, 1:2], axis=0)), reads=[dest_i], writes=[fb[b]])

            dst_x = out if last else X2
            dst_xT = None if last else X2T

            def P(tt):
                b = tt % 3
                y = ff[b]
                S.op("act", lambda e: e.activation(y[:, :], y[:, :], AF.Identity, scale=wab_all[:, tt, 0:1]), reads=[wab_all], writes=[y])
                S.op("dve", lambda e: e.scalar_tensor_tensor(y[:, :], fb[b][:, :], wab_all[:, tt, 1:2], y[:, :], ALU.mult, ALU.add), reads=[fb[b], wab_all], writes=[y])
                S.op("dve", lambda e: e.scalar_tensor_tensor(y[:, :], x1[b][:, :], ALPHA, y[:, :], ALU.mult, ALU.add), reads=[x1[b]], writes=[y])
                ln_tail(y, G, Bv, sms[tt % 2], dst_x, dst_xT, tt, x1b[tt % 2], xTt[tt % 2], part="p")

            def Q(tt):
                ln_tail(ff[tt % 3], G, Bv, sms[tt % 2], dst_x, dst_xT, tt, x1b[tt % 2], xTt[tt % 2], part="q")

            load(0)
            load(1)
            load(2)
            P(0)
            for tt in range(16):
                if tt + 1 < 16:
                    P(tt + 1)
                Q(tt)
                if tt + 3 < 16:
                    load(tt + 3)

        for L in range(n_layers):
            phase_A(L)
            phase_B(L)
            phase_C(L)
            phase_D(L)
            phase_E(L)
            phase_F(L)
            if no_indirect:
                continue
            phase_X(L)
            if stop_after == "X":
                continue
            phase_M(L)
            if stop_after == "M":
                continue
            phase_G(L, L == n_layers - 1)
        S.finish()
        S.emit()
    return nc, consts


_CACHE = {}


def _prep_shared(inp):
    f = lambda a: np.ascontiguousarray(np.asarray(a, dtype=np.float32))
    sh = {}
    for k in ["w_in", "p_moba", "p_ret", "p_mem", "w_mem_kv", "w_o"]:
        sh[k] = f(inp[k])
    g = f(inp["w_gate_up"]).reshape(DEPTH, N_EXP, 16, 128, 1024).transpose(0, 1, 3, 2, 4)
    sh["w_gup"] = np.ascontiguousarray(g).reshape(DEPTH * N_EXP * 128 * 8, 2048)
    dn = f(inp["w_down"]).reshape(DEPTH, N_EXP, 4, 128, D).transpose(0, 1, 3, 2, 4)
    sh["w_dnp"] = np.ascontiguousarray(dn).reshape(DEPTH * N_EXP * 128 * 4, 2048)
    lnp = np.stack([f(inp["ln1_g"]), f(inp["ln1_b"]), f(inp["ln2_g"]), f(inp["ln2_b"])], 1)
    sh["ln_par"] = np.ascontiguousarray(np.broadcast_to(lnp[:, :, None, :], (DEPTH, 4, 128, D)))
    sh["w_r"] = np.ascontiguousarray(np.concatenate([f(inp["w_group"]), f(inp["w_expert"])], -1))
    br = np.concatenate([f(inp["b_group"]), f(inp["b_expert"])], -1)
    sh["b_r"] = np.ascontiguousarray(np.broadcast_to(br[:, None, :], (DEPTH, 128, 36)))
    return sh


def kernel(**inputs):
    if "nc" not in _CACHE:
        _CACHE["nc"] = build()
    nc, consts = _CACHE["nc"]
    sh = _prep_shared(inputs)
    x = np.asarray(inputs["x"], dtype=np.float32)
    mem = np.asarray(inputs["mem"], dtype=np.float32)
    in_maps = []
    for b in range(8):
        m = dict(sh)
        m.update(consts)
        m["x"] = np.ascontiguousarray(x[b])
        m["xT"] = np.ascontiguousarray(x[b].T)
        m["memT"] = np.ascontiguousarray(mem[b].T)
        in_maps.append(m)
    res = run_bass_kernel_spmd(nc, in_maps, core_ids=list(range(8)))
    return np.stack([np.asarray(r["out"], dtype=np.float32) for r in res.results], 0)
```

```python
import math
import numpy as np
import ml_dtypes
import concourse.bass as bass
import concourse.mybir as mybir
from concourse.bass_utils import run_bass_kernel_spmd
from contextlib import ExitStack

F32 = mybir.dt.float32
BF16 = mybir.dt.bfloat16
I32 = mybir.dt.int32
AF = mybir.ActivationFunctionType
ALU = mybir.AluOpType
AX = mybir.AxisListType

T = 2048
D = 2048
DEPTH = 2
ALPHA = float((2 * DEPTH) ** 0.25)
LN_EPS = 1e-5
GN_EPS = 1e-6
NEG = -30000.0
N_EXP = 32
BLK_ROWS = 256
N_BLK = 48
OOB = float(2 ** 20)


class Buf:
    def __init__(self, ap, name=""):
        self.t = ap
        self.name = name
        self.w = []
        self.pr = []
        self.r = []
        self.bf = None

    def __getitem__(self, idx):
        return self.t[idx]


class _Rec:
    def __init__(self):
        self.call = None

    def __getattr__(self, name):
        def f(*a, **k):
            self.call = (name, a, k)
            return self
        return f


class RegConst:
    def __init__(self, v):
        self.v = v


_REG_CACHE = {}


def _resolve(e, k):
    out = {}
    for key, val in k.items():
        if isinstance(val, RegConst):
            ck = (id(e), val.v)
            if ck not in _REG_CACHE:
                _REG_CACHE[ck] = e.to_reg(val.v)
            val = _REG_CACHE[ck]
        out[key] = val
    return out


def _record(fn):
    r = _Rec()
    fn(r)
    return r.call


class EngState:
    def __init__(self, name):
        self.name = name
        self.cnt = 0
        self.ops = []
        self.seen = {}


class Sched:
    N_DMA_SEMS = 32

    def __init__(self, nc, stack):
        self.nc = nc
        self.sems = {}
        self.engs = {}
        for name in ["pe", "dve", "act", "pool", "sp"]:
            self.sems[name] = stack.enter_context(nc.semaphore("s_" + name))
            self.engs[name] = EngState(name)
        self.dma_sems = {}
        self.dma_next = {}
        self.dma_uses = {}
        for q in ["sp", "pool"]:
            lst = []
            for i in range(self.N_DMA_SEMS):
                key = "d_%s_%d" % (q, i)
                self.sems[key] = stack.enter_context(nc.semaphore(key))
                lst.append(key)
                self.dma_uses[key] = 0
            self.dma_sems[q] = lst
            self.dma_next[q] = 0

    def _wait(self, eng, deps):
        best = {}
        for d in deps:
            if d is None:
                continue
            k, v = d
            if best.get(k, 0) < v:
                best[k] = v
        for k, v in best.items():
            if k == eng.name and eng.name == "pe":
                continue
            if eng.seen.get(k, 0) >= v:
                continue
            eng.seen[k] = v
            sem = self.sems[k]
            eng.ops.append(lambda e, sem=sem, v=v: e.wait_ge(sem, v))

    @staticmethod
    def _deps(reads, writes, is_dma=False):
        deps = []
        for b in reads:
            deps.extend(b.w)
        for b in writes:
            deps.extend(b.r)
            if is_dma:
                deps.extend(t for t in b.w if not t[0].startswith("d_"))
                deps.extend(b.pr)
            else:
                deps.extend(b.w)
        return deps

    @staticmethod
    def _commit(tok, reads, writes, is_dma=False):
        for b in writes:
            if is_dma and not b.r and all(t[0].startswith("d_") for t in b.w):
                b.w = b.w + [tok]
            else:
                b.pr = list(b.r)
                b.w = [tok]
            b.r = []
        for b in reads:
            if b not in writes:
                b.r.append(tok)

    def op(self, engname, fn, reads=(), writes=()):
        eng = self.engs[engname]
        self._wait(eng, self._deps(reads, writes))
        eng.cnt += 1
        tok = (eng.name, eng.cnt)
        sem = self.sems[eng.name]
        name, a, k = _record(fn)
        eng.ops.append(lambda e, name=name, a=a, k=k, sem=sem: getattr(e, name)(*a, **k).then_inc(sem, 1))
        self._commit(tok, reads, writes)
        return tok

    def dma(self, q, fn, reads=(), writes=()):
        eng = self.engs[q]
        i = self.dma_next[q]
        self.dma_next[q] = (i + 1) % self.N_DMA_SEMS
        key = self.dma_sems[q][i]
        deps = self._deps(reads, writes, True)
        if self.dma_uses[key] > 0:
            deps.append((key, 16 * self.dma_uses[key]))
        self._wait(eng, deps)
        self.dma_uses[key] += 1
        tok = (key, 16 * self.dma_uses[key])
        sem = self.sems[key]
        name, a, k = _record(fn)
        eng.ops.append(lambda e, name=name, a=a, k=k, sem=sem: getattr(e, name)(*a, **_resolve(e, k)).then_inc(sem, 16))
        self._commit(tok, reads, writes, True)
        return tok

    def raw_dma(self, q, fn, reads=(), writes=()):
        eng = self.engs[q]
        i = self.dma_next[q]
        self.dma_next[q] = (i + 1) % self.N_DMA_SEMS
        key = self.dma_sems[q][i]
        deps = self._deps(reads, writes)
        if self.dma_uses[key] > 0:
            deps.append((key, 16 * self.dma_uses[key]))
        self._wait(eng, deps)
        self.dma_uses[key] += 1
        tok = (key, 16 * self.dma_uses[key])
        sem = self.sems[key]
        eng.ops.append(lambda e, fn=fn, sem=sem: fn(e).then_inc(sem, 16))
        self._commit(tok, reads, writes)
        return tok

    def _all_tokens(self):
        deps = []
        for key, n in self.dma_uses.items():
            if n > 0:
                deps.append((key, 16 * n))
        for name in ["pe", "dve", "act", "pool"]:
            if self.engs[name].cnt > 0:
                deps.append((name, self.engs[name].cnt))
        return deps

    def barrier(self):
        deps = self._all_tokens()
        for name in ["pe", "dve", "act", "pool", "sp"]:
            self._wait(self.engs[name], deps)

    def finish(self):
        self._wait(self.engs["sp"], self._all_tokens())

    def emit(self):
        nc = self.nc
        engs = self.engs
        with nc.Block() as block:
            @block.tensor
            def _(e):
                for f in engs["pe"].ops:
                    f(e)

            @block.vector
            def _(e):
                for f in engs["dve"].ops:
                    f(e)

            @block.scalar
            def _(e):
                for f in engs["act"].ops:
                    f(e)

            @block.gpsimd
            def _(e):
                for f in engs["pool"].ops:
                    f(e)

            @block.sync
            def _(e):
                for f in engs["sp"].ops:
                    f(e)


class Arena:
    def __init__(self, t, nbytes):
        self.t = t
        self.nbytes = nbytes
        self.off = 0

    def reset(self):
        self.off = 0

    def alloc(self, free_shape, dtype, parts=128):
        n = 1
        for s in free_shape:
            n *= s
        esz = 4 if dtype in (F32, I32) else 2
        nb = (n * esz + 63) // 64 * 64
        assert self.off + nb <= self.nbytes, ("arena overflow", self.off, nb)
        v = self.t[0:parts, self.off // 2:(self.off + n * esz) // 2]
        self.off += nb
        if dtype in (F32, I32):
            v = v.bitcast(dtype)
        if len(free_shape) == 2:
            v = v.rearrange("p (a b) -> p a b", a=free_shape[0])
        elif len(free_shape) == 3:
            v = v.rearrange("p (a b c) -> p a b c", a=free_shape[0], b=free_shape[1])
        return Buf(v)


def _consts():
    c = {}
    bf = ml_dtypes.bfloat16
    c["c_ident"] = np.eye(128, dtype=np.float32).astype(bf)
    c["c_identf"] = np.eye(128, dtype=np.float32)
    c["c_ones"] = np.ones((128, 128), np.float32).astype(bf)
    half = 128
    inv = (10000.0 ** (-np.linspace(0.0, 1.0, half, dtype=np.float32))).astype(np.float32)
    pos = np.arange(T, dtype=np.float32)
    ang = (pos[None, :] * inv[:, None]).astype(np.float32)
    cos = np.cos(ang).astype(np.float32)
    sin = np.sin(ang).astype(np.float32)
    c["c_rot"] = np.stack([cos, sin, cos / 16.0, sin / 16.0], 0).astype(np.float32)
    nh = 4
    log_g = np.log1p(-np.power(2.0, -5.0 - np.arange(nh, dtype=np.float64)))
    idx = np.arange(128, dtype=np.float64)
    diff = idx[None, :] - idx[:, None]
    decT = np.where(diff >= 0, np.exp(log_g[:, None, None] * np.maximum(diff, 0.0)[None]), 0.0)
    c["c_decT"] = np.ascontiguousarray(decT.transpose(1, 0, 2)).astype(np.float32)
    xi = np.exp(log_g[:, None] * (idx + 1.0)[None, :])
    xi_t = np.tile(xi, (1, T // 128))
    c["c_xi"] = np.ascontiguousarray(np.broadcast_to(xi_t[:, None, :], (nh, 128, T))).astype(np.float32)
    zeta = np.exp(log_g[:, None] * (127.0 - idx)[None, :])
    c["c_zeta"] = np.ascontiguousarray(zeta.T).astype(np.float32)
    cd = np.exp(log_g * 128.0)
    qt = np.arange(16)[:, None]
    n = np.arange(8)[None, :]
    past = (n < (qt // 2)).astype(np.float32).reshape(1, 128)
    own = (n == (qt // 2)).astype(np.float32).reshape(1, 128)
    c["c_moba"] = np.ascontiguousarray(np.stack([
        np.broadcast_to((past - 1.0) * 1e30, (128, 128)),
        np.broadcast_to(past, (128, 128)),
        np.broadcast_to(own, (128, 128))], 0)).astype(np.float32)
    e8 = np.zeros((128, 1024), np.float32)
    for i in range(8):
        e8[i, i * 128:(i + 1) * 128] = 1.0
    c["c_e8"] = e8.astype(bf)
    k = np.arange(128)[:, None, None]
    j = np.arange(4)[None, :, None]
    q = np.arange(512)[None, None, :]
    c["c_cm"] = np.where((128 * j + k) > q, NEG, 0.0).astype(np.float32).astype(bf)
    pp = np.arange(128)
    c["c_ustrict"] = (pp[:, None] < pp[None, :]).astype(np.float32).astype(bf)
    c["c_thr"] = np.ascontiguousarray(np.broadcast_to((float(BLK_ROWS) * np.arange(N_BLK, dtype=np.float32))[None, :], (128, N_BLK)))
    c["c_base"] = np.concatenate([(pp[:, None] * 8 + np.arange(8)[None, :]), (pp[:, None] * 4 + np.arange(4)[None, :])], 1).astype(np.float32)
    return c, [float(v) for v in cd]


def build(n_layers=DEPTH, debug=False, no_indirect=False, stop_after=None):
    _REG_CACHE.clear()
    nc = bass.Bass("TRN2", target_bir_lowering=False)
    consts, CD = _consts()

    def din(name, shape, dt=F32):
        return nc.dram_tensor(name, list(shape), dt, kind="ExternalInput").ap()

    def dscr(name, shape, dt):
        return nc.dram_tensor(name, list(shape), dt, kind=("ExternalOutput" if debug else "Internal")).ap()

    x_in = din("x", [T, D])
    xT_in = din("xT", [D, T])
    memT_in = din("memT", [D, 256])
    w_in = din("w_in", [DEPTH, D, 16384])
    p_moba = din("p_moba", [DEPTH, 1024, D])
    p_ret = din("p_ret", [DEPTH, 2048, D])
    p_mem = din("p_mem", [DEPTH, 1024, D])
    w_mem_kv = din("w_mem_kv", [DEPTH, D, 2048])
    w_o = din("w_o", [DEPTH, D, D])
    ln_par = din("ln_par", [DEPTH, 4, 128, D])
    w_r = din("w_r", [DEPTH, D, 36])
    b_r = din("b_r", [DEPTH, 128, 36])
    w_gup = din("w_gup", [DEPTH * N_EXP * 128 * 8, 2048])
    w_dnp = din("w_dnp", [DEPTH * N_EXP * 128 * 4, 2048])
    cin = {}
    for k, v in consts.items():
        cin[k] = din(k, v.shape, BF16 if v.dtype == ml_dtypes.bfloat16 else F32)
    out = nc.dram_tensor("out", [T, D], F32, kind="ExternalOutput").ap()

    QaT = dscr("QaT", [8, 128, T], BF16)
    KaT = dscr("KaT", [8, 128, T], BF16)
    Va = dscr("Va", [T, 1024], BF16)
    RqT = dscr("RqT", [8, 128, T], BF16)
    RkT = dscr("RkT", [8, 128, T], BF16)
    Rv = dscr("Rv", [T, 2048], BF16)
    RgT = dscr("RgT", [16, 128, T], BF16)
    MqT = dscr("MqT", [8, 128, T], BF16)
    GT = dscr("GT", [48, 128, T], BF16)
    YT = dscr("YT", [32, 128, T], BF16)
    MT = dscr("MT", [16, 128, T], BF16)
    X1 = dscr("X1", [T, D], F32)
    X1T = dscr("X1T", [16, 128, T], BF16)
    X2 = dscr("X2", [T, D], F32)
    X2T = dscr("X2T", [16, 128, T], BF16)
    X1B = dscr("X1B", [T, D], BF16)
    Xs = dscr("Xs", [N_BLK * BLK_ROWS, D], BF16)
    Ys = dscr("Ys", [N_BLK * BLK_ROWS, D], F32)

    if debug:
        DBG_dest = nc.dram_tensor("DBG_dest", [128, 32], I32, kind="ExternalOutput").ap()
        DBG_blk = nc.dram_tensor("DBG_blk", [128, N_BLK], F32, kind="ExternalOutput").ap()
        DBG_wab = nc.dram_tensor("DBG_wab", [128, 32], F32, kind="ExternalOutput").ap()
    with ExitStack() as st:
        S = Sched(nc, st)
        ARENA_BYTES = 198 * 1024
        arena_t = st.enter_context(nc.sbuf_tensor("arena", [128, ARENA_BYTES // 2], BF16))
        ar = Arena(arena_t, ARENA_BYTES)
        ident = Buf(st.enter_context(nc.sbuf_tensor("ident", [128, 128], BF16)))
        identf = Buf(st.enter_context(nc.sbuf_tensor("identf", [128, 128], F32)))
        ones = Buf(st.enter_context(nc.sbuf_tensor("ones", [128, 128], BF16)))
        def pers(name, shape, dt):
            return Buf(st.enter_context(nc.sbuf_tensor(name, shape, dt)))
        ustrict = pers("ustrict", [128, 128], BF16)
        selS_all = pers("selS_all", [128, 16, 32], BF16)
        selA_all = pers("selA_all", [128, 16, 32], BF16)
        wab_all = pers("wab_all", [128, 16, 2], F32)
        dest_i = pers("dest_i", [128, 16, 2], I32)
        blkE = pers("blkE", [128, N_BLK], F32)
        blkU = pers("blkU", [128, N_BLK], F32)
        ps = []
        for i in range(8):
            b = Buf(st.enter_context(nc.psum_tensor("ps%d" % i, [128, 512], F32)))
            b.bf = b.t[:, :].bitcast(BF16)
            ps.append(b)
        psn = [0]

        psm = [0]

        def nextps_m():
            p = ps[3 + (psm[0] % 3)]
            psm[0] += 1
            return p

        def nextps():
            p = ps[psn[0] % 8]
            psn[0] += 1
            return p

        S.dma("sp", lambda e: e.dma_start(out=ident[:, :], in_=cin["c_ident"][:, :]), writes=[ident])
        S.dma("sp", lambda e: e.dma_start(out=identf[:, :], in_=cin["c_identf"][:, :]), writes=[identf])
        S.dma("sp", lambda e: e.dma_start(out=ones[:, :], in_=cin["c_ones"][:, :]), writes=[ones])
        S.dma("sp", lambda e: e.dma_start(out=ustrict[:, :], in_=cin["c_ustrict"][:, :]), writes=[ustrict])

        def sl(i, n):
            return slice(i * n, (i + 1) * n)

        def mm(pt, o, l, r, start, stop, reads):
            S.op("pe", lambda e: e.matmul(o, l, r, start=start, stop=stop), reads=reads, writes=[pt])

        def load_w512(wbuf, wsrc2d, col0, nk=16):
            src = wsrc2d[:, col0:col0 + 512].rearrange("(kc p) f -> p kc f", p=128)
            S.dma("pool", lambda e: e.dma_start(out=wbuf[:, 0:nk, :], in_=src), writes=[wbuf])

        def load_actT(actT, L):
            if L == 0:
                src = xT_in.rearrange("(kc p) t -> p kc t", p=128)
                for kc in range(16):
                    S.dma("pool", lambda e, kc=kc: e.dma_start(out=actT[:, kc, :], in_=src[:, kc, :]), writes=[actT])
            else:
                S.dma("sp", lambda e: e.dma_start(out=actT[:, :, :], in_=X2T.rearrange("c p t -> p c t")), writes=[actT])

        def phase_A(L):
            S.barrier()
            ar.reset()
            actT = ar.alloc([16, T], BF16)
            load_actT(actT, L)
            rot = ar.alloc([4, T], F32)
            S.dma("sp", lambda e: e.dma_start(out=rot[:, :, :], in_=cin["c_rot"].rearrange("c p t -> p c t")), writes=[rot])
            wb = [ar.alloc([16, 512], BF16) for _ in range(2)]
            ot = [ar.alloc([T], BF16) for _ in range(4)]
            tm = [ar.alloc([512], BF16) for _ in range(3)]
            tmp = [ar.alloc([512], F32) for _ in range(4)]
            W = w_in[L]
            cnt = {"wb": 0, "ot": 0, "tm": 0}

            def proj_chunk(wbuf, c, tg):
                p = nextps()
                for kc in range(16):
                    mm(p, p[:, :], wbuf[:, kc, sl(c, 128)], actT[:, kc, sl(tg, 512)], kc == 0, kc == 15, [wbuf, actT])
                return p

            def tform(col0, ncols, kind, dest, dchunk0):
                for g in range(ncols // 512):
                    wbuf = wb[cnt["wb"] % 2]
                    cnt["wb"] += 1
                    load_w512(wbuf, W, col0 + g * 512)
                    if kind == "rot" or kind == "rotk":
                        ci, si = (0, 1) if kind == "rot" else (2, 3)
                        for pr in range(2):
                            o1 = ot[cnt["ot"] % 4]
                            o2 = ot[(cnt["ot"] + 1) % 4]
                            cnt["ot"] += 2
                            for tg in range(4):
                                p1 = proj_chunk(wbuf, 2 * pr, tg)
                                p2 = proj_chunk(wbuf, 2 * pr + 1, tg)
                                ts = sl(tg, 512)
                                S.op("dve", lambda e, p1=p1, ts=ts: e.tensor_tensor(tmp[0][:, :], p1[:, :], rot[:, ci, ts], ALU.mult), reads=[p1, rot], writes=[tmp[0]])
                                S.op("dve", lambda e, p2=p2, ts=ts: e.tensor_tensor(tmp[1][:, :], p2[:, :], rot[:, si, ts], ALU.mult), reads=[p2, rot], writes=[tmp[1]])
                                S.op("dve", lambda e, p1=p1, ts=ts: e.tensor_tensor(tmp[2][:, :], p1[:, :], rot[:, si, ts], ALU.mult), reads=[p1, rot], writes=[tmp[2]])
                                S.op("dve", lambda e, p2=p2, ts=ts: e.tensor_tensor(tmp[3][:, :], p2[:, :], rot[:, ci, ts], ALU.mult), reads=[p2, rot], writes=[tmp[3]])
                                S.op("pool", lambda e, o1=o1, ts=ts: e.tensor_tensor(o1[:, ts], tmp[0][:, :], tmp[1][:, :], ALU.subtract), reads=[tmp[0], tmp[1]], writes=[o1])
                                S.op("pool", lambda e, o2=o2, ts=ts: e.tensor_tensor(o2[:, ts], tmp[2][:, :], tmp[3][:, :], ALU.add), reads=[tmp[2], tmp[3]], writes=[o2])
                            ch = dchunk0 + g * 4 + pr * 2
                            S.dma("sp", lambda e, o1=o1, ch=ch: e.dma_start(out=dest[ch, :, :], in_=o1[:, :]), reads=[o1])
                            S.dma("sp", lambda e, o2=o2, ch=ch: e.dma_start(out=dest[ch + 1, :, :], in_=o2[:, :]), reads=[o2])
                    else:
                        for c in range(4):
                            o = ot[cnt["ot"] % 4]
                            cnt["ot"] += 1
                            for tg in range(4):
                                p = proj_chunk(wbuf, c, tg)
                                ts = sl(tg, 512)
                                if kind == "copy":
                                    if tg % 2 == 0:
                                        S.op("act", lambda e, p=p, o=o, ts=ts: e.copy(o[:, ts], p[:, :]), reads=[p], writes=[o])
                                    else:
                                        S.op("dve", lambda e, p=p, o=o, ts=ts: e.tensor_copy(o[:, ts], p[:, :]), reads=[p], writes=[o])
                                else:
                                    fn = AF.Silu if kind == "silu" else AF.Sigmoid
                                    S.op("act", lambda e, p=p, o=o, ts=ts, fn=fn: e.activation(o[:, ts], p[:, :], fn), reads=[p], writes=[o])
                            ch = dchunk0 + g * 4 + c
                            S.dma("sp", lambda e, o=o, ch=ch: e.dma_start(out=dest[ch, :, :], in_=o[:, :]), reads=[o])

            def tokmaj(col0, ncols, dest):
                for g in range(ncols // 512):
                    wbuf = wb[cnt["wb"] % 2]
                    cnt["wb"] += 1
                    load_w512(wbuf, W, col0 + g * 512)
                    for tt in range(16):
                        p = nextps()
                        for kc in range(16):
                            mm(p, p[:, :], actT[:, kc, sl(tt, 128)], wbuf[:, kc, :], kc == 0, kc == 15, [wbuf, actT])
                        o = tm[cnt["tm"] % 3]
                        cnt["tm"] += 1
                        if tt % 2 == 0:
                            S.op("act", lambda e, p=p, o=o: e.copy(o[:, :], p[:, :]), reads=[p], writes=[o])
                        else:
                            S.op("dve", lambda e, p=p, o=o: e.tensor_copy(o[:, :], p[:, :]), reads=[p], writes=[o])
                        S.dma("sp", lambda e, o=o, tt=tt, g=g: e.dma_start(out=dest[sl(tt, 128), sl(g, 512)], in_=o[:, :]), reads=[o])

            tform(0, 1024, "copy", QaT, 0)
            tform(1024, 1024, "copy", KaT, 0)
            tokmaj(2048, 1024, Va)
            tform(3072, 1024, "rot", RqT, 0)
            tform(4096, 1024, "rotk", RkT, 0)
            tokmaj(5120, 2048, Rv)
            tform(9216, 1024, "copy", MqT, 0)
            tform(7168, 2048, "silu", RgT, 0)
            tform(10240, 6144, "sigmoid", GT, 0)

        def phase_B(L):
            S.barrier()
            ar.reset()
            NB3 = 3
            QT = [ar.alloc([T], BF16) for _ in range(NB3)]
            KT = [ar.alloc([T], BF16) for _ in range(NB3)]
            V = [ar.alloc([16, 128], BF16) for _ in range(NB3)]
            mob = ar.alloc([3, 128], F32)
            e8 = ar.alloc([1024], BF16)
            cm = ar.alloc([4, 512], BF16)
            S.dma("sp", lambda e: e.dma_start(out=mob[:, :, :], in_=cin["c_moba"].rearrange("c p t -> p c t")), writes=[mob])
            S.dma("sp", lambda e: e.dma_start(out=e8[:, :], in_=cin["c_e8"][:, :]), writes=[e8])
            S.dma("sp", lambda e: e.dma_start(out=cm[:, :, :], in_=cin["c_cm"][:, :, :]), writes=[cm])
            km = [ar.alloc([8], F32) for _ in range(2)]
            kmb = [ar.alloc([8], BF16) for _ in range(2)]
            gm = ar.alloc([16, 8], F32)
            mx = ar.alloc([16, 8], F32)
            sel = ar.alloc([16, 8], F32)
            mbb = ar.alloc([128], BF16)
            MbT = [ar.alloc([T], BF16) for _ in range(2)]
            for m_ in MbT:
                S.op("pool", lambda e, m_=m_: e.memset(m_[:, :], 0.0), writes=[m_])
            PT = [ar.alloc([512], BF16) for _ in range(3)]
            rden = [ar.alloc([512], F32) for _ in range(2)]
            yo = [ar.alloc([T], BF16) for _ in range(2)]
            Vsrc = Va.rearrange("(tt p) f -> p tt f", p=128)

            def load(h):
                b = h % NB3
                S.dma("sp", lambda e: e.dma_start(out=QT[b][:, :], in_=QaT[h, :, :]), writes=[QT[b]])
                S.dma("sp", lambda e: e.dma_start(out=KT[b][:, :], in_=KaT[h, :, :]), writes=[KT[b]])
                S.dma("sp", lambda e: e.dma_start(out=V[b][:, :, :], in_=Vsrc[:, :, sl(h, 128)]), writes=[V[b]])

            def setup_a(h):
                k = KT[h % NB3]
                S.op("dve", lambda e: e.tensor_reduce(km[h % 2][:, :], k[:, :].rearrange("p (n j) -> p n j", n=8), AX.X, ALU.add), reads=[k], writes=[km[h % 2]])
                S.op("act", lambda e: e.mul(kmb[h % 2][:, :], km[h % 2][:, :], 1.0 / 256.0), reads=[km[h % 2]], writes=[kmb[h % 2]])

            def setup_b(h):
                q = QT[h % NB3]
                gp = ps[6]
                for qt in range(16):
                    mm(gp, gp[:, sl(qt, 8)], q[:, sl(qt, 128)], kmb[h % 2][:, :], True, True, [q, kmb[h % 2]])
                S.op("dve", lambda e: e.tensor_tensor(gm[:, :, :], gp[:, 0:128].rearrange("p (a b) -> p a b", a=16), mob[:, 0, :].rearrange("p (a b) -> p a b", a=16), ALU.add), reads=[gp, mob], writes=[gm])
                for qt in range(16):
                    S.op("dve", lambda e, qt=qt: e.max(mx[:, qt, :], gm[:, qt, :]), reads=[gm], writes=[mx])
                for qt in range(16):
                    S.op("dve", lambda e, qt=qt: e.tensor_scalar(sel[:, qt, :], gm[:, qt, :], mx[:, qt, 2:3], None, ALU.is_ge), reads=[gm, mx], writes=[sel])
                selv = sel[:, :, :].rearrange("p a b -> p (a b)")
                S.op("dve", lambda e: e.tensor_tensor(selv, selv, mob[:, 1, :], ALU.mult), reads=[sel, mob], writes=[sel])
                S.op("dve", lambda e: e.tensor_tensor(selv, selv, mob[:, 2, :], ALU.add), reads=[sel, mob], writes=[sel])
                S.op("dve", lambda e: e.tensor_scalar(mbb[:, :], selv, -1.0, -NEG, ALU.add, ALU.mult), reads=[sel], writes=[mbb])

            def setup_c(h):
                M = MbT[h % 2]
                for half in range(2):
                    tp = ps[6 + half]
                    for j in range(8):
                        qt = half * 8 + j
                        S.op("pe", lambda e, tp=tp, j=j, qt=qt: e.transpose(tp.bf[0:8, sl(j, 128)], mbb[:, sl(qt, 8)], ident[:, :]), reads=[mbb, ident], writes=[tp])
                    S.op("act", lambda e, tp=tp, half=half: e.copy(M[0:8, sl(half, 1024)], tp.bf[0:8, 0:1024]), reads=[tp], writes=[M])

            cnt = {"it": 0, "npt": 0}

            def main(h, mid):
                q, k, v, M, y = QT[h % NB3], KT[h % NB3], V[h % NB3], MbT[h % 2], yo[h % 2]
                items = [(g, kt) for g in range(4) for kt in range(4 * g + 4)]
                banks = {}
                for g in range(4):
                    banks[g] = (ps[2 + cnt["it"] % 2], ps[4 + cnt["it"] % 2])
                    cnt["it"] += 1
                sp_pt = {}

                def emit_S(i):
                    g, kt = items[i]
                    Sp = ps[cnt["npt"] % 2]
                    pt = PT[cnt["npt"] % 3]
                    cnt["npt"] += 1
                    sp_pt[i] = pt
                    diag = kt >= 4 * g
                    mm(Sp, Sp[:, :], k[:, sl(kt, 128)], q[:, sl(g, 512)], True, False, [k, q])
                    mm(Sp, Sp[:, :], e8[:, sl(kt // 2, 128)], M[:, sl(g, 512)], False, not diag, [e8, M])
                    if diag:
                        mm(Sp, Sp[:, :], ident[:, :], cm[:, kt - 4 * g, :], False, True, [ident, cm])
                    S.op("act", lambda e: e.activation(pt[:, :], Sp[:, :], AF.Exp, scale=128.0 ** -0.5), reads=[Sp], writes=[pt])

                def emit_PV(i):
                    g, kt = items[i]
                    Op, Dp = banks[g]
                    pt = sp_pt[i]
                    nkt = 4 * g + 4
                    mm(Op, Op[:, :], v[:, kt, :], pt[:, :], kt == 0, kt == nkt - 1, [v, pt])
                    mm(Dp, Dp[:, :], ones[:, :], pt[:, :], kt == 0, kt == nkt - 1, [ones, pt])
                    if kt == nkt - 1:
                        rd = rden[g % 2]
                        S.op("dve", lambda e: e.reciprocal(rd[:, :], Dp[:, :]), reads=[Dp], writes=[rd])
                        S.op("dve", lambda e: e.tensor_tensor(y[:, sl(g, 512)], Op[:, :], rd[:, :], ALU.mult), reads=[Op, rd], writes=[y])
                        if g == 0 and mid is not None:
                            mid()

                emit_S(0)
                for i in range(len(items)):
                    if i + 1 < len(items):
                        emit_S(i + 1)
                    emit_PV(i)
                S.dma("sp", lambda e: e.dma_start(out=YT[h, :, :], in_=y[:, :]), reads=[y])

            load(0)
            load(1)
            setup_a(0)
            setup_b(0)
            setup_c(0)
            setup_a(1)
            for h in range(8):
                if h + 2 < 8:
                    load(h + 2)
                nxt = (lambda h=h: setup_b(h + 1)) if h + 1 < 8 else None
                main(h, nxt)
                if h + 1 < 8:
                    setup_c(h + 1)
                if h + 2 < 8:
                    setup_a(h + 2)

        def phase_C(L):
            S.barrier()
            ar.reset()
            Rq = [ar.alloc([2, T], BF16) for _ in range(2)]
            Rk = [ar.alloc([2, T], BF16) for _ in range(2)]
            Rvt = [ar.alloc([16, 512], BF16) for _ in range(2)]
            Rg = [ar.alloc([4, T], BF16) for _ in range(2)]
            xi = [ar.alloc([T], F32) for _ in range(2)]
            decT = ar.alloc([4, 128], F32)
            zeta = ar.alloc([4], F32)
            S.dma("sp", lambda e: e.dma_start(out=decT[:, :, :], in_=cin["c_decT"][:, :, :]), writes=[decT])
            S.dma("sp", lambda e: e.dma_start(out=zeta[:, :], in_=cin["c_zeta"][:, :]), writes=[zeta])
            Qxi = ar.alloc([2, T], BF16)
            Kz = ar.alloc([16, 256], BF16)
            state = ar.alloc([2, 512], F32)
            stateb = ar.alloc([2, 512], BF16)
            onb = [ar.alloc([512], BF16) for _ in range(2)]
            STb = [ar.alloc([128], BF16) for _ in range(2)]
            st6 = ar.alloc([6], F32)
            mv = ar.alloc([2], F32)
            lnv = ar.alloc([1], F32)
            rstd = ar.alloc([1], F32)
            nmr = ar.alloc([1], F32)
            yr = [ar.alloc([4, T], BF16) for _ in range(2)]
            Rvsrc = Rv.rearrange("(tt p) f -> p tt f", p=128)

            def load(h):
                b = h % 2
                S.dma("sp", lambda e: e.dma_start(out=Rq[b][:, :, :], in_=RqT[2 * h:2 * h + 2].rearrange("c p t -> p c t")), writes=[Rq[b]])
                S.dma("sp", lambda e: e.dma_start(out=Rk[b][:, :, :], in_=RkT[2 * h:2 * h + 2].rearrange("c p t -> p c t")), writes=[Rk[b]])
                S.dma("sp", lambda e: e.dma_start(out=Rvt[b][:, :, :], in_=Rvsrc[:, :, sl(h, 512)]), writes=[Rvt[b]])
                S.dma("sp", lambda e: e.dma_start(out=Rg[b][:, :, :], in_=RgT[4 * h:4 * h + 4].rearrange("c p t -> p c t")), writes=[Rg[b]])
                S.dma("sp", lambda e: e.dma_start(out=xi[b][:, :], in_=cin["c_xi"][h, :, :]), writes=[xi[b]])

            load(0)
            for h in range(4):
                if h + 1 < 4:
                    load(h + 1)
                b = h % 2
                rq, rk, rv, rg, x_i, y = Rq[b], Rk[b], Rvt[b], Rg[b], xi[b], yr[b]
                for dc in range(2):
                    S.op("dve", lambda e, dc=dc: e.tensor_tensor(Qxi[:, dc, :], rq[:, dc, :], x_i[:, :], ALU.mult), reads=[rq, x_i], writes=[Qxi])
                for n in range(16):
                    tp = ps[5 + n % 2]
                    for dc in range(2):
                        S.op("pe", lambda e, tp=tp, dc=dc, n=n: e.transpose(tp.bf[:, sl(dc, 128)], rk[:, dc, sl(n, 128)], ident[:, :]), reads=[rk, ident], writes=[tp])
                    S.op("act", lambda e, tp=tp, n=n: e.activation(Kz[:, n, :], tp.bf[:, 0:256], AF.Identity, scale=zeta[:, h:h + 1]), reads=[tp, zeta], writes=[Kz])
                def emit_ST(n):
                    ns = sl(n, 128)
                    STp = ps[0]
                    for dc in range(2):
                        mm(STp, STp[:, 0:128], rk[:, dc, ns], rq[:, dc, ns], dc == 0, dc == 1, [rk, rq])
                    sb = STb[n % 2]
                    S.op("dve", lambda e: e.tensor_tensor(sb[:, :], STp[:, 0:128], decT[:, h, :], ALU.mult), reads=[STp, decT], writes=[sb])

                def emit_O(n):
                    ns = sl(n, 128)
                    sb = STb[n % 2]
                    Op = ps[1 + n % 2]
                    mm(Op, Op[:, :], sb[:, :], rv[:, n, :], True, n == 0, [sb, rv])
                    if n > 0:
                        for dc in range(2):
                            mm(Op, Op[:, :], Qxi[:, dc, ns], stateb[:, dc, :], False, dc == 1, [Qxi, stateb])
                    if n < 15:
                        for dc in range(2):
                            kv = ps[3 + dc]
                            mm(kv, kv[:, :], Kz[:, n, sl(dc, 128)], rv[:, n, :], True, True, [Kz, rv])
                            if n == 0:
                                S.op("dve", lambda e, kv=kv, dc=dc: e.tensor_copy(state[:, dc, :], kv[:, :]), reads=[kv], writes=[state])
                            else:
                                S.op("dve", lambda e, kv=kv, dc=dc: e.scalar_tensor_tensor(state[:, dc, :], state[:, dc, :], CD[h], kv[:, :], ALU.mult, ALU.add), reads=[kv, state], writes=[state])
                        S.op("act", lambda e: e.copy(stateb[:, :, :], state[:, :, :]), reads=[state], writes=[stateb])
                    S.op("dve", lambda e: e.bn_stats(st6[:, :], Op[:, :]), reads=[Op], writes=[st6])
                    S.op("dve", lambda e: e.bn_aggr(mv[:, :], st6[:, :]), reads=[st6], writes=[mv])
                    S.op("act", lambda e: e.activation(lnv[:, :], mv[:, 1:2], AF.Ln, bias=GN_EPS), reads=[mv], writes=[lnv])
                    S.op("act", lambda e: e.activation(rstd[:, :], lnv[:, :], AF.Exp, scale=-0.5), reads=[lnv], writes=[rstd])
                    S.op("dve", lambda e: e.scalar_tensor_tensor(nmr[:, :], mv[:, 0:1], -1.0, rstd[:, :], ALU.mult, ALU.mult), reads=[mv, rstd], writes=[nmr])
                    ob = onb[n % 2]
                    S.op("act", lambda e: e.activation(ob[:, :], Op[:, :], AF.Identity, bias=nmr[:, 0:1], scale=rstd[:, 0:1]), reads=[Op, nmr, rstd], writes=[ob])

                def emit_T(n):
                    ns = sl(n, 128)
                    ob = onb[n % 2]
                    tp = ps[7]
                    for ec in range(4):
                        S.op("pe", lambda e, ec=ec: e.transpose(tp.bf[:, sl(ec, 128)], ob[:, sl(ec, 128)], ident[:, :]), reads=[ob, ident], writes=[tp])
                    S.op("dve", lambda e: e.tensor_tensor(y[:, :, ns], tp.bf[:, 0:512].rearrange("p (a b) -> p a b", a=4), rg[:, :, ns], ALU.mult), reads=[tp, rg], writes=[y])

                emit_ST(0)
                for n in range(16):
                    if n + 1 < 16:
                        emit_ST(n + 1)
                    emit_O(n)
                    if n > 0:
                        emit_T(n - 1)
                emit_T(15)
                S.dma("sp", lambda e, y=y, h=h: e.dma_start(out=YT[8 + 4 * h:12 + 4 * h].rearrange("c p t -> p c t"), in_=y[:, :, :]), reads=[y])

        def phase_D(L):
            memT = ar.alloc([16, 256], BF16)
            S.dma("pool", lambda e: e.dma_start(out=memT[:, :, :], in_=memT_in.rearrange("(kc p) m -> p kc m", p=128)), writes=[memT])
            wb = [ar.alloc([16, 512], BF16) for _ in range(2)]
            kmT = ar.alloc([8, 256], BF16)
            vm = ar.alloc([2, 1024], BF16)
            Mq = [ar.alloc([2, T], BF16) for _ in range(2)]
            PT = [ar.alloc([512], BF16) for _ in range(4)]
            rden = [ar.alloc([512], F32) for _ in range(2)]
            ym = [ar.alloc([2, T], BF16) for _ in range(2)]
            W = w_mem_kv[L]
            for g in range(4):
                wbuf = wb[g % 2]
                load_w512(wbuf, W, g * 512)
                if g < 2:
                    for c in range(4):
                        p = nextps()
                        for kc in range(16):
                            mm(p, p[:, 0:256], wbuf[:, kc, sl(c, 128)], memT[:, kc, :], kc == 0, kc == 15, [wbuf, memT])
                        S.op("act", lambda e, p=p, g=g, c=c: e.copy(kmT[:, g * 4 + c, :], p[:, 0:256]), reads=[p], writes=[kmT])
                else:
                    for mt in range(2):
                        p = nextps()
                        for kc in range(16):
                            mm(p, p[:, :], memT[:, kc, sl(mt, 128)], wbuf[:, kc, :], kc == 0, kc == 15, [wbuf, memT])
                        S.op("dve", lambda e, p=p, g=g, mt=mt: e.tensor_copy(vm[:, mt, sl(g - 2, 512)], p[:, :]), reads=[p], writes=[vm])

            def load(h):
                S.dma("sp", lambda e: e.dma_start(out=Mq[h % 2][:, :, :], in_=MqT[2 * h:2 * h + 2].rearrange("c p t -> p c t")), writes=[Mq[h % 2]])

            load(0)
            npt = 0
            for h in range(4):
                if h + 1 < 4:
                    load(h + 1)
                mq, y = Mq[h % 2], ym[h % 2]
                for g in range(4):
                    pts = []
                    for mt in range(2):
                        Sp = ps[mt]
                        for dc in range(2):
                            mm(Sp, Sp[:, :], kmT[:, 2 * h + dc, sl(mt, 128)], mq[:, dc, sl(g, 512)], dc == 0, dc == 1, [kmT, mq])
                        pt = PT[npt % 4]
                        npt += 1
                        S.op("act", lambda e, Sp=Sp, pt=pt: e.activation(pt[:, :], Sp[:, :], AF.Exp, scale=1.0 / 16.0), reads=[Sp], writes=[pt])
                        pts.append(pt)
                    Dp = ps[4 + g % 2]
                    for mt in range(2):
                        mm(Dp, Dp[:, :], ones[:, :], pts[mt][:, :], mt == 0, mt == 1, [ones, pts[mt]])
                    rd = rden[g % 2]
                    S.op("dve", lambda e, rd=rd, Dp=Dp: e.reciprocal(rd[:, :], Dp[:, :]), reads=[Dp], writes=[rd])
                    for dc in range(2):
                        Op = ps[2 + dc]
                        for mt in range(2):
                            mm(Op, Op[:, :], vm[:, mt, sl(2 * h + dc, 128)], pts[mt][:, :], mt == 0, mt == 1, [vm, pts[mt]])
                        S.op("dve", lambda e, rd=rd, Op=Op, y=y, g=g, dc=dc: e.tensor_tensor(y[:, dc, sl(g, 512)], Op[:, :], rd[:, :], ALU.mult), reads=[Op, rd], writes=[y])
                S.dma("sp", lambda e, y=y, h=h: e.dma_start(out=YT[24 + 2 * h:26 + 2 * h].rearrange("c p t -> p c t"), in_=y[:, :, :]), reads=[y])

        def phase_E(L):
            S.barrier()
            ar.reset()
            yT = ar.alloc([32, T], BF16)
            for c0 in range(0, 32, 8):
                S.dma("sp", lambda e, c0=c0: e.dma_start(out=yT[:, c0:c0 + 8, :], in_=YT[c0:c0 + 8].rearrange("c p t -> p c t")), writes=[yT])
            wbE = [ar.alloc([32, 256], BF16) for _ in range(2)]
            sg = [ar.alloc([3, 2, 512], BF16) for _ in range(2)]
            tmp = [ar.alloc([512], F32) for _ in range(6)]
            mo = [ar.alloc([2, T], BF16) for _ in range(1)]
            it = 0

            def loadw(fg):
                wbuf = wbE[fg % 2]
                cs = slice(fg * 256, fg * 256 + 256)
                S.dma("pool", lambda e: e.dma_start(out=wbuf[:, 0:8, :], in_=p_moba[L][:, cs].rearrange("(kc p) f -> p kc f", p=128)), writes=[wbuf])
                S.dma("pool", lambda e: e.dma_start(out=wbuf[:, 8:24, :], in_=p_ret[L][:, cs].rearrange("(kc p) f -> p kc f", p=128)), writes=[wbuf])
                S.dma("pool", lambda e: e.dma_start(out=wbuf[:, 24:32, :], in_=p_mem[L][:, cs].rearrange("(kc p) f -> p kc f", p=128)), writes=[wbuf])

            loadw(0)
            for fg in range(8):
                if fg + 1 < 8:
                    loadw(fg + 1)
                wbuf = wbE[fg % 2]
                mout = mo[0]
                for tg in range(4):
                    ts = sl(tg, 512)
                    sgt = sg[it % 2]
                    it += 1
                    for br in range(3):
                        S.dma("sp", lambda e, br=br: e.dma_start(out=sgt[:, br, :, :], in_=GT[br * 16 + fg * 2:br * 16 + fg * 2 + 2, :, ts].rearrange("c p t -> p c t")), writes=[sgt])
                    for c in range(2):
                        zs = []
                        for (k0, k1) in ((0, 8), (8, 24), (24, 32)):
                            p = nextps()
                            for kc in range(k0, k1):
                                mm(p, p[:, :], wbuf[:, kc, sl(c, 128)], yT[:, kc, ts], kc == k0, kc == k1 - 1, [wbuf, yT])
                            zs.append(p)
                        t3 = tmp[(c % 2) * 3:(c % 2) * 3 + 3]
                        for br in range(3):
                            S.op("dve", lambda e, br=br: e.tensor_tensor(t3[br][:, :], zs[br][:, :], sgt[:, br, c, :], ALU.mult), reads=[zs[br], sgt], writes=[t3[br]])
                        S.op("pool", lambda e: e.tensor_tensor(t3[0][:, :], t3[0][:, :], t3[1][:, :], ALU.add), reads=[t3[1]], writes=[t3[0]])
                        S.op("pool", lambda e: e.tensor_tensor(mout[:, c, ts], t3[0][:, :], t3[2][:, :], ALU.add), reads=[t3[0], t3[2]], writes=[mout])
                S.dma("sp", lambda e: e.dma_start(out=MT[fg * 2:fg * 2 + 2].rearrange("c p t -> p c t"), in_=mout[:, :, :]), reads=[mout])

        def ln_tail(y, G, Bv, sm, x_dst, xT_dst, tt, x1b, xTt, router=None, part=None):
            if part != "q":
                ln_p(y, G, sm)
            if part != "p":
                ln_q(y, Bv, x_dst, xT_dst, tt, x1b, xTt, router)

        def ln_p(y, G, sm):
            st = sm["st"]
            for fs in range(4):
                S.op("dve", lambda e, fs=fs: e.bn_stats(st[:, fs, :], y[:, sl(fs, 512)]), reads=[y], writes=[st])
            S.op("dve", lambda e: e.bn_aggr(sm["mv"][:, :], st[:, :, :].rearrange("p a b -> p (a b)")), reads=[st], writes=[sm["mv"]])
            S.op("act", lambda e: e.activation(sm["lnv"][:, :], sm["mv"][:, 1:2], AF.Ln, bias=LN_EPS), reads=[sm["mv"]], writes=[sm["lnv"]])
            S.op("act", lambda e: e.activation(sm["rstd"][:, :], sm["lnv"][:, :], AF.Exp, scale=-0.5), reads=[sm["lnv"]], writes=[sm["rstd"]])
            S.op("dve", lambda e: e.scalar_tensor_tensor(sm["nmr"][:, :], sm["mv"][:, 0:1], -1.0, sm["rstd"][:, :], ALU.mult, ALU.mult), reads=[sm["mv"], sm["rstd"]], writes=[sm["nmr"]])
            S.op("act", lambda e: e.activation(y[:, :], y[:, :], AF.Identity, bias=sm["nmr"][:, 0:1], scale=sm["rstd"][:, 0:1]), reads=[sm["nmr"], sm["rstd"]], writes=[y])
            S.op("pool", lambda e: e.tensor_tensor(y[:, :], y[:, :], G[:, :], ALU.mult), reads=[G], writes=[y])

        def ln_q(y, Bv, x_dst, xT_dst, tt, x1b, xTt, router):
            S.op("dve", lambda e: e.tensor_tensor(y[:, :], y[:, :], Bv[:, :], ALU.add), reads=[Bv], writes=[y])
            S.dma("sp", lambda e: e.dma_start(out=x_dst[sl(tt, 128), :], in_=y[:, :]), reads=[y])
            if xT_dst is None:
                return
            S.op("act", lambda e: e.copy(x1b[:, :], y[:, :]), reads=[y], writes=[x1b])
            if router is not None:
                S.dma("sp", lambda e: e.dma_start(out=X1B[sl(tt, 128), :], in_=x1b[:, :]), reads=[x1b])
            for half in range(2):
                tp = ps[4 + half]
                for j in range(8):
                    kc = half * 8 + j
                    S.op("pe", lambda e, tp=tp, j=j, kc=kc: e.transpose(tp.bf[:, sl(j, 128)], x1b[:, sl(kc, 128)], ident[:, :]), reads=[x1b, ident], writes=[tp])
                if half == 0:
                    S.op("act", lambda e, tp=tp: e.copy(xTt[:, 0:8, :], tp.bf[:, :].rearrange("p (a b) -> p a b", a=8)), reads=[tp], writes=[xTt])
                else:
                    S.op("dve", lambda e, tp=tp: e.tensor_copy(xTt[:, 8:16, :], tp.bf[:, :].rearrange("p (a b) -> p a b", a=8)), reads=[tp], writes=[xTt])
            S.dma("sp", lambda e: e.dma_start(out=xT_dst[:, :, sl(tt, 128)].rearrange("c p t -> p c t"), in_=xTt[:, :, :]), reads=[xTt])
            if router is not None:
                router(xTt, tt)

        def ln_small():
            return {"st": ar.alloc([4, 6], F32), "mv": ar.alloc([2], F32), "lnv": ar.alloc([1], F32), "rstd": ar.alloc([1], F32), "nmr": ar.alloc([1], F32)}

        def phase_F(L):
            S.barrier()
            ar.reset()
            wo = [ar.alloc([16, 512], BF16) for _ in range(4)]
            for g in range(4):
                src = w_o[L][:, sl(g, 512)].rearrange("(kc p) f -> p kc f", p=128)
                S.dma("pool", lambda e, src=src, g=g: e.dma_start(out=wo[g][:, :, :], in_=src), writes=[wo[g]])
            G = ar.alloc([D], F32)
            Bv = ar.alloc([D], F32)
            S.dma("sp", lambda e: e.dma_start(out=G[:, :], in_=ln_par[L, 0, :, :]), writes=[G])
            S.dma("sp", lambda e: e.dma_start(out=Bv[:, :], in_=ln_par[L, 1, :, :]), writes=[Bv])
            wr = ar.alloc([16, 36], BF16)
            S.dma("pool", lambda e: e.dma_start(out=wr[:, :, :], in_=w_r[L].rearrange("(kc p) f -> p kc f", p=128)), writes=[wr])
            br = ar.alloc([36], F32)
            S.dma("sp", lambda e: e.dma_start(out=br[:, :], in_=b_r[L, :, :]), writes=[br])
            mT = [ar.alloc([16, 128], BF16) for _ in range(2)]
            xres = [ar.alloc([D], F32) for _ in range(2)]
            ys = [ar.alloc([D], F32) for _ in range(2)]
            x1b = [ar.alloc([D], BF16) for _ in range(2)]
            xTt = [ar.alloc([16, 128], BF16) for _ in range(2)]
            sms = [ln_small() for _ in range(2)]
            lg = ar.alloc([36], F32)
            r_ = {k: ar.alloc([n], F32) for k, n in (("gmax", 1), ("ngmax", 1), ("ge", 4), ("gsum", 1), ("gmask", 4), ("pen", 4),
                                                        ("em", 32), ("mx", 8), ("sel", 32), ("nv1", 1), ("ex", 32), ("e2", 1), ("den", 1), ("rr", 1), ("W", 32))}
            xsrc = x_in if L == 0 else X2

            def router(xt, tt):
                lp = ps[6]
                for kc in range(16):
                    mm(lp, lp[:, 0:36], xt[:, kc, :], wr[:, kc, :], kc == 0, kc == 15, [xt, wr])
                R = r_
                S.op("dve", lambda e: e.tensor_tensor(lg[:, :], lp[:, 0:36], br[:, :], ALU.add), reads=[lp, br], writes=[lg])
                S.op("dve", lambda e: e.tensor_reduce(R["gmax"][:, :], lg[:, 0:4], AX.X, ALU.max), reads=[lg], writes=[R["gmax"]])
                S.op("dve", lambda e: e.tensor_scalar(R["ngmax"][:, :], R["gmax"][:, :], -1.0, None, ALU.mult), reads=[R["gmax"]], writes=[R["ngmax"]])
                S.op("act", lambda e: e.activation(R["ge"][:, :], lg[:, 0:4], AF.Exp, bias=R["ngmax"][:, 0:1], accum_out=R["gsum"][:, 0:1]), reads=[lg, R["ngmax"]], writes=[R["ge"], R["gsum"]])
                S.op("dve", lambda e: e.tensor_scalar(R["gmask"][:, :], lg[:, 0:4], R["gmax"][:, 0:1], None, ALU.is_ge), reads=[lg, R["gmax"]], writes=[R["gmask"]])
                S.op("dve", lambda e: e.tensor_scalar(R["pen"][:, :], R["gmask"][:, :], -1.0, 1e30, ALU.add, ALU.mult), reads=[R["gmask"]], writes=[R["pen"]])
                for g in range(4):
                    S.op("dve", lambda e, g=g: e.tensor_scalar(R["em"][:, sl(g, 8)], lg[:, 4 + g * 8:12 + g * 8], R["pen"][:, g:g + 1], None, ALU.add), reads=[lg, R["pen"]], writes=[R["em"]])
                S.op("dve", lambda e: e.max(R["mx"][:, :], R["em"][:, :]), reads=[R["em"]], writes=[R["mx"]])
                S.op("dve", lambda e: e.tensor_scalar(R["sel"][:, :], R["em"][:, :], R["mx"][:, 1:2], None, ALU.is_ge), reads=[R["em"], R["mx"]], writes=[R["sel"]])
                S.op("dve", lambda e: e.tensor_scalar(R["nv1"][:, :], R["mx"][:, 0:1], -1.0, None, ALU.mult), reads=[R["mx"]], writes=[R["nv1"]])
                S.op("act", lambda e: e.activation(R["ex"][:, :], R["em"][:, :], AF.Exp, bias=R["nv1"][:, 0:1]), reads=[R["em"], R["nv1"]], writes=[R["ex"]])
                S.op("act", lambda e: e.activation(R["e2"][:, :], R["mx"][:, 1:2], AF.Exp, bias=R["nv1"][:, 0:1]), reads=[R["mx"], R["nv1"]], writes=[R["e2"]])
                S.op("dve", lambda e: e.scalar_tensor_tensor(R["den"][:, :], R["e2"][:, :], 1.0, R["gsum"][:, :], ALU.add, ALU.mult), reads=[R["e2"], R["gsum"]], writes=[R["den"]])
                S.op("dve", lambda e: e.reciprocal(R["rr"][:, :], R["den"][:, :]), reads=[R["den"]], writes=[R["rr"]])
                S.op("act", lambda e: e.copy(selS_all[:, tt, :], R["sel"][:, :]), reads=[R["sel"]], writes=[selS_all])
                S.op("dve", lambda e: e.tensor_scalar(selA_all[:, tt, :], R["em"][:, :], R["mx"][:, 0:1], None, ALU.is_ge), reads=[R["em"], R["mx"]], writes=[selA_all])
                S.op("act", lambda e: e.copy(wab_all[:, tt, 0:1], R["rr"][:, :]), reads=[R["rr"]], writes=[wab_all])
                S.op("dve", lambda e: e.tensor_tensor(wab_all[:, tt, 1:2], R["rr"][:, :], R["e2"][:, :], ALU.mult), reads=[R["rr"], R["e2"]], writes=[wab_all])

            def load(tt):
                b = tt % 2
                S.dma("sp", lambda e: e.dma_start(out=mT[b][:, :, :], in_=MT[:, :, sl(tt, 128)].rearrange("c p t -> p c t")), writes=[mT[b]])
                S.dma("sp", lambda e: e.dma_start(out=xres[b][:, :], in_=xsrc[sl(tt, 128), :]), writes=[xres[b]])

            def mm_pe(tt):
                b = tt % 2
                for fs in range(4):
                    p = ps[fs]
                    for kc in range(16):
                        mm(p, p[:, :], mT[b][:, kc, :], wo[fs][:, kc, :], kc == 0, kc == 15, [mT[b], wo[fs]])

            def mm_stt(tt):
                b = tt % 2
                y = ys[b]
                for fs in range(4):
                    p = ps[fs]
                    S.op("dve", lambda e, p=p, fs=fs: e.scalar_tensor_tensor(y[:, sl(fs, 512)], xres[b][:, sl(fs, 512)], ALPHA, p[:, :], ALU.mult, ALU.add), reads=[p, xres[b]], writes=[y])

            load(0)
            load(1)
            mm_pe(0)
            mm_stt(0)
            for tt in range(16):
                b = tt % 2
                if tt + 1 < 16:
                    mm_pe(tt + 1)
                ln_tail(ys[b], G, Bv, sms[b], X1, X1T, tt, x1b[b], xTt[b], router, part="p")
                if tt > 0:
                    pb = (tt - 1) % 2
                    ln_tail(ys[pb], G, Bv, sms[pb], X1, X1T, tt - 1, x1b[pb], xTt[pb], router, part="q")
                if tt + 1 < 16:
                    mm_stt(tt + 1)
                if tt + 2 < 16:
                    load(tt + 2)
            ln_tail(ys[1], G, Bv, sms[1], X1, X1T, 15, x1b[1], xTt[1], router, part="q")
            cnt = ar.alloc([32], F32)
            accs = [ar.alloc([32], F32) for _ in range(2)]
            cps = ps[6]
            for tt in range(16):
                mm(cps, cps[:, 0:32], ones[:, :], selS_all[:, tt, :], tt == 0, tt == 15, [ones, selS_all])
            S.op("dve", lambda e: e.tensor_copy(cnt[:, :], cps[:, 0:32]), reads=[cps], writes=[cnt])
            S.op("dve", lambda e: e.tensor_scalar(accs[0][:, :], cnt[:, :], 0.5, None, ALU.is_gt), reads=[cnt], writes=[accs[0]])
            NJ = T // BLK_ROWS
            for j in range(1, NJ):
                S.op("dve", lambda e, j=j: e.scalar_tensor_tensor(accs[j % 2][:, :], cnt[:, :], float(BLK_ROWS) * j + 0.5, accs[(j + 1) % 2][:, :], ALU.is_gt, ALU.add), reads=[cnt, accs[(j + 1) % 2]], writes=[accs[j % 2]])
            padded = ar.alloc([32], F32)
            S.op("dve", lambda e: e.tensor_scalar(padded[:, :], accs[(NJ - 1) % 2][:, :], float(BLK_ROWS), None, ALU.mult), reads=[accs[(NJ - 1) % 2]], writes=[padded])
            cs = [ar.alloc([32], F32) for _ in range(2)]
            S.op("dve", lambda e: e.tensor_copy(cs[0][:, :], padded[:, :]), reads=[padded], writes=[cs[0]])
            k = 0
            for dsh in (1, 2, 4, 8, 16):
                a_, b_ = cs[k % 2], cs[(k + 1) % 2]
                S.op("dve", lambda e, a_=a_, b_=b_, dsh=dsh: e.tensor_copy(b_[:, 0:dsh], a_[:, 0:dsh]), reads=[a_], writes=[b_])
                S.op("dve", lambda e, a_=a_, b_=b_, dsh=dsh: e.tensor_tensor(b_[:, dsh:32], a_[:, dsh:32], a_[:, 0:32 - dsh], ALU.add), reads=[a_], writes=[b_])
                k += 1
            pend = cs[k % 2]
            pstart = ar.alloc([32], F32)
            S.op("dve", lambda e: e.tensor_tensor(pstart[:, :], pend[:, :], padded[:, :], ALU.subtract), reads=[pend, padded], writes=[pstart])
            thr = ar.alloc([N_BLK], F32)
            S.dma("sp", lambda e: e.dma_start(out=thr[:, :], in_=cin["c_thr"][:, :]), writes=[thr])
            bacc = [ar.alloc([N_BLK], F32) for _ in range(2)]
            S.op("dve", lambda e: e.tensor_scalar(bacc[0][:, :], thr[:, :], pend[:, 0:1], None, ALU.is_ge), reads=[thr, pend], writes=[bacc[0]])
            for ex in range(1, 32):
                S.op("dve", lambda e, ex=ex: e.scalar_tensor_tensor(bacc[ex % 2][:, :], thr[:, :], pend[:, ex:ex + 1], bacc[(ex + 1) % 2][:, :], ALU.is_ge, ALU.add), reads=[thr, pend, bacc[(ex + 1) % 2]], writes=[bacc[ex % 2]])
            S.op("dve", lambda e: e.tensor_scalar(blkE[:, :], bacc[1][:, :], 31.0, None, ALU.min), reads=[bacc[1]], writes=[blkE])
            S.op("dve", lambda e: e.tensor_scalar(blkU[:, :], thr[:, :], pend[:, 31:32], OOB, ALU.is_ge, ALU.mult), reads=[thr, pend], writes=[blkU])
            dtmp = ar.alloc([32], F32)
            dprod = ar.alloc([32], F32)
            selB = ar.alloc([32], F32)
            dflt = ar.alloc([16, 2], F32)
            for tt in range(16):
                rp = ps[tt % 2]
                for t2 in range(tt):
                    mm(rp, rp[:, 0:32], ones[:, :], selS_all[:, t2, :], t2 == 0, False, [ones, selS_all])
                mm(rp, rp[:, 0:32], ustrict[:, :], selS_all[:, tt, :], tt == 0, True, [ustrict, selS_all])
                S.op("dve", lambda e, rp=rp: e.tensor_tensor(dtmp[:, :], rp[:, 0:32], pstart[:, :], ALU.add), reads=[rp, pstart], writes=[dtmp])
                S.op("dve", lambda e, tt=tt: e.tensor_tensor(dprod[:, :], dtmp[:, :], selA_all[:, tt, :], ALU.mult), reads=[dtmp, selA_all], writes=[dprod])
                S.op("dve", lambda e, tt=tt: e.tensor_reduce(dflt[:, tt, 0:1], dprod[:, :], AX.X, ALU.add), reads=[dprod], writes=[dflt])
                S.op("dve", lambda e, tt=tt: e.tensor_tensor(selB[:, :], selS_all[:, tt, :], selA_all[:, tt, :], ALU.subtract), reads=[selS_all, selA_all], writes=[selB])
                S.op("dve", lambda e: e.tensor_tensor(dprod[:, :], dtmp[:, :], selB[:, :], ALU.mult), reads=[dtmp, selB], writes=[dprod])
                S.op("dve", lambda e, tt=tt: e.tensor_reduce(dflt[:, tt, 1:2], dprod[:, :], AX.X, ALU.add), reads=[dprod], writes=[dflt])
            S.op("dve", lambda e: e.tensor_copy(dest_i[:, :, :], dflt[:, :, :]), reads=[dflt], writes=[dest_i])
            if debug:
                S.dma("sp", lambda e: e.dma_start(out=DBG_dest[:, :], in_=dest_i[:, :, :].rearrange("p a b -> p (a b)")), reads=[dest_i])
                S.dma("sp", lambda e: e.dma_start(out=DBG_blk[:, :], in_=blkE[:, :]), reads=[blkE])
                S.dma("sp", lambda e: e.dma_start(out=DBG_wab[:, :], in_=wab_all[:, :, :].rearrange("p a b -> p (a b)")), reads=[wab_all])

        def phase_X(L):
            S.barrier()
            ar.reset()
            xb = [ar.alloc([D], BF16) for _ in range(3)]
            for tt in range(16):
                b = xb[tt % 3]
                S.dma("sp", lambda e, b=b, tt=tt: e.dma_start(out=b[:, :], in_=X1B[sl(tt, 128), :]), writes=[b])
                for s_ in range(2):
                    S.dma("pool", lambda e, b=b, tt=tt, s_=s_: e.indirect_dma_start(
                        out=Xs[:, :], out_offset=bass.IndirectOffsetOnAxis(ap=dest_i[:, tt, s_:s_ + 1], axis=0),
                        in_=b[:, :], in_offset=None), reads=[b, dest_i])

        def phase_M(L):
            S.barrier()
            ar.reset()
            base = ar.alloc([12], F32)
            S.dma("sp", lambda e: e.dma_start(out=base[:, :], in_=cin["c_base"][:, :]), writes=[base])
            b1024 = ar.alloc([N_BLK], F32)
            b512 = ar.alloc([N_BLK], F32)
            S.op("dve", lambda e: e.tensor_scalar(b1024[:, :], blkE[:, :], 1024.0, float(L * N_EXP * 1024), ALU.mult, ALU.add), reads=[blkE], writes=[b1024])
            S.op("dve", lambda e: e.tensor_scalar(b512[:, :], blkE[:, :], 512.0, float(L * N_EXP * 512), ALU.mult, ALU.add), reads=[blkE], writes=[b512])
            S.op("dve", lambda e: e.tensor_tensor(b1024[:, :], b1024[:, :], blkU[:, :], ALU.add), reads=[blkU], writes=[b1024])
            S.op("dve", lambda e: e.tensor_tensor(b512[:, :], b512[:, :], blkU[:, :], ALU.add), reads=[blkU], writes=[b512])
            idf = ar.alloc([N_BLK, 12], F32)
            idi = ar.alloc([N_BLK, 12], I32)
            for j in range(12):
                srcb = b1024 if j < 8 else b512
                S.op("dve", lambda e, j=j, srcb=srcb: e.tensor_scalar(idf[:, :, j], srcb[:, :], base[:, j:j + 1], None, ALU.add), reads=[srcb, base], writes=[idf])
            S.op("dve", lambda e: e.tensor_copy(idi[:, :, :], idf[:, :, :]), reads=[idf], writes=[idi])
            NWB = 3
            wgu = [ar.alloc([8, 2048], BF16) for _ in range(NWB)]
            wdn = [ar.alloc([4, 2048], BF16) for _ in range(NWB)]
            xb = [ar.alloc([D], BF16) for _ in range(2)]
            xbT = [ar.alloc([16, 128], BF16) for _ in range(2)]
            sg = [ar.alloc([512], F32) for _ in range(2)]
            actb = [ar.alloc([512], BF16) for _ in range(2)]
            actT = [ar.alloc([4, 128], BF16) for _ in range(2)]
            ysb = [ar.alloc([D], F32) for _ in range(2)]
            RT = BLK_ROWS // 128
            order = []
            lo, hi = 0, N_BLK - 1
            while lo <= hi:
                for _ in range(2):
                    if lo <= hi:
                        order.append(lo)
                        lo += 1
                if lo <= hi:
                    order.append(hi)
                    hi -= 1
            assert sorted(order) == list(range(N_BLK))
            rows = [(bk * RT + rt, pos % NWB) for pos, bk in enumerate(order) for rt in range(RT)]
            NR = len(rows)

            def loadw(pos):
                bk = order[pos]
                i = pos % NWB
                for j in range(8):
                    S.dma("pool", lambda e, j=j: e.indirect_dma_start(out=wgu[i][:, j, :], out_offset=None, in_=w_gup[:, :],
                                                                     in_offset=bass.IndirectOffsetOnAxis(ap=idi[:, bk, j:j + 1], axis=0),
                                                                     bounds_check=RegConst(DEPTH * N_EXP * 128 * 8 - 1), oob_is_err=False), reads=[idi], writes=[wgu[i]])
                for j in range(4):
                    S.dma("pool", lambda e, j=j: e.indirect_dma_start(out=wdn[i][:, j, :], out_offset=None, in_=w_dnp[:, :],
                                                                     in_offset=bass.IndirectOffsetOnAxis(ap=idi[:, bk, 8 + j:9 + j], axis=0),
                                                                     bounds_check=RegConst(DEPTH * N_EXP * 128 * 4 - 1), oob_is_err=False), reads=[idi], writes=[wdn[i]])

            def loadx(n):
                S.dma("sp", lambda e: e.dma_start(out=xb[n % 2][:, :], in_=Xs[sl(rows[n][0], 128), :]), writes=[xb[n % 2]])

            def st1(n):
                x_, xt = xb[n % 2], xbT[n % 2]
                for half in range(2):
                    tp = ps[6 + half]
                    for j in range(8):
                        kc = half * 8 + j
                        S.op("pe", lambda e, tp=tp, j=j, kc=kc: e.transpose(tp.bf[:, sl(j, 128)], x_[:, sl(kc, 128)], ident[:, :]), reads=[x_, ident], writes=[tp])
                    if half == 0:
                        S.op("act", lambda e, tp=tp: e.copy(xt[:, 0:8, :], tp.bf[:, :].rearrange("p (a b) -> p a b", a=8)), reads=[tp], writes=[xt])
                    else:
                        S.op("dve", lambda e, tp=tp: e.tensor_copy(xt[:, 8:16, :], tp.bf[:, :].rearrange("p (a b) -> p a b", a=8)), reads=[tp], writes=[xt])

            def st2(n):
                i = rows[n][1]
                k = n % 2
                wg = wgu[i][:, :, :].rearrange("p a (b c) -> p (a b) c", b=2)
                xt = xbT[k]
                gp, up = ps[0], ps[1]
                for kc in range(16):
                    mm(gp, gp[:, :], xt[:, kc, :], wg[:, kc, 0:512], kc == 0, kc == 15, [xt, wgu[i]])
                for kc in range(16):
                    mm(up, up[:, :], xt[:, kc, :], wg[:, kc, 512:1024], kc == 0, kc == 15, [xt, wgu[i]])
                S.op("act", lambda e: e.activation(sg[k][:, :], gp[:, :], AF.Silu), reads=[gp], writes=[sg[k]])
                S.op("dve", lambda e: e.tensor_tensor(actb[k][:, :], up[:, :], sg[k][:, :], ALU.mult), reads=[up, sg[k]], writes=[actb[k]])

            def st3(n):
                k = n % 2
                tp = ps[2]
                for c in range(4):
                    S.op("pe", lambda e, c=c: e.transpose(tp.bf[:, sl(c, 128)], actb[k][:, sl(c, 128)], ident[:, :]), reads=[actb[k], ident], writes=[tp])
                S.op("act", lambda e: e.copy(actT[k][:, :, :], tp.bf[:, 0:512].rearrange("p (a b) -> p a b", a=4)), reads=[tp], writes=[actT[k]])

            def st4(n):
                r, i = rows[n]
                k = n % 2
                wd, at, yb = wdn[i], actT[k], ysb[k]
                for dg in range(4):
                    yp = nextps_m()
                    for c in range(4):
                        mm(yp, yp[:, :], at[:, c, :], wd[:, c, sl(dg, 512)], c == 0, c == 3, [at, wd])
                    if dg % 2 == 0:
                        S.op("act", lambda e, yp=yp, dg=dg: e.copy(yb[:, sl(dg, 512)], yp[:, :]), reads=[yp], writes=[yb])
                    else:
                        S.op("dve", lambda e, yp=yp, dg=dg: e.tensor_copy(yb[:, sl(dg, 512)], yp[:, :]), reads=[yp], writes=[yb])
                S.dma("sp", lambda e: e.dma_start(out=Ys[sl(r, 128), :], in_=yb[:, :]), reads=[yb])

            loadw(0)
            loadw(1)
            loadx(0)
            loadx(1)
            st1(0)
            for n in range(NR):
                st2(n)
                if n > 0:
                    st4(n - 1)
                if n % RT == 0 and n // RT + 2 < N_BLK:
                    loadw(n // RT + 2)
                if n + 1 < NR:
                    st1(n + 1)
                if n + 2 < NR:
                    loadx(n + 2)
                st3(n)
            st4(NR - 1)

        def phase_G(L, last):
            S.barrier()
            ar.reset()
            G = ar.alloc([D], F32)
            Bv = ar.alloc([D], F32)
            S.dma("sp", lambda e: e.dma_start(out=G[:, :], in_=ln_par[L, 2, :, :]), writes=[G])
            S.dma("sp", lambda e: e.dma_start(out=Bv[:, :], in_=ln_par[L, 3, :, :]), writes=[Bv])
            x1 = [ar.alloc([D], F32) for _ in range(3)]
            ff = [ar.alloc([D], F32) for _ in range(3)]
            fb = [ar.alloc([D], F32) for _ in range(3)]
            x1b = [ar.alloc([D], BF16) for _ in range(2)]
            xTt = [ar.alloc([16, 128], BF16) for _ in range(2)]
            sms = [ln_small() for _ in range(2)]

            def load(tt):
                b = tt % 3
                S.dma("sp", lambda e: e.dma_start(out=x1[b][:, :], in_=X1[sl(tt, 128), :]), writes=[x1[b]])
                S.dma("pool", lambda e: e.indirect_dma_start(out=ff[b][:, :], out_offset=None, in_=Ys[:, :], in_offset=bass.IndirectOffsetOnAxis(ap=dest_i[:, tt, 0:1], axis=0)), reads=[dest_i], writes=[ff[b]])
                S.dma("pool", lambda e: e.indirect_dma_start(out=fb[b][:, :], out_offset=None, in_=Ys[:, :], in_offset=bass.IndirectOffsetOnAxis(ap=dest_i[:, tt, 1:2], axis=0)), reads=[dest_i], writes=[fb[b]])

            dst_x = out if last else X2
            dst_xT = None if last else X2T

            def P(tt):
                b = tt % 3
                y = ff[b]
                S.op("act", lambda e: e.activation(y[:, :], y[:, :], AF.Identity, scale=wab_all[:, tt, 0:1]), reads=[wab_all], writes=[y])
                S.op("dve", lambda e: e.scalar_tensor_tensor(y[:, :], fb[b][:, :], wab_all[:, tt, 1:2], y[:, :], ALU.mult, ALU.add), reads=[fb[b], wab_all], writes=[y])
                S.op("dve", lambda e: e.scalar_tensor_tensor(y[:, :], x1[b][:, :], ALPHA, y[:, :], ALU.mult, ALU.add), reads=[x1[b]], writes=[y])
                ln_tail(y, G, Bv, sms[tt % 2], dst_x, dst_xT, tt, x1b[tt % 2], xTt[tt % 2], part="p")

            def Q(tt):
                ln_tail(ff[tt % 3], G, Bv, sms[tt % 2], dst_x, dst_xT, tt, x1b[tt % 2], xTt[tt % 2], part="q")

            load(0)
            load(1)
            load(2)
            P(0)
            for tt in range(16):
                if tt + 1 < 16:
                    P(tt + 1)
                Q(tt)
                if tt + 3 < 16:
                    load(tt + 3)

        for L in range(n_layers):
            phase_A(L)
            phase_B(L)
            phase_D(L)
            phase_C(L)
            phase_E(L)
            phase_F(L)
            if no_indirect:
                continue
            phase_X(L)
            if stop_after == "X":
                continue
            phase_M(L)
            if stop_after == "M":
                continue
            phase_G(L, L == n_layers - 1)
        S.finish()
        S.emit()
    return nc, consts


_CACHE = {}


def _prep_shared(inp):
    f = lambda a: np.ascontiguousarray(np.asarray(a, dtype=np.float32))
    sh = {}
    for k in ["w_in", "p_moba", "p_ret", "p_mem", "w_mem_kv", "w_o"]:
        sh[k] = f(inp[k])
    g = f(inp["w_gate_up"]).reshape(DEPTH, N_EXP, 16, 128, 1024).transpose(0, 1, 3, 2, 4)
    sh["w_gup"] = np.ascontiguousarray(g).reshape(DEPTH * N_EXP * 128 * 8, 2048)
    dn = f(inp["w_down"]).reshape(DEPTH, N_EXP, 4, 128, D).transpose(0, 1, 3, 2, 4)
    sh["w_dnp"] = np.ascontiguousarray(dn).reshape(DEPTH * N_EXP * 128 * 4, 2048)
    lnp = np.stack([f(inp["ln1_g"]), f(inp["ln1_b"]), f(inp["ln2_g"]), f(inp["ln2_b"])], 1)
    sh["ln_par"] = np.ascontiguousarray(np.broadcast_to(lnp[:, :, None, :], (DEPTH, 4, 128, D)))
    sh["w_r"] = np.ascontiguousarray(np.concatenate([f(inp["w_group"]), f(inp["w_expert"])], -1))
    br = np.concatenate([f(inp["b_group"]), f(inp["b_expert"])], -1)
    sh["b_r"] = np.ascontiguousarray(np.broadcast_to(br[:, None, :], (DEPTH, 128, 36)))
    return sh


def kernel(**inputs):
    if "nc" not in _CACHE:
        _CACHE["nc"] = build()
    nc, consts = _CACHE["nc"]
    sh = _prep_shared(inputs)
    x = np.asarray(inputs["x"], dtype=np.float32)
    mem = np.asarray(inputs["mem"], dtype=np.float32)
    in_maps = []
    for b in range(8):
        m = dict(sh)
        m.update(consts)
        m["x"] = np.ascontiguousarray(x[b])
        m["xT"] = np.ascontiguousarray(x[b].T)
        m["memT"] = np.ascontiguousarray(mem[b].T)
        in_maps.append(m)
    res = run_bass_kernel_spmd(nc, in_maps, core_ids=list(range(8)))
    return np.stack([np.asarray(r["out"], dtype=np.float32) for r in res.results], 0)
```
